# Optimizing a Trainium2 kernel written in Bass

```python
import math
import jax, jax.numpy as jnp
from jax import lax
import numpy as np

D_MODEL = 1024
BATCH = 8
SEQ = 4096
DEPTH = 2

CHUNK = 64
N_MIXERS = 2
N_SSD_LAYERS = (DEPTH + 1) // 2
N_ATT_LAYERS = DEPTH // 2

SSD_EXPAND = 2
D_INNER = SSD_EXPAND * D_MODEL
SSD_HEAD_DIM = 64
SSD_HEADS = D_INNER // SSD_HEAD_DIM
SSD_GROUPS = 4
SSD_HEADS_PER_GROUP = SSD_HEADS // SSD_GROUPS
SSD_STATE = 128
SSD_CONV = 4
SSD_CONV_DIM = D_INNER + 2 * SSD_GROUPS * SSD_STATE
SSD_IN_DIM = 2 * D_INNER + 2 * SSD_GROUPS * SSD_STATE + SSD_HEADS

ATT_HEADS = 16
ATT_HEAD_DIM = D_MODEL // ATT_HEADS
LEFT_CHUNKS = 8
BAND = (LEFT_CHUNKS + 1) * CHUNK
MAX_REL = 128

N_EXPERTS = 16
N_EXPERT_GROUPS = 4
EXPERTS_PER_GROUP = N_EXPERTS // N_EXPERT_GROUPS
TOP_K = 2
D_FF_EXPERT = 512

DEEPNORM_ALPHA = (2.0 * DEPTH) ** 0.25
DEEPNORM_BETA = (8.0 * DEPTH) ** -0.25
LN_EPS = 1e-5
RMS_EPS = 1e-5

kernel_name = "hybrid_ssd_chunkattn_groupmoe_deepnorm"


def layer_norm(x, g, b):
    xf = x.astype(jnp.float32)
    mu = jnp.mean(xf, axis=-1, keepdims=True)
    var = jnp.mean(jnp.square(xf - mu), axis=-1, keepdims=True)
    out = (xf - mu) * lax.rsqrt(var + LN_EPS) * g.astype(jnp.float32) + b.astype(jnp.float32)
    return out.astype(x.dtype)


def causal_depthwise_conv(u, w, b):
    k_w = w.shape[0]
    s = u.shape[1]
    up = jnp.pad(u, ((0, 0), (k_w - 1, 0), (0, 0)))
    return sum(up[:, k:k + s] * w[k] for k in range(k_w)) + b


def ssd_mixer(x, w_in, conv_w, conv_b, dt_bias, a_log, d_skip, norm_w, w_out):
    bsz, s, _ = x.shape
    nc = s // CHUNK
    g, hg, p, n = SSD_GROUPS, SSD_HEADS_PER_GROUP, SSD_HEAD_DIM, SSD_STATE
    f32 = jnp.float32
    zxbcdt = x @ w_in
    z, xbc, dt = jnp.split(zxbcdt, [D_INNER, D_INNER + SSD_CONV_DIM], axis=-1)
    xbc = jax.nn.silu(causal_depthwise_conv(xbc, conv_w, conv_b))
    xs, bm, cm = jnp.split(xbc, [D_INNER, D_INNER + g * n], axis=-1)
    xs = xs.astype(f32).reshape(bsz, nc, CHUNK, g, hg, p)
    bm = bm.astype(f32).reshape(bsz, nc, CHUNK, g, n)
    cm = cm.astype(f32).reshape(bsz, nc, CHUNK, g, n)
    dt = jax.nn.softplus(dt.astype(f32) + dt_bias.astype(f32)).reshape(bsz, nc, CHUNK, g, hg)
    a = -jnp.exp(a_log.astype(f32)).reshape(g, hg)
    da = dt * a
    causal = jnp.tril(jnp.ones((CHUNK, CHUNK), dtype=bool))

    def step(h, inp):
        xc, bc, cc, dtc, dac = inp
        cum = jnp.cumsum(dac, axis=1)
        seg = cum[:, :, None] - cum[:, None, :]
        decay = jnp.exp(jnp.where(causal[None, :, :, None, None], seg, -jnp.inf))
        cb = jnp.einsum('bign,bjgn->bijg', cc, bc)
        w_ij = cb[..., None] * decay * dtc[:, None]
        y = jnp.einsum('bijgh,bjghp->bighp', w_ij, xc)
        y = y + jnp.einsum('bign,bghpn->bighp', cc, h) * jnp.exp(cum)[..., None]
        last = cum[:, -1]
        wj = jnp.exp(last[:, None] - cum) * dtc
        h = h * jnp.exp(last)[..., None, None] + jnp.einsum('bjgn,bjgh,bjghp->bghpn', bc, wj, xc)
        return h, y

    h0 = jnp.zeros((bsz, g, hg, p, n), f32)
    inputs = tuple(jnp.moveaxis(t, 1, 0) for t in (xs, bm, cm, dt, da))
    _, ys = lax.scan(step, h0, inputs)
    ys = jnp.moveaxis(ys, 0, 1)
    y = ys + d_skip.astype(f32).reshape(g, hg)[..., None] * xs
    y = y.reshape(bsz, s, D_INNER) * jax.nn.silu(z.astype(f32))
    yg = y.reshape(bsz, s, g, D_INNER // g)
    yg = yg * lax.rsqrt(jnp.mean(jnp.square(yg), axis=-1, keepdims=True) + RMS_EPS)
    y = yg.reshape(bsz, s, D_INNER) * norm_w.astype(f32)
    return y.astype(x.dtype) @ w_out


def chunked_attention(x, w_qkv, b_qkv, rel_table, w_o, b_o):
    bsz, s, _ = x.shape
    nc = s // CHUNK
    pad = LEFT_CHUNKS * CHUNK
    qkv = x @ w_qkv + b_qkv
    q, k, v = jnp.split(qkv, 3, axis=-1)
    q = q.reshape(bsz, s, ATT_HEADS, ATT_HEAD_DIM) * (ATT_HEAD_DIM ** -0.5)
    k = k.reshape(bsz, s, ATT_HEADS, ATT_HEAD_DIM)
    v = v.reshape(bsz, s, ATT_HEADS, ATT_HEAD_DIM)
    k_pad = jnp.pad(k, ((0, 0), (pad, 0), (0, 0), (0, 0)))
    v_pad = jnp.pad(v, ((0, 0), (pad, 0), (0, 0), (0, 0)))
    q_chunks = jnp.moveaxis(q.reshape(bsz, nc, CHUNK, ATT_HEADS, ATT_HEAD_DIM), 1, 0)
    qi = jnp.arange(CHUNK)
    kj = jnp.arange(BAND)
    dist = qi[:, None] + pad - kj[None, :]
    rel_idx = jnp.clip(dist, -MAX_REL, MAX_REL) + MAX_REL
    bias = jnp.transpose(rel_table[rel_idx], (2, 0, 1)).astype(jnp.float32)

    def one_chunk(args):
        qc, c = args
        start = c * CHUNK
        kb = lax.dynamic_slice_in_dim(k_pad, start, BAND, axis=1)
        vb = lax.dynamic_slice_in_dim(v_pad, start, BAND, axis=1)
        key_pos = start - pad + kj
        sc = jnp.einsum('bqhd,bkhd->bhqk', qc, kb).astype(jnp.float32) + bias
        sc = jnp.where((key_pos >= 0)[None, None, None, :], sc, -jnp.inf)
        pr = jax.nn.softmax(sc, axis=-1).astype(vb.dtype)
        return jnp.einsum('bhqk,bkhd->bqhd', pr, vb)

    out = lax.map(one_chunk, (q_chunks, jnp.arange(nc)))
    out = jnp.moveaxis(out, 0, 1).reshape(bsz, s, D_MODEL)
    return out @ w_o + b_o


def group_limited_moe(x, router_w, router_bias, w_gate, w_up, w_down):
    bsz, s, dm = x.shape
    t = x.reshape(-1, dm)
    scores = jax.nn.softmax((t @ router_w).astype(jnp.float32), axis=-1)
    sel = scores + router_bias.astype(jnp.float32)
    grp_top = lax.top_k(sel.reshape(-1, N_EXPERT_GROUPS, EXPERTS_PER_GROUP), TOP_K)[0]
    grp = jnp.argmax(jnp.sum(grp_top, axis=-1), axis=-1)
    expert_group = jnp.arange(N_EXPERTS) // EXPERTS_PER_GROUP
    masked = jnp.where(expert_group[None, :] == grp[:, None], sel, -jnp.inf)
    _, idx = lax.top_k(masked, TOP_K)
    gate = jnp.take_along_axis(scores, idx, axis=-1)
    gate = gate / jnp.sum(gate, axis=-1, keepdims=True)
    combine = jnp.sum(jax.nn.one_hot(idx, N_EXPERTS, dtype=jnp.float32) * gate[..., None], axis=1)
    out = jnp.zeros_like(t)
    for e in range(N_EXPERTS):
        h = jax.nn.silu(t @ w_gate[e]) * (t @ w_up[e])
        out = out + combine[:, e:e + 1].astype(t.dtype) * (h @ w_down[e])
    return out.reshape(bsz, s, dm)


def setup_inputs(seed: int = 0) -> dict:
    key = jax.random.key(seed)
    keys = jax.random.split(key, 24)
    f32 = jnp.float32

    def nrm(k, shape, scale):
        return jax.random.normal(k, shape, f32) * scale

    dt0 = jnp.exp(jax.random.uniform(keys[4], (N_SSD_LAYERS, SSD_HEADS), f32)
                  * (math.log(0.1) - math.log(1e-3)) + math.log(1e-3))
    ssm_dt_bias = dt0 + jnp.log(-jnp.expm1(-dt0))
    att_w_qkv = nrm(keys[9], (N_ATT_LAYERS, D_MODEL, 3 * D_MODEL), D_MODEL ** -0.5)
    att_w_qkv = att_w_qkv.at[..., 2 * D_MODEL:].multiply(DEEPNORM_BETA)
    return {
        "x": nrm(keys[0], (BATCH, SEQ, D_MODEL), 1.0),
        "ssm_w_in": nrm(keys[1], (N_SSD_LAYERS, D_MODEL, SSD_IN_DIM), D_MODEL ** -0.5),
        "ssm_conv_w": nrm(keys[2], (N_SSD_LAYERS, SSD_CONV, SSD_CONV_DIM), SSD_CONV ** -0.5),
        "ssm_conv_b": nrm(keys[3], (N_SSD_LAYERS, SSD_CONV_DIM), 0.02),
        "ssm_dt_bias": ssm_dt_bias,
        "ssm_a_log": jnp.log(jax.random.uniform(keys[5], (N_SSD_LAYERS, SSD_HEADS), f32, 1.0, 16.0)),
        "ssm_d": 1.0 + nrm(keys[6], (N_SSD_LAYERS, SSD_HEADS), 0.02),
        "ssm_norm_w": 1.0 + nrm(keys[7], (N_SSD_LAYERS, D_INNER), 0.02),
        "ssm_w_out": nrm(keys[8], (N_SSD_LAYERS, D_INNER, D_MODEL), D_INNER ** -0.5 * DEEPNORM_BETA),
        "att_w_qkv": att_w_qkv,
        "att_b_qkv": nrm(keys[10], (N_ATT_LAYERS, 3 * D_MODEL), 0.02),
        "att_rel_bias": nrm(keys[11], (N_ATT_LAYERS, 2 * MAX_REL + 1, ATT_HEADS), 0.5),
        "att_w_o": nrm(keys[12], (N_ATT_LAYERS, D_MODEL, D_MODEL), D_MODEL ** -0.5 * DEEPNORM_BETA),
        "att_b_o": nrm(keys[13], (N_ATT_LAYERS, D_MODEL), 0.02),
        "router_w": nrm(keys[14], (D_MODEL, N_EXPERTS), D_MODEL ** -0.5),
        "router_bias": nrm(keys[15], (N_EXPERTS,), 0.01),
        "moe_w_gate": nrm(keys[16], (DEPTH, N_EXPERTS, D_MODEL, D_FF_EXPERT), D_MODEL ** -0.5 * DEEPNORM_BETA),
        "moe_w_up": nrm(keys[17], (DEPTH, N_EXPERTS, D_MODEL, D_FF_EXPERT), D_MODEL ** -0.5 * DEEPNORM_BETA),
        "moe_w_down": nrm(keys[18], (DEPTH, N_EXPERTS, D_FF_EXPERT, D_MODEL), D_FF_EXPERT ** -0.5 * DEEPNORM_BETA),
        "ln_mix_g": 1.0 + nrm(keys[19], (DEPTH, D_MODEL), 0.02),
        "ln_mix_b": nrm(keys[20], (DEPTH, D_MODEL), 0.02),
        "ln_ffn_g": 1.0 + nrm(keys[21], (DEPTH, D_MODEL), 0.02),
        "ln_ffn_b": nrm(keys[22], (DEPTH, D_MODEL), 0.02),
    }


def reference(x, ssm_w_in, ssm_conv_w, ssm_conv_b, ssm_dt_bias, ssm_a_log, ssm_d,
              ssm_norm_w, ssm_w_out, att_w_qkv, att_b_qkv, att_rel_bias, att_w_o, att_b_o,
              router_w, router_bias, moe_w_gate, moe_w_up, moe_w_down,
              ln_mix_g, ln_mix_b, ln_ffn_g, ln_ffn_b):
    for i in range(DEPTH):
        j = i // N_MIXERS
        if i % N_MIXERS == 0:
            mix = ssd_mixer(x, ssm_w_in[j], ssm_conv_w[j], ssm_conv_b[j], ssm_dt_bias[j],
                            ssm_a_log[j], ssm_d[j], ssm_norm_w[j], ssm_w_out[j])
        else:
            mix = chunked_attention(x, att_w_qkv[j], att_b_qkv[j], att_rel_bias[j],
                                    att_w_o[j], att_b_o[j])
        x = layer_norm(DEEPNORM_ALPHA * x + mix, ln_mix_g[i], ln_mix_b[i])
        ffn = group_limited_moe(x, router_w, router_bias, moe_w_gate[i], moe_w_up[i], moe_w_down[i])
        x = layer_norm(DEEPNORM_ALPHA * x + ffn, ln_ffn_g[i], ln_ffn_b[i])
    return x
```

```python
import numpy as np
from contextlib import ExitStack
import concourse.bass as bass
import concourse.mybir as mybir
from concourse.bass_utils import run_bass_kernel_spmd

F32 = mybir.dt.float32
BF16 = mybir.dt.bfloat16
U8 = mybir.dt.uint8
AF = mybir.ActivationFunctionType
ALU = mybir.AluOpType
AX = mybir.AxisListType

ENGS = ("pe", "act", "dve", "pool", "sp")

D = 1024
KC = 8
DEPTH = 2
ALPHA = (2.0 * DEPTH) ** 0.25
LN_EPS = 1e-5
RMS_EPS = 1e-5
NE = 16
DFF = 512
D_INNER = 2048
NH = 32
NG = 4
NST = 128
SSD_IN = 5152
AH = 16
AD = 64


class Res:
    __slots__ = ("name", "lw", "rd", "dcount", "dsem")

    def __init__(self, name):
        self.name = name
        self.lw = []
        self.rd = []
        self.dcount = 0
        self.dsem = None


class Ins:
    __slots__ = ("eng", "fn", "reads", "writes", "dma", "deps", "needed", "sem", "val", "clock", "pseudo", "key",
                 "phase")

    def __init__(self, eng, fn, reads, writes, dma, pseudo=False):
        self.eng = eng
        self.fn = fn
        self.reads = reads
        self.writes = writes
        self.dma = dma
        self.deps = []
        self.needed = False
        self.sem = None
        self.val = 0
        self.clock = None
        self.pseudo = pseudo


class Phys:
    __slots__ = ("count", "dsem")

    def __init__(self):
        self.count = 0
        self.dsem = None


class Prog:
    def __init__(self, nc):
        self.nc = nc
        self.ins = []
        self.res = []
        self.phase = 0

    def R(self, name):
        r = Res(name)
        self.res.append(r)
        return r

    def Rs(self, name, n):
        return [self.R("%s%d" % (name, i)) for i in range(n)]

    def op(self, eng, fn, reads=(), writes=()):
        i = Ins(eng, fn, tuple(reads), tuple(writes), False)
        i.phase = self.phase
        self.ins.append(i)
        return i

    def dma(self, eng, out, in_, reads=(), writes=(), key=None):
        assert len(writes) == 1
        i = Ins(eng, (lambda e, o=out, s=in_: e.dma_start(out=o, in_=s)), tuple(reads), tuple(writes), True)
        i.key = key if key is not None else writes[0]
        i.phase = self.phase
        self.ins.append(i)
        return i

    def store(self, eng, out, in_, src, dst, extra_reads=()):
        return self.dma(eng, out, in_, reads=[src] + list(extra_reads), writes=[dst], key=src)

    def mark(self):
        return len(self.ins)

    def interleave(self, start, mid):
        A = self.ins[start:mid]
        Bq = self.ins[mid:]
        if not A or not Bq:
            return
        keyed = [((i + 0.5) / len(A), 0, i, x) for i, x in enumerate(A)] + \
                [((j + 0.5) / len(Bq), 1, j, x) for j, x in enumerate(Bq)]
        keyed.sort(key=lambda z: (z[0], z[1], z[2]))
        self.ins[start:] = [z[3] for z in keyed]

    def barrier(self):
        allr = tuple(self.res)
        for e in ENGS:
            i = Ins(e, None, (), allr, False, pseudo=True)
            i.phase = self.phase
            self.ins.append(i)
        self.phase += 1

    def _analyse(self, final_wait):
        fin = Ins("sp", None, tuple(final_wait), (), False, pseudo=True)
        fin.phase = self.phase
        self.ins.append(fin)
        for i in self.ins:
            deps = []
            for r in i.reads:
                deps.extend(r.lw)
            par = {}
            for w in i.writes:
                p = bool(i.dma and w.lw and all(x.dma for x in w.lw) and not w.rd)
                par[id(w)] = p
                if not p:
                    deps.extend(w.lw)
                deps.extend(w.rd)
            seen = set()
            for d in deps:
                if d is i or id(d) in seen:
                    continue
                seen.add(id(d))
                if (not d.dma) and (not i.dma) and d.eng == "pe" and i.eng == "pe" and not i.pseudo:
                    continue
                i.deps.append(d)
                d.needed = True
            if i.pseudo:
                continue
            for r in i.reads:
                r.rd.append(i)
            for w in i.writes:
                if par[id(w)]:
                    w.lw = w.lw + [i]
                else:
                    w.lw = [i]
                w.rd = []
        cnt = {e: 0 for e in ENGS}
        dres = []
        free = []
        live = []
        cur_phase = 0
        for i in self.ins:
            if i.phase != cur_phase:
                free.extend(p for (_, p) in live)
                live = []
                cur_phase = i.phase
            if i.pseudo:
                continue
            if i.dma:
                w = i.key
                if w.dsem is None or w.dsem[0] != cur_phase:
                    if i.eng == "pool":
                        ph = Phys()
                        dres.append(ph)
                    else:
                        if free:
                            ph = free.pop()
                        else:
                            ph = Phys()
                            dres.append(ph)
                        live.append((cur_phase, ph))
                    w.dsem = (cur_phase, ph)
                ph = w.dsem[1]
                ph.count += 16
                i.sem = ph
                i.val = ph.count
            elif i.needed:
                cnt[i.eng] += 1
                i.sem = i.eng
                i.val = cnt[i.eng]
        seen = {e: {} for e in ENGS}
        per_eng = {e: [] for e in ENGS}
        nwaits = 0
        for i in self.ins:
            s = seen[i.eng]
            waits = {}
            for d in i.deps:
                kid = id(d.sem) if d.dma else d.sem
                if s.get(kid, 0) >= d.val:
                    continue
                if kid not in waits or waits[kid][1] < d.val:
                    waits[kid] = (d.sem, d.val)
            for d in i.deps:
                if d.clock:
                    for k, v in d.clock.items():
                        if s.get(k, 0) < v:
                            s[k] = v
            for kid, (key, v) in waits.items():
                if s.get(kid, 0) < v:
                    s[kid] = v
            wl = list(waits.values())
            nwaits += len(wl)
            if i.sem is not None:
                c = dict(s)
                c[id(i.sem) if i.dma else i.sem] = i.val
                i.clock = c
            per_eng[i.eng].append((i, wl))
        self.stats = dict(n_ins=len(self.ins), n_waits=nwaits, n_dma_sems=len(dres),
                          per_eng={e: len(v) for e, v in per_eng.items()})
        return per_eng, dres

    def emit(self, final_wait=()):
        nc = self.nc
        per_eng, dres = self._analyse(final_wait)
        with ExitStack() as es:
            esem = {e: es.enter_context(nc.semaphore("S_" + e)) for e in ENGS}
            for k, r in enumerate(dres):
                r.dsem = es.enter_context(nc.semaphore("D%d" % k))
            block = es.enter_context(nc.Block())

            def run(e, name):
                for (i, wl) in per_eng[name]:
                    for (key, v) in wl:
                        sem = esem[key] if isinstance(key, str) else key.dsem
                        e.wait_ge(sem, v)
                    if i.fn is None:
                        continue
                    bi = i.fn(e)
                    if i.dma:
                        bi.then_inc(i.sem.dsem, 16)
                    elif i.needed:
                        bi.then_inc(esem[name], 1)

            @block.tensor
            def _(e):
                run(e, "pe")

            @block.scalar
            def _(e):
                run(e, "act")

            @block.vector
            def _(e):
                run(e, "dve")

            @block.gpsimd
            def _(e):
                run(e, "pool")

            @block.sync
            def _(e):
                run(e, "sp")
        return nc


ARENA_BYTES = 210944


def _sz(dt):
    return 4 if dt in (F32, mybir.dt.int32, mybir.dt.uint32) else (2 if dt == BF16 else 1)


class Builder:
    def __init__(self, T=4096):
        self.T = T
        self.NT = T // 128
        self.nc = bass.Bass("TRN2", target_bir_lowering=False)
        self.P = Prog(self.nc)
        self.es = ExitStack()
        nc = self.nc
        self.arena = self.es.enter_context(nc.sbuf_tensor("arena", [128, ARENA_BYTES], U8))
        self.off = 0
        self.pairs = [self.es.enter_context(nc.psum_tensor("pair%d" % i, [128, 1024], F32)) for i in range(4)]
        self.Rbank = self.P.Rs("bank", 8)
        self.dram = {}
        self._rr = 0

    def sb(self, shape, dt):
        n = int(np.prod(shape[1:])) * _sz(dt)
        n_al = (n + 63) // 64 * 64
        off = self.off
        assert off + n_al <= ARENA_BYTES, ("SBUF arena overflow", off, n_al)
        self.off += n_al
        v = self.arena[0:shape[0], off:off + n].bitcast(dt)
        if len(shape) == 3:
            v = v.rearrange("p (a b) -> p a b", a=shape[1])
        elif len(shape) == 4:
            v = v.rearrange("p (a b c) -> p a b c", a=shape[1], b=shape[2])
        elif len(shape) == 5:
            v = v.rearrange("p (a b c d) -> p a b c d", a=shape[1], b=shape[2], c=shape[3])
        return v

    def bank(self, i, dt=F32):
        b = self.pairs[i // 2][:, (i % 2) * 512:(i % 2 + 1) * 512]
        if dt != F32:
            b = b.bitcast(dt)
        return b

    def pair(self, i):
        return self.pairs[i][:]

    def dt_in(self, name, shape, dt=F32):
        t = self.nc.dram_tensor(name, list(shape), dt, kind="ExternalInput").ap()
        self.dram[name] = t
        return t

    def dt_out(self, name, shape, dt=F32):
        t = self.nc.dram_tensor(name, list(shape), dt, kind="ExternalOutput").ap()
        self.dram[name] = t
        return t

    def dt_scr(self, name, shape, dt=F32, debug=False):
        t = self.nc.dram_tensor(name, list(shape), dt, kind=("ExternalOutput" if debug else "Internal")).ap()
        self.dram[name] = t
        return t

    def dq(self):
        self._rr += 1
        return ("sp", "act")[self._rr % 2]

    def consts(self):
        P = self.P
        self.identF = self.sb([128, 128], F32)
        self.identB = self.sb([128, 128], BF16)
        self.maskU = self.sb([128, 128], F32)
        self.maskSL = self.sb([128, 128], F32)
        self.onesF = self.sb([128, 128], F32)
        self.onesB = self.sb([128, 128], BF16)
        self.Rc = P.R("consts")
        Rc = self.Rc
        iF, iB, mU, mS, oF, oB = self.identF, self.identB, self.maskU, self.maskSL, self.onesF, self.onesB
        P.op("pool", lambda e: e.memset(iF, 0.0), writes=[Rc])
        P.op("pool", lambda e: e.affine_select(out=iF, in_=iF, pattern=[[-1, 128]], compare_op=ALU.not_equal,
                                               fill=1.0, base=0, channel_multiplier=1), reads=[Rc], writes=[Rc])
        P.op("pool", lambda e: e.tensor_copy(out=iB, in_=iF), reads=[Rc], writes=[Rc])
        P.op("pool", lambda e: e.memset(oF, 1.0), reads=[Rc], writes=[Rc])
        P.op("pool", lambda e: e.memset(oB, 1.0), reads=[Rc], writes=[Rc])
        P.op("pool", lambda e: e.affine_select(out=mU, in_=oF, pattern=[[1, 128]], compare_op=ALU.is_ge,
                                               fill=0.0, base=0, channel_multiplier=-1), reads=[Rc], writes=[Rc])
        P.op("pool", lambda e: e.affine_select(out=mS, in_=oF, pattern=[[-1, 128]], compare_op=ALU.is_gt,
                                               fill=0.0, base=0, channel_multiplier=1), reads=[Rc], writes=[Rc])

    def wconv_steps(self, layer, wg_d, wu_d, wd_d, wb, Rwb, ex_range=None, nbuf=2):
        P = self.P
        bnc = [self.sb([128, 4096], BF16) for _ in range(nbuf)]
        Rbn = P.Rs("bnc", nbuf)
        RbnS = P.Rs("bncS", nbuf)
        n = 0
        pend = [None]

        def flush():
            if pend[0] is not None:
                b_, dst_ = pend[0]
                P.dma("pool", dst_, bnc[b_], reads=[Rbn[b_]], writes=[Rwb], key=RbnS[b_])
                pend[0] = None

        for ex in (ex_range if ex_range is not None else range(NE)):
            for which in range(3):
                b = n % nbuf
                n += 1
                if which < 2:
                    src = (wg_d, wu_d)[which][layer, ex].rearrange("(p k) f -> p (k f)", k=KC)
                    dst = wb[which][ex * 128:(ex + 1) * 128, :]
                else:
                    src = wd_d[layer, ex].rearrange("(p k) n -> p (k n)", k=4)
                    dst = wb[2][ex * DFF:(ex + 1) * DFF, :].rearrange("(p k) n -> p (k n)", k=4)

                def step(b=b, src=src, dst=dst):
                    P.dma("pool", bnc[b], src, writes=[Rbn[b]])
                    flush()
                    pend[0] = (b, dst)
                yield step
        yield flush

    def _bc_reg(self, e):
        if getattr(self, "_bcr", None) is None:
            self._bcr = e.to_reg(NE * 128 - 1)
        return self._bcr

    def _bc_reg2(self, e):
        if getattr(self, "_bcr2", None) is None:
            self._bcr2 = e.to_reg(NE * DFF - 1)
        return self._bcr2

    def zero_fill(self, dram2d, Rz):
        P = self.P
        if getattr(self, "zt", None) is None:
            self.zt = self.sb([128, 4, D], BF16)
            self.Rzt = P.R("zt")
            P.op("pool", lambda e, zt=self.zt: e.memset(zt, 0.0), writes=[self.Rzt])
        n = dram2d.shape[0] // 512
        for i in range(n):
            P.dma(self.dq(), dram2d[i * 512:(i + 1) * 512, :].rearrange("(a p) d -> p a d", p=128), self.zt,
                  reads=[self.Rzt], writes=[Rz], key=self.Rzt)

    def bcast_load(self, dram_row, n, eng="sp"):
        t = self.sb([128, n], F32)
        r = self.P.R("bc")
        self.P.dma(eng, t, dram_row.partition_broadcast(128), writes=[r])
        return t, r

    def layer_norm(self, xt, Rx, st, mv, Rs, g_b, b_b, Rgb, eng2="pool"):
        P = self.P
        for c in range(2):
            P.op("dve", lambda e, c=c: e.bn_stats(out=st[:, c, :], in_=xt[:, c * 512:(c + 1) * 512]),
                 reads=[Rx], writes=[Rs])
        P.op("dve", lambda e: e.bn_aggr(out=mv[:, 0:2], in_=st), reads=[Rs], writes=[Rs])
        P.op("dve", lambda e: e.tensor_scalar(out=mv[:, 2:3], in0=mv[:, 1:2], scalar1=LN_EPS, scalar2=None,
                                              op0=ALU.add), reads=[Rs], writes=[Rs])
        P.op("act", lambda e: e.activation(out=mv[:, 3:4], in_=mv[:, 2:3], func=AF.Sqrt), reads=[Rs], writes=[Rs])
        P.op("dve", lambda e: e.reciprocal(out=mv[:, 4:5], in_=mv[:, 3:4]), reads=[Rs], writes=[Rs])
        P.op("dve", lambda e: e.scalar_tensor_tensor(out=mv[:, 5:6], in0=mv[:, 0:1], scalar=-1.0, in1=mv[:, 4:5],
                                                     op0=ALU.mult, op1=ALU.mult), reads=[Rs], writes=[Rs])
        P.op("act", lambda e: e.activation(out=xt, in_=xt, func=AF.Identity, bias=mv[:, 5:6], scale=mv[:, 4:5]),
             reads=[Rs, Rx], writes=[Rx])
        P.op("dve", lambda e: e.tensor_tensor(out=xt, in0=xt, in1=g_b, op=ALU.mult), reads=[Rx, Rgb], writes=[Rx])
        P.op(eng2, lambda e: e.tensor_tensor(out=xt, in0=xt, in1=b_b, op=ALU.add), reads=[Rx, Rgb], writes=[Rx])

    def make_xT(self, xt, Rx, bk, xTb, RxT, xTf=None, RxTf=None):
        P = self.P
        iF = self.identF
        for h in range(2):
            pb = self.bank(bk + h)
            Rb = self.Rbank[bk + h]
            for k in range(4):
                kk = h * 4 + k
                P.op("pe", lambda e, pb=pb, k=k, kk=kk: e.transpose(pb[:, k * 128:(k + 1) * 128],
                                                                    xt[:, kk * 128:(kk + 1) * 128], iF),
                     reads=[Rx, self.Rc], writes=[Rb])
            P.op("act", lambda e, pb=pb, h=h: e.copy(out=xTb[:, h * 4:(h + 1) * 4, :],
                                                     in_=pb.rearrange("p (a b) -> p a b", a=4)),
                 reads=[Rb], writes=[RxT, Rb])
            if xTf is not None:
                P.op("dve", lambda e, pb=pb, h=h: e.tensor_copy(out=xTf[:, h * 4:(h + 1) * 4, :],
                                                                in_=pb.rearrange("p (a b) -> p a b", a=4)),
                     reads=[Rb], writes=[RxTf, Rb])

    def router_setup(self, router_w, router_bias):
        P = self.P
        self.rw = self.sb([128, KC, NE], F32)
        self.Rrw = P.R("rw")
        P.dma("sp", self.rw, router_w.rearrange("(k p) e -> p k e", p=128), writes=[self.Rrw])
        self.rb_b, self.Rrb = self.bcast_load(router_bias.rearrange("(o e) -> o e", o=1), NE)
        self.rt = self.sb([128, 160], F32)
        self.Rrt = P.R("rt")

    def router(self, xTf, RxTf, bk, comb, Rcomb):
        P = self.P
        pb = self.bank(bk)
        Rb = self.Rbank[bk]
        rw = self.rw
        for k in range(KC):
            P.op("pe", lambda e, k=k: e.matmul(pb[:, 0:NE], xTf[:, k, :], rw[:, k, :], start=(k == 0),
                                                stop=(k == KC - 1)), reads=[RxTf, self.Rrw], writes=[Rb])
        P.op("dve", lambda e: e.tensor_copy(out=comb, in_=pb[:, 0:NE]), reads=[Rb], writes=[Rcomb, Rb])

    def route_batched(self, lg, sc, w1, w2, w3, sm4, d):
        P = self.P
        NT = self.NT
        f = lambda a: a.rearrange("p t e -> p (t e)")
        v4 = lambda a: a.rearrange("p t (g j) -> p t g j", g=4)
        be = lambda a: a.unsqueeze(2).to_broadcast([128, NT, NE])
        mx, ssum, rs, gmx, gsum = sm4[:, 0, :], sm4[:, 1, :], sm4[:, 2, :], sm4[:, 3, :], sm4[:, 4, :]
        m1 = w3[:, :, 0:4]
        m2 = w3[:, :, 4:8]
        gs = w3[:, :, 8:12]
        gm = w3[:, :, 12:16]
        b4 = lambda a: a.unsqueeze(3).to_broadcast([128, NT, 4, 4])
        d(lambda e: e.tensor_reduce(out=mx, in_=lg, axis=AX.X, op=ALU.max))
        d(lambda e: e.tensor_tensor(out=sc, in0=lg, in1=be(mx), op=ALU.subtract))
        P.op("act", lambda e: e.activation(out=f(sc), in_=f(sc), func=AF.Exp), reads=[self._Rroute],
             writes=[self._Rroute])
        d(lambda e: e.tensor_reduce(out=ssum, in_=sc, axis=AX.X, op=ALU.add))
        d(lambda e: e.reciprocal(out=rs, in_=ssum))
        d(lambda e: e.tensor_tensor(out=sc, in0=sc, in1=be(rs), op=ALU.mult))
        d(lambda e: e.tensor_tensor(out=w1, in0=sc, in1=self.rb_b.unsqueeze(1).to_broadcast([128, NT, NE]),
                                    op=ALU.add), rd=[self.Rrb])
        d(lambda e: e.tensor_reduce(out=m1, in_=v4(w1), axis=AX.X, op=ALU.max))
        d(lambda e: e.tensor_tensor(out=v4(w2), in0=v4(w1), in1=b4(m1), op=ALU.is_equal))
        d(lambda e: e.scalar_tensor_tensor(out=f(w1), in0=f(w2), scalar=-1e9, in1=f(w1), op0=ALU.mult,
                                           op1=ALU.add))
        d(lambda e: e.tensor_reduce(out=m2, in_=v4(w1), axis=AX.X, op=ALU.max))
        d(lambda e: e.tensor_tensor(out=v4(lg), in0=v4(w1), in1=b4(m2), op=ALU.is_equal))
        d(lambda e: e.tensor_tensor(out=gs, in0=m1, in1=m2, op=ALU.add))
        d(lambda e: e.tensor_reduce(out=gmx, in_=gs, axis=AX.X, op=ALU.max))
        d(lambda e: e.tensor_tensor(out=gm, in0=gs, in1=gmx.unsqueeze(2).to_broadcast([128, NT, 4]),
                                    op=ALU.is_equal))
        d(lambda e: e.tensor_tensor(out=w2, in0=w2, in1=lg, op=ALU.add))
        d(lambda e: e.tensor_tensor(out=v4(w2), in0=v4(w2), in1=b4(gm), op=ALU.mult))
        d(lambda e: e.tensor_tensor(out=w2, in0=w2, in1=sc, op=ALU.mult))
        d(lambda e: e.tensor_reduce(out=gsum, in_=w2, axis=AX.X, op=ALU.add))
        d(lambda e: e.reciprocal(out=rs, in_=gsum))
        d(lambda e: e.tensor_tensor(out=lg, in0=w2, in1=be(rs), op=ALU.mult))

    def phase_pre(self, x_d, xT_d, comb_d, RxT_d, Rcomb_d):
        P = self.P
        mark = self.off
        xt = [self.sb([128, D], F32) for _ in range(2)]
        Rx = P.Rs("pre_x", 2)
        xTb = [self.sb([128, KC, 128], BF16) for _ in range(2)]
        RxTb = P.Rs("pre_xTb", 2)
        xTf = [self.sb([128, KC, 128], F32) for _ in range(2)]
        RxTf = P.Rs("pre_xTf", 2)
        cb = [self.sb([128, NE], F32) for _ in range(2)]
        Rcb = P.Rs("pre_cb", 2)
        for t in range(self.NT):
            b = t % 2
            P.dma("sp", xt[b], x_d[t * 128:(t + 1) * 128, :], writes=[Rx[b]])
            self.make_xT(xt[b], Rx[b], 0 + 2 * b, xTb[b], RxTb[b], xTf[b], RxTf[b])
            self.router(xTf[b], RxTf[b], 4 + b, cb[b], Rcb[b])
            P.store("act", xT_d[t], xTb[b], RxTb[b], RxT_d)
            P.store("act", comb_d[t], cb[b], Rcb[b], Rcomb_d)
        P.barrier()
        self.off = mark

    def phase_moe(self, layer, x_d, xT_d, comb_d, Rin, wg_d, wu_d, wd_d, lng_d, lnb_d,
                  out_d, Rout, outT_d=None, RoutT=None):
        P = self.P
        T, NT = self.T, self.NT
        mark = self.off
        HT = min(16, NT)
        NHALF = NT // HT
        NBLK = HT // 4
        acc = self.sb([128, HT, D], F32)
        Racc = P.Rs("acc", HT)
        xT = self.sb([128, HT, KC, 128], BF16)
        RxT = P.R("moe_xT")
        comb = self.sb([128, HT, NE], F32)
        Rcomb = P.R("moe_comb")
        wg = [self.sb([128, KC, DFF], BF16) for _ in range(2)]
        wu = [self.sb([128, KC, DFF], BF16) for _ in range(2)]
        wd = [self.sb([128, 4, D], BF16) for _ in range(2)]
        Rwg = P.Rs("wg", 2)
        Rwu = P.Rs("wu", 2)
        Rwd = P.Rs("wd", 2)
        sg = [self.sb([128, 512], F32) for _ in range(2)]
        Rsg = P.Rs("sg", 2)
        hT = [self.sb([128, 4, 512], BF16) for _ in range(2)]
        RhT = P.Rs("hT", 2)
        g_b, Rg = self.bcast_load(lng_d[layer:layer + 1, :], D, "sp")
        b_b, Rb_ = self.bcast_load(lnb_d[layer:layer + 1, :], D, "act")
        st = self.sb([128, 2, 6], F32)
        mv = self.sb([128, 8], F32)
        Rs = P.R("moe_ln")
        xTo = [self.sb([128, KC, 128], BF16) for _ in range(2)]
        RxTo = P.Rs("moe_xTo", 2)
        Rgb = P.R("moe_gb")
        P.op("pool", lambda e: e.tensor_copy(out=mv[:, 7:8], in_=g_b[:, 0:1]), reads=[Rg, Rb_], writes=[Rgb])

        it = 0
        for half in range(NHALF):
            t0 = half * HT
            for j in range(HT):
                P.dma(self.dq(), acc[:, j, :], x_d[(t0 + j) * 128:(t0 + j + 1) * 128, :], reads=Rin,
                      writes=[Racc[j]])
                P.op("act", lambda e, j=j: e.mul(out=acc[:, j, :], in_=acc[:, j, :], mul=ALPHA),
                     reads=[Racc[j]], writes=[Racc[j]])
            P.dma("sp", xT, xT_d[t0:t0 + HT].rearrange("t p k c -> p t k c"), reads=Rin, writes=[RxT])
            P.dma("act", comb, comb_d[t0:t0 + HT].rearrange("t p e -> p t e"), reads=Rin, writes=[Rcomb])
            for ex in range(NE):
                wb = (half * NE + ex) % 2
                P.dma("pool", wg[wb], wg_d[layer, ex].rearrange("(k p) f -> p k f", p=128), writes=[Rwg[wb]])
                P.dma("pool", wu[wb], wu_d[layer, ex].rearrange("(k p) f -> p k f", p=128), writes=[Rwu[wb]])
                P.dma("pool", wd[wb], wd_d[layer, ex].rearrange("(k p) f -> p k f", p=128), writes=[Rwd[wb]])
                for blk in range(NBLK):
                    hb = it % 2
                    it += 1
                    rhs_x = lambda k, blk=blk: xT[:, blk * 4:(blk + 1) * 4, k, :]
                    for f in range(4):
                        pg = self.bank(f % 2)
                        Rpg = self.Rbank[f % 2]
                        pu = self.bank(2 + f % 2)
                        Rpu = self.Rbank[2 + f % 2]
                        sgb = f % 2
                        for k in range(KC):
                            P.op("pe", lambda e, pg=pg, k=k, f=f, wb=wb, rx=rhs_x: e.matmul(
                                pg, wg[wb][:, k, f * 128:(f + 1) * 128], rx(k), start=(k == 0), stop=(k == KC - 1)),
                                reads=[Rwg[wb], RxT], writes=[Rpg])
                        for k in range(KC):
                            P.op("pe", lambda e, pu=pu, k=k, f=f, wb=wb, rx=rhs_x: e.matmul(
                                pu, wu[wb][:, k, f * 128:(f + 1) * 128], rx(k), start=(k == 0), stop=(k == KC - 1)),
                                reads=[Rwu[wb], RxT], writes=[Rpu])
                        P.op("act", lambda e, pg=pg, sgb=sgb: e.activation(out=sg[sgb], in_=pg, func=AF.Silu),
                             reads=[Rpg], writes=[Rsg[sgb]])
                        P.op("dve", lambda e, pu=pu, sgb=sgb, hb=hb, f=f: e.tensor_tensor(
                            out=hT[hb][:, f, :], in0=pu, in1=sg[sgb], op=ALU.mult),
                            reads=[Rpu, Rsg[sgb]], writes=[RhT[hb]])
                    for s in range(4):
                        j = blk * 4 + s
                        for n in range(2):
                            bi = 4 + (s % 2) * 2 + n
                            po = self.bank(bi)
                            Rpo = self.Rbank[bi]
                            for f in range(4):
                                P.op("pe", lambda e, po=po, f=f, s=s, n=n, hb=hb, wb=wb: e.matmul(
                                    po, hT[hb][:, f, s * 128:(s + 1) * 128], wd[wb][:, f, n * 512:(n + 1) * 512],
                                    start=(f == 0), stop=(f == 3)), reads=[RhT[hb], Rwd[wb]], writes=[Rpo])
                            P.op("dve", lambda e, po=po, j=j, n=n, ex=ex: e.scalar_tensor_tensor(
                                out=acc[:, j, n * 512:(n + 1) * 512], in0=po, scalar=comb[:, j, ex:ex + 1],
                                in1=acc[:, j, n * 512:(n + 1) * 512], op0=ALU.mult, op1=ALU.add),
                                reads=[Rpo, Rcomb, Racc[j]], writes=[Racc[j]])
            for j in range(HT):
                t = t0 + j
                self.layer_norm(acc[:, j, :], Racc[j], st, mv, Rs, g_b, b_b, Rgb)
                P.store(self.dq(), out_d[t * 128:(t + 1) * 128, :], acc[:, j, :], Racc[j], Rout)
                if outT_d is not None:
                    b = j % 2
                    self.make_xT(acc[:, j, :], Racc[j], 0 + 2 * b, xTo[b], RxTo[b])
                    P.store(self.dq(), outT_d[t], xTo[b], RxTo[b], RoutT)
        P.barrier()
        self.off = mark


    def phase_a0z(self, x_d, win_d, dtb_d, alog_d, xT_d, RxT_d, sz_d, Rsz_d, dt_d, Rdt_d, early=None, wconv=None):
        P = self.P
        NT = self.NT
        mark = self.off
        wsteps = wconv() if wconv is not None else None
        winz = self.sb([128, KC, 2048], BF16)
        Rwz = P.Rs("winz", 4)
        for n in range(4):
            P.dma("pool", winz[:, :, n * 512:(n + 1) * 512],
                  win_d[0, :, n * 512:(n + 1) * 512].rearrange("(k p) n -> p k n", p=128), writes=[Rwz[n]])
        windt = self.sb([128, KC, 32], F32)
        Rwdt = P.R("windt")
        P.dma("sp", windt, win_d[0, :, 5120:5152].rearrange("(k p) n -> p k n", p=128), writes=[Rwdt])
        dtb_b, Rdtb = self.bcast_load(dtb_d[0:1, :], 32)
        vall = self.sb([128, NT, 32], F32)
        av = self.sb([128, NT, 32], F32)
        Rv = P.R("vall")
        xt = [self.sb([128, D], F32) for _ in range(2)]
        Rx = P.Rs("a0_x", 2)
        xTb = [self.sb([128, KC, 128], BF16) for _ in range(2)]
        RxTb = P.Rs("a0_xTb", 2)
        xTf = [self.sb([128, KC, 128], F32) for _ in range(2)]
        RxTf = P.Rs("a0_xTf", 2)
        sz = [self.sb([128, 2048], BF16) for _ in range(2)]
        Rsz = P.Rs("a0_sz", 2)
        for t in range(NT):
            b = t % 2
            if early is not None and t == min(3, NT - 1):
                early()
            if wsteps is not None and t >= 2:
                for _ in range(2 if t % 2 == 0 else 1):
                    st_ = next(wsteps, None)
                    if st_ is not None:
                        st_()
            P.dma("sp", xt[b], x_d[t * 128:(t + 1) * 128, :], writes=[Rx[b]])
            self.make_xT(xt[b], Rx[b], 2 * b, xTb[b], RxTb[b], xTf[b], RxTf[b])
            P.store("act", xT_d[t], xTb[b], RxTb[b], RxT_d)
            for n in range(4):
                bi = 4 + n % 2
                pz = self.bank(bi)
                Rpz = self.Rbank[bi]
                for k in range(KC):
                    P.op("pe", lambda e, pz=pz, k=k, n=n, b=b: e.matmul(
                        pz, xTb[b][:, k, :], winz[:, k, n * 512:(n + 1) * 512], start=(k == 0), stop=(k == KC - 1)),
                        reads=[RxTb[b], Rwz[n]], writes=[Rpz])
                P.op("act", lambda e, pz=pz, n=n, b=b: e.activation(out=sz[b][:, n * 512:(n + 1) * 512], in_=pz,
                                                                     func=AF.Silu),
                     reads=[Rpz], writes=[Rsz[b], Rpz])
            P.store(self.dq(), sz_d[t], sz[b], Rsz[b], Rsz_d)
            pdt = self.bank(6 + b)
            Rpdt = self.Rbank[6 + b]
            for k in range(KC):
                P.op("pe", lambda e, pdt=pdt, k=k, b=b: e.matmul(pdt[:, 0:32], xTf[b][:, k, :], windt[:, k, :],
                                                                  start=(k == 0), stop=(k == KC - 1)),
                     reads=[RxTf[b], Rwdt], writes=[Rpdt])
            P.op("dve", lambda e, pdt=pdt, t=t: e.tensor_tensor(out=vall[:, t, :], in0=pdt[:, 0:32], in1=dtb_b,
                                                                 op=ALU.add),
                 reads=[Rpdt, Rdtb], writes=[Rv, Rpdt])
        if wsteps is not None:
            for st_ in wsteps:
                st_()
        P.op("dve", lambda e: e.scalar_tensor_tensor(out=av, in0=vall, scalar=-1.0, in1=vall, op0=ALU.mult,
                                                     op1=ALU.max), reads=[Rv], writes=[Rv])
        P.op("act", lambda e: e.activation(out=av, in_=av, func=AF.Exp, scale=-1.0), reads=[Rv], writes=[Rv])
        P.op("act", lambda e: e.activation(out=av, in_=av, func=AF.Ln, bias=1.0), reads=[Rv], writes=[Rv])
        P.op("dve", lambda e: e.scalar_tensor_tensor(out=vall, in0=vall, scalar=0.0, in1=av, op0=ALU.max,
                                                     op1=ALU.add), reads=[Rv], writes=[Rv])
        a_b, Ra = self.bcast_load(alog_d[0:1, :], NH, "act")
        P.op("act", lambda e: e.activation(out=a_b, in_=a_b, func=AF.Exp), reads=[Ra], writes=[Ra])
        P.op("dve", lambda e: e.tensor_scalar(out=a_b, in0=a_b, scalar1=-1.0, scalar2=None, op0=ALU.mult),
             reads=[Ra], writes=[Ra])
        dtx = self.sb([128, NT, 5, NH], F32)
        cumt = self.sb([128, 2, NT, NH], F32)
        Rdx = P.R("dtx")
        NTH = NT * NH
        P.op("dve", lambda e: e.tensor_copy(out=dtx[:, :, 0, :], in_=vall), reads=[Rv], writes=[Rdx])
        P.op("dve", lambda e: e.tensor_tensor(out=av, in0=vall, in1=a_b.unsqueeze(1).to_broadcast([128, NT, NH]),
                                              op=ALU.mult), reads=[Rv, Ra], writes=[Rv])
        P.op("dve", lambda e: e.tensor_copy(out=dtx[:, :, 1, :], in_=av), reads=[Rv], writes=[Rdx])
        avf = av.rearrange("p t h -> p (t h)")
        for which, lhs in enumerate((self.maskU, self.onesF)):
            for c0 in range(0, NTH, 512):
                c1 = min(NTH, c0 + 512)
                bi = 4 + which
                pb = self.bank(bi)
                P.op("pe", lambda e, pb=pb, lhs=lhs, c0=c0, c1=c1: e.matmul(pb[:, 0:c1 - c0], lhs, avf[:, c0:c1],
                                                                           start=True, stop=True),
                     reads=[Rv, self.Rc], writes=[self.Rbank[bi]])
                P.op("dve", lambda e, pb=pb, which=which, c0=c0, c1=c1: e.tensor_copy(
                    out=cumt[:, which].rearrange("p t h -> p (t h)")[:, c0:c1], in_=pb[:, 0:c1 - c0]),
                    reads=[self.Rbank[bi]], writes=[Rdx, self.Rbank[bi]])
        P.op("act", lambda e: e.activation(out=dtx[:, :, 2, :], in_=cumt[:, 0], func=AF.Exp), reads=[Rdx],
             writes=[Rdx])
        P.op("act", lambda e: e.activation(out=dtx[:, :, 3, :], in_=cumt[:, 1], func=AF.Exp), reads=[Rdx],
             writes=[Rdx])
        P.op("dve", lambda e: e.tensor_tensor(out=cumt[:, 1], in0=cumt[:, 1], in1=cumt[:, 0], op=ALU.subtract),
             reads=[Rdx], writes=[Rdx])
        P.op("act", lambda e: e.activation(out=dtx[:, :, 4, :], in_=cumt[:, 1], func=AF.Exp), reads=[Rdx],
             writes=[Rdx])
        P.store("sp", dt_d, dtx, Rdx, Rdt_d)
        P.barrier()
        self.off = mark

    def phase_a0x(self, win_d, convw_d, convb_d, xT_d, RxT_d, xs_d, Rxs_d, bt_d, Rbt_d, bct_d, Rbct_d, zfill=(),
                  wconv=None):
        P = self.P
        self.zt = None
        wsteps = wconv() if wconv is not None else None
        NT = self.NT
        NB = NT // 4
        mark = self.off
        NCH = 24
        winx = self.sb([128, KC, 3072], BF16)
        Rwx = P.Rs("winx", 3)
        for j in range(6):
            P.dma("pool", winx[:, :, j * 512:(j + 1) * 512],
                  win_d[0, :, 2048 + j * 512:2048 + (j + 1) * 512].rearrange("(k p) n -> p k n", p=128),
                  writes=[Rwx[j // 2]])
        cw4 = self.sb([4, 3072], F32)
        cbrow = self.sb([1, 3072], F32)
        cbrowB = self.sb([1, 3072], BF16)
        cwT = self.sb([128, NCH, 4], F32)
        Rcw = P.R("convw")
        P.dma("sp", cw4, convw_d[0], writes=[Rcw])
        Rcb = P.R("convb")
        P.dma("act", cbrow, convb_d[0:1, :], writes=[Rcb])
        P.op("dve", lambda e: e.tensor_copy(out=cbrowB, in_=cbrow), reads=[Rcb], writes=[Rcb])
        pb0 = self.bank(0)
        for c in range(NCH):
            P.op("pe", lambda e, c=c: e.matmul(pb0[:, c * 4:(c + 1) * 4], cw4[0:4, c * 128:(c + 1) * 128],
                                                self.identF[0:4, 0:4], start=True, stop=True),
                 reads=[Rcw, self.Rc], writes=[self.Rbank[0]])
        P.op("dve", lambda e: e.tensor_copy(out=cwT, in_=pb0[:, 0:NCH * 4].rearrange("p (c k) -> p c k", k=4)),
             reads=[self.Rbank[0]], writes=[Rcw, self.Rbank[0]])
        diagW = self.sb([128, NCH, 4, 128], BF16)
        Rdg = P.R("diagW")
        for c in range(NCH):
            P.op("dve", lambda e, c=c: e.tensor_tensor(
                out=diagW[:, c, :, :], in0=self.identF.unsqueeze(1).to_broadcast([128, 4, 128]),
                in1=cwT[:, c, :].unsqueeze(2).to_broadcast([128, 4, 128]), op=ALU.mult),
                reads=[Rcw, self.Rc], writes=[Rdg])
        xT = [self.sb([128, 4, KC, 128], BF16) for _ in range(2)]
        RxTl = P.Rs("a0x_xT", 2)
        uT = [self.sb([128, NCH, 515], BF16) for _ in range(2)]
        RuT = P.Rs("a0x_uT", 2)
        xs_tok = [self.sb([128, 2048], BF16) for _ in range(2)]
        Rxs = P.Rs("a0x_xs", 2)
        b_tok = [self.sb([128, 512], BF16) for _ in range(2)]
        Rbt = P.Rs("a0x_bt", 2)
        bct = [self.sb([128, 8, 128], BF16) for _ in range(2)]
        Rbct = P.Rs("a0x_bct", 2)
        oB = self.onesB
        for nb in range(NB):
            ub = nb % 2
            P.dma("sp", xT[ub], xT_d[nb * 4:(nb + 1) * 4].rearrange("t p k c -> p t k c"), reads=[RxT_d],
                  writes=[RxTl[ub]])
            for zi, z in enumerate(zfill):
                if min(zi, NB - 1) == nb:
                    self.zero_fill(*z)
            if nb == 0:
                P.op("pool", lambda e, ub=ub: e.memset(uT[ub][:, :, 0:3], 0.0), writes=[RuT[ub]])
            else:
                P.op("pool", lambda e, ub=ub: e.tensor_copy(out=uT[ub][:, :, 0:3], in_=uT[1 - ub][:, :, 512:515]),
                     reads=[RuT[1 - ub]], writes=[RuT[ub]])
            for c in range(NCH):
                bi = c % 2
                pu = self.bank(bi)
                Rpu = self.Rbank[bi]
                for k in range(KC):
                    P.op("pe", lambda e, pu=pu, k=k, c=c, ub=ub: e.matmul(
                        pu, winx[:, k, c * 128:(c + 1) * 128], xT[ub][:, :, k, :], start=(k == 0),
                        stop=(k == KC - 1)), reads=[Rwx[c // 8], RxTl[ub]], writes=[Rpu])
                if c % 2 == 0:
                    P.op("act", lambda e, pu=pu, c=c, ub=ub: e.copy(out=uT[ub][:, c, 3:515], in_=pu),
                         reads=[Rpu], writes=[RuT[ub], Rpu])
                else:
                    P.op("dve", lambda e, pu=pu, c=c, ub=ub: e.tensor_copy(out=uT[ub][:, c, 3:515], in_=pu),
                         reads=[Rpu], writes=[RuT[ub], Rpu])
            for s in range(4):
                t = nb * 4 + s
                ob = t % 2
                if wsteps is not None:
                    st_ = next(wsteps, None)
                    if st_ is not None:
                        st_()
                for q in range(5):
                    bi = 2 + q % 2
                    pc = self.bank(bi)
                    Rpc = self.Rbank[bi]
                    for cc in range(4):
                        c = q * 4 + cc
                        for k in range(4):
                            P.op("pe", lambda e, pc=pc, cc=cc, c=c, k=k, s=s, ub=ub: e.matmul(
                                pc[:, cc * 128:(cc + 1) * 128], uT[ub][:, c, s * 128 + k:s * 128 + k + 128],
                                diagW[:, c, k, :], start=(k == 0), stop=False),
                                reads=[RuT[ub], Rdg], writes=[Rpc])
                        P.op("pe", lambda e, pc=pc, cc=cc, c=c: e.matmul(
                            pc[:, cc * 128:(cc + 1) * 128], oB[0:1, 0:128], cbrowB[0:1, c * 128:(c + 1) * 128],
                            start=False, stop=True), reads=[Rcb, self.Rc], writes=[Rpc])
                    if q < 4:
                        P.op("act", lambda e, pc=pc, q=q, ob=ob: e.activation(
                            out=xs_tok[ob][:, q * 512:(q + 1) * 512], in_=pc, func=AF.Silu),
                            reads=[Rpc], writes=[Rxs[ob], Rpc])
                    else:
                        P.op("act", lambda e, pc=pc, ob=ob: e.activation(out=b_tok[ob], in_=pc, func=AF.Silu),
                             reads=[Rpc], writes=[Rbt[ob], Rpc])
                P.store(self.dq(), xs_d[t], xs_tok[ob], Rxs[ob], Rxs_d)
                P.store(self.dq(), bt_d[t], b_tok[ob], Rbt[ob], Rbt_d)
                for q2 in range(2):
                    bi = 4 + q2
                    pf = self.bank(bi)
                    Rpf = self.Rbank[bi]
                    for cc in range(4):
                        c = 16 + q2 * 4 + cc
                        for k in range(4):
                            P.op("pe", lambda e, pf=pf, cc=cc, c=c, k=k, s=s, ub=ub: e.matmul(
                                pf[:, cc * 128:(cc + 1) * 128], diagW[:, c, k, :],
                                uT[ub][:, c, s * 128 + k:s * 128 + k + 128], start=(k == 0), stop=False),
                                reads=[RuT[ub], Rdg], writes=[Rpf])
                        P.op("pe", lambda e, pf=pf, cc=cc, c=c: e.matmul(
                            pf[:, cc * 128:(cc + 1) * 128], cbrowB[0:1, c * 128:(c + 1) * 128], oB[0:1, 0:128],
                            start=False, stop=True), reads=[Rcb, self.Rc], writes=[Rpf])
                    P.op("act", lambda e, pf=pf, q2=q2, ob=ob: e.activation(
                        out=bct[ob][:, q2 * 4:(q2 + 1) * 4, :], in_=pf.rearrange("p (a b) -> p a b", a=4),
                        func=AF.Silu), reads=[Rpf], writes=[Rbct[ob], Rpf])
                P.store(self.dq(), bct_d[t], bct[ob], Rbct[ob], Rbct_d)
        if wsteps is not None:
            for st_ in wsteps:
                st_()
        P.barrier()
        self.off = mark

    def phase_b0(self, x_d, sz_d, dt_d, xs_d, bt_d, bct_d, Rin, alog_d, dskip_d, normw_d, wout_d, lng_d, lnb_d,
                 x1_d, Rx1_d, x1T_d, Rx1T_d, comb_d, Rcomb_d):
        P = self.P
        NT = self.NT
        mark = self.off
        iB, mU, mS, oF = self.identB, self.maskU, self.maskSL, self.onesF
        Rc = self.Rc
        wout = self.sb([128, 16, D], BF16)
        Rwo = P.R("wout")
        for q in range(4):
            P.dma("pool", wout[:, q * 4:(q + 1) * 4, :],
                  wout_d[0, q * 512:(q + 1) * 512, :].rearrange("(k p) n -> p k n", p=128), writes=[Rwo])
        normw_b, Rnw = self.bcast_load(normw_d[0:1, :], D_INNER, "sp")
        g_b, Rg = self.bcast_load(lng_d[0:1, :], D, "act")
        b_b, Rb_ = self.bcast_load(lnb_d[0:1, :], D, "sp")
        dsk_b, Rdsk = self.bcast_load(dskip_d[0:1, :], NH, "sp")
        Rgb = P.R("b0_gb")
        st = self.sb([128, 2, 6], F32)
        mv = self.sb([128, 8], F32)
        Rs = P.R("b0_ln")
        P.op("pool", lambda e: e.tensor_copy(out=mv[:, 7:8], in_=g_b[:, 0:1]), reads=[Rg, Rb_], writes=[Rgb])
        hT = self.sb([128, D_INNER], F32)
        hTb = [self.sb([128, D_INNER], BF16) for _ in range(3)]
        RhT = P.R("hT")
        RhTb = P.Rs("hTb", 3)
        P.op("pool", lambda e: e.memset(hT, 0.0), writes=[RhT])
        P.op("pool", lambda e: e.memset(hTb[0], 0.0), writes=[RhTb[0]])
        NLB = 3
        xs = [self.sb([128, D_INNER], BF16) for _ in range(2)]
        btok = [self.sb([128, 512], BF16) for _ in range(2)]
        RldF = P.Rs("b0_ldf", 2)
        bct = [self.sb([128, 8, 128], BF16) for _ in range(NLB)]
        sz = [self.sb([128, D_INNER], BF16) for _ in range(NLB)]
        xr = [self.sb([128, D], F32) for _ in range(NLB)]
        Rld = P.Rs("b0_ld", NLB)
        sm = [self.sb([128, 256], F32) for _ in range(NLB)]
        Rsm = P.Rs("b0_sm", NLB)
        xsdt = [self.sb([128, D_INNER], BF16) for _ in range(2)]
        xsD = [self.sb([128, D_INNER], BF16) for _ in range(2)]
        Rxsdt, RxsD = P.Rs("xsdt", 2), P.Rs("xsD", 2)
        xw = self.sb([128, D_INNER], BF16)
        Rxw = P.R("xw")
        rhsS = self.sb([128, NH // 2, 128], F32)
        RrhsS = P.R("rhsS")
        E = self.sb([128, NH, 128], BF16)
        RE = P.R("E")
        cbm = self.sb([128, NG, 128], BF16)
        Rcbm = P.R("cbm")
        MT = [self.sb([128, NH, 128], BF16) for _ in range(2)]
        RMT = P.Rs("MT", 2)
        y = self.sb([128, D_INNER], F32)
        Ry = P.Rs("y", NG)
        junk = self.sb([128, 512], BF16)
        Rjunk = P.R("junk")
        yn = self.sb([128, D_INNER], BF16)
        Ryn = P.R("yn")
        ynT = self.sb([128, 16, 128], BF16)
        RynT = P.R("ynT")
        r1 = self.sb([128, D], F32)
        r = [r1, r1]
        Rr1 = P.R("b0_r")
        Rr = [Rr1, Rr1]
        xTb = [self.sb([128, KC, 128], BF16) for _ in range(2)]
        RxTb = P.Rs("b0_xTb", 2)
        xTf1 = self.sb([128, KC, 128], F32)
        xTf = [xTf1, xTf1]
        RxTf1 = P.R("b0_xTf")
        RxTf = [RxTf1, RxTf1]
        cb = [self.sb([128, NE], F32) for _ in range(2)]
        Rcb = P.Rs("b0_cb", 2)

        def v64(a):
            return a.rearrange("p (h q) -> p h q", q=64)

        def bh(a, n=NH):
            return a.unsqueeze(2).to_broadcast([128, n, 64])

        def smv(b):
            s_ = sm[b]
            return dict(s=s_, dt=s_[:, 0:32], da=s_[:, 32:64], ecum=s_[:, 64:96], etot=s_[:, 96:128],
                        wexp=s_[:, 128:160], ss=s_[:, 192:196], rstd=s_[:, 196:200], tmp=s_[:, 200:204])

        def loads(t):
            lb = t % NLB
            P.dma("sp", xs[t % 2], xs_d[t], reads=Rin, writes=[RldF[t % 2]])
            P.dma("sp", btok[t % 2], bt_d[t], reads=Rin, writes=[RldF[t % 2]])
            P.dma("sp", bct[lb], bct_d[t], reads=Rin, writes=[Rld[lb]])
            P.dma("sp", sz[lb], sz_d[t], reads=Rin, writes=[Rld[lb]])
            P.dma("sp", xr[lb], x_d[t * 128:(t + 1) * 128, :], writes=[Rld[lb]])
            P.dma("sp", sm[lb][:, 0:160], dt_d[:, t].rearrange("p a h -> p (a h)"), reads=Rin, writes=[Rsm[lb]])

        def front(t):
            b = t % 2
            lb = t % NLB
            v = smv(lb)
            s_, da, etot, wexp, dtt = v["s"], v["da"], v["etot"], v["wexp"], v["dt"]
            Rdt = Rsm[lb]
            P.op("dve", lambda e: e.tensor_tensor(out=v64(xsdt[b]), in0=v64(xs[b]), in1=bh(dtt), op=ALU.mult),
                 reads=[RldF[b], Rdt], writes=[Rxsdt[b]])
            P.op("pool", lambda e: e.tensor_tensor(out=v64(xsD[b]), in0=v64(xs[b]), in1=bh(dsk_b), op=ALU.mult),
                 reads=[RldF[b], Rdsk], writes=[RxsD[b]])
            P.op("dve", lambda e: e.tensor_tensor(out=v64(xw), in0=v64(xsdt[b]), in1=bh(wexp), op=ALU.mult),
                 reads=[Rxsdt[b], Rsm[lb]], writes=[Rxw])
            for q in range(8):
                if q % 4 == 0:
                    hh0 = (q // 4) * 16
                    P.op("dve", lambda e, hh0=hh0: e.tensor_tensor(
                        out=rhsS, in0=mU.unsqueeze(1).to_broadcast([128, NH // 2, 128]),
                        in1=da[:, hh0:hh0 + 16].unsqueeze(2).to_broadcast([128, NH // 2, 128]), op=ALU.mult),
                        reads=[Rsm[lb], Rc], writes=[RrhsS])
                bi = 1 + q % 2
                pq = self.bank(bi)
                Rpq = self.Rbank[bi]
                P.op("pe", lambda e, pq=pq, q=q: e.matmul(pq, mS, rhsS[:, 4 * (q % 4):4 * (q % 4) + 4, :],
                                                           start=True, stop=True),
                     reads=[RrhsS, Rc], writes=[Rpq])
                P.op("act", lambda e, pq=pq, q=q: e.activation(
                    out=E[:, 4 * q:4 * q + 4, :], in_=pq.rearrange("p (a b) -> p a b", a=4), func=AF.Exp),
                    reads=[Rpq], writes=[RE, Rpq])
            b3 = self.bank(3)
            Rb3 = self.Rbank[3]
            for g in range(NG):
                P.op("pe", lambda e, g=g: e.matmul(b3[:, g * 128:(g + 1) * 128], bct[lb][:, g, :],
                                                    bct[lb][:, 4 + g, :], start=True, stop=True),
                     reads=[Rld[lb]], writes=[Rb3])
            P.op("dve", lambda e: e.tensor_tensor(out=cbm, in0=b3.rearrange("p (a b) -> p a b", a=4),
                                                  in1=mU.unsqueeze(1).to_broadcast([128, NG, 128]), op=ALU.mult),
                 reads=[Rb3, Rc], writes=[Rcbm, Rb3])
            for g in range(NG):
                eng = "dve" if g != 3 else "pool"
                P.op(eng, lambda e, g=g: e.tensor_tensor(
                    out=MT[b][:, g * 8:(g + 1) * 8, :], in0=E[:, g * 8:(g + 1) * 8, :],
                    in1=cbm[:, g:g + 1, :].to_broadcast([128, 8, 128]), op=ALU.mult),
                    reads=[RE, Rcbm], writes=[RMT[b]])
            hn = (t + 1) % 3
            for g in range(NG):
                gs = slice(g * 512, (g + 1) * 512)
                ph = self.bank(0)
                Rph = self.Rbank[0]
                P.op("pe", lambda e, g=g, gs=gs: e.matmul(ph, btok[b][:, g * 128:(g + 1) * 128], xw[:, gs],
                                                           start=True, stop=True),
                     reads=[RldF[b], Rxw], writes=[Rph])
                P.op("dve", lambda e, gs=gs, g=g: e.tensor_tensor(
                    out=v64(hT[:, gs]), in0=v64(hT[:, gs]), in1=bh(etot[:, g * 8:(g + 1) * 8], 8), op=ALU.mult),
                    reads=[RhT, Rsm[lb]], writes=[RhT])
                P.op("dve", lambda e, gs=gs: e.tensor_tensor(out=hT[:, gs], in0=ph, in1=hT[:, gs], op=ALU.add),
                     reads=[RhT, Rph], writes=[RhT, Rph])
                P.op("act", lambda e, gs=gs: e.copy(out=hTb[hn][:, gs], in_=hT[:, gs]), reads=[RhT],
                     writes=[RhTb[hn]])

        def back(t):
            b = t % 2
            lb = t % NLB
            hc = t % 3
            v = smv(lb)
            ecum, ss, rstd, tmp = v["ecum"], v["ss"], v["rstd"], v["tmp"]
            for g in range(NG):
                py = self.bank(4 + 2 * (g % 2))
                Rpy = self.Rbank[4 + 2 * (g % 2)]
                pys = self.bank(5 + 2 * (g % 2))
                Rpys = self.Rbank[5 + 2 * (g % 2)]
                gs = slice(g * 512, (g + 1) * 512)
                P.op("pe", lambda e, py=py, gs=gs: e.matmul(py, iB, xsD[b][:, gs], start=True, stop=False),
                     reads=[RxsD[b], Rc], writes=[Rpy])
                for hl in range(8):
                    h = g * 8 + hl
                    P.op("pe", lambda e, py=py, hl=hl, h=h: e.matmul(
                        py[:, hl * 64:(hl + 1) * 64], MT[b][:, h, :], xsdt[b][:, h * 64:(h + 1) * 64], start=False,
                        stop=(hl == 7)), reads=[RMT[b], Rxsdt[b]], writes=[Rpy])
                P.op("pe", lambda e, pys=pys, g=g, gs=gs: e.matmul(pys, bct[lb][:, 4 + g, :], hTb[hc][:, gs],
                                                                    start=True, stop=True),
                     reads=[Rld[lb], RhTb[hc]], writes=[Rpys])
                P.op("dve", lambda e, pys=pys, gs=gs, g=g: e.tensor_tensor(
                    out=v64(y[:, gs]), in0=v64(pys), in1=bh(ecum[:, g * 8:(g + 1) * 8], 8), op=ALU.mult),
                    reads=[Rpys, Rsm[lb]], writes=[Ry[g], Rpys])
                P.op("dve", lambda e, py=py, gs=gs: e.tensor_tensor(out=y[:, gs], in0=py, in1=y[:, gs], op=ALU.add),
                     reads=[Rpy, Ry[g]], writes=[Ry[g], Rpy])
                P.op("dve", lambda e, gs=gs: e.tensor_tensor(out=y[:, gs], in0=y[:, gs], in1=sz[lb][:, gs],
                                                              op=ALU.mult),
                     reads=[Ry[g], Rld[lb]], writes=[Ry[g]])
                P.op("act", lambda e, gs=gs, g=g: e.activation(out=junk, in_=y[:, gs], func=AF.Square,
                                                                accum_out=ss[:, g:g + 1]),
                     reads=[Ry[g]], writes=[Rjunk, Rsm[lb]])
            P.op("dve", lambda e: e.tensor_scalar(out=tmp, in0=ss, scalar1=1.0 / 512.0, scalar2=RMS_EPS,
                                                  op0=ALU.mult, op1=ALU.add), reads=[Rsm[lb]], writes=[Rsm[lb]])
            P.op("act", lambda e: e.activation(out=tmp, in_=tmp, func=AF.Sqrt), reads=[Rsm[lb]], writes=[Rsm[lb]])
            P.op("dve", lambda e: e.reciprocal(out=rstd, in_=tmp), reads=[Rsm[lb]], writes=[Rsm[lb]])
            for g in range(NG):
                gs = slice(g * 512, (g + 1) * 512)
                P.op("dve", lambda e, gs=gs, g=g: e.scalar_tensor_tensor(
                    out=yn[:, gs], in0=y[:, gs], scalar=rstd[:, g:g + 1], in1=normw_b[:, gs], op0=ALU.mult,
                    op1=ALU.mult), reads=[Ry[g], Rsm[lb], Rnw], writes=[Ryn])
            for hh in range(2):
                pt = self.bank(4 + hh, BF16)
                Rpt = self.Rbank[4 + hh]
                for k in range(8):
                    kk = hh * 8 + k
                    P.op("pe", lambda e, pt=pt, k=k, kk=kk: e.transpose(pt[:, k * 128:(k + 1) * 128],
                                                                        yn[:, kk * 128:(kk + 1) * 128], iB),
                         reads=[Ryn, Rc], writes=[Rpt])
                P.op("act", lambda e, pt=pt, hh=hh: e.copy(out=ynT[:, hh * 8:(hh + 1) * 8, :],
                                                           in_=pt.rearrange("p (a b) -> p a b", a=8)),
                     reads=[Rpt], writes=[RynT, Rpt])
            for n in range(2):
                pm = self.bank(6 + n)
                Rpm = self.Rbank[6 + n]
                ns = slice(n * 512, (n + 1) * 512)
                for k in range(16):
                    P.op("pe", lambda e, pm=pm, k=k, ns=ns: e.matmul(pm, ynT[:, k, :], wout[:, k, ns],
                                                                      start=(k == 0), stop=(k == 15)),
                         reads=[RynT, Rwo], writes=[Rpm])
                P.op("dve", lambda e, pm=pm, ns=ns: e.scalar_tensor_tensor(
                    out=r[b][:, ns], in0=xr[lb][:, ns], scalar=ALPHA, in1=pm, op0=ALU.mult, op1=ALU.add),
                    reads=[Rld[lb], Rpm], writes=[Rr[b], Rpm])
            self.layer_norm(r[b], Rr[b], st, mv, Rs, g_b, b_b, Rgb)
            P.store("sp", x1_d[t * 128:(t + 1) * 128, :], r[b], Rr[b], Rx1_d)
            self.make_xT(r[b], Rr[b], 4, xTb[b], RxTb[b], xTf[b], RxTf[b])
            P.store("sp", x1T_d[t], xTb[b], RxTb[b], Rx1T_d)
            self.router(xTf[b], RxTf[b], 6, cb[b], Rcb[b])
            P.store("sp", comb_d[t], cb[b], Rcb[b], Rcomb_d)

        loads(0)
        if NT > 1:
            loads(1)
        front(0)
        for t in range(NT):
            if t + 2 < NT:
                loads(t + 2)
            m0 = P.mark()
            if t + 1 < NT:
                front(t + 1)
            m1 = P.mark()
            back(t)
            P.interleave(m0, m1)
        P.barrier()
        self.off = mark

    def attn_bias_setup(self, rel_d, frep_d, expb_d, Rexpb_d):
        P = self.P
        expB = self.sb([128, AH, 5, 128], BF16)
        Rbias = P.R("bias_build")
        tab = self.sb([128, 2, AH], F32)
        Rtab = P.R("tab")
        P.dma("sp", tab, rel_d[0, 1:257, :].rearrange("(a p) h -> p a h", p=128), writes=[Rtab])
        FT = self.sb([AH, 768], F32)
        RFT = P.R("FT")
        pb = self.bank(0)
        Rpb = self.Rbank[0]
        for a in range(2):
            P.op("pe", lambda e, a=a: e.transpose(pb[0:AH, a * 128:(a + 1) * 128], tab[:, a, :], self.identF),
                 reads=[Rtab, self.Rc], writes=[Rpb])
        P.op("dve", lambda e: e.tensor_copy(out=FT[:, 0:256], in_=pb[0:AH, 0:256]), reads=[Rpb], writes=[RFT, Rpb])
        P.op("dve", lambda e: e.tensor_copy(out=FT[:, 256:768], in_=FT[:, 255:256].to_broadcast([AH, 512])),
             reads=[RFT], writes=[RFT])
        Rfrep = P.R("frep")
        P.store("sp", frep_d, FT.unsqueeze(1).to_broadcast([AH, 128, 768]), RFT, Rfrep)
        stage = self.sb([128, AH, 5, 128], F32)
        Rst = P.R("bstage")
        for h in range(AH):
            src = bass.AP(frep_d.tensor, h * 128 * 768 + 127, [[767, 128], [128, 5], [1, 128]])
            P.dma(("sp", "act")[h % 2], stage[:, h, :, :], src, reads=[Rfrep], writes=[Rst])
        P.op("pool", lambda e: e.memset(stage[0:64, :, 4, 64:128], -30000.0), reads=[Rst], writes=[Rst])
        P.op("pool", lambda e: e.memset(stage[64:128, :, 0, 0:64], -30000.0), reads=[Rst], writes=[Rst])
        for jr in range(5):
            P.op("act", lambda e, jr=jr: e.activation(out=expB[:, :, 4 - jr, :], in_=stage[:, :, jr, :],
                                                      func=AF.Exp), reads=[Rst], writes=[Rbias])
        P.store("sp", expb_d, expB, Rbias, Rexpb_d)

    def phase_attn(self, x_d, xT_d, Rin, wqkv_d, bqkv_d, wo_d, bo_d, rel_d, frep_d, lng_d, lnb_d,
                   x3_d, Rx3_d, x3T_d, Rx3T_d, comb_d, Rcomb_d, wconv=None):
        P = self.P
        NT = self.NT
        mark = self.off
        wsteps = wconv() if wconv is not None else None
        self.expB = self.sb([128, AH, 5, 128], BF16)
        self.Rbias = P.R("bias")
        P.dma("sp", self.expB, rel_d, reads=[frep_d], writes=[self.Rbias])
        iB, oB, Rc = self.identB, self.onesB, self.Rc
        RS = 6
        wqkv = self.sb([128, KC, 3 * D], BF16)
        Rwq = P.Rs("wqkv", 3)
        for j in range(6):
            P.dma("pool", wqkv[:, :, j * 512:(j + 1) * 512],
                  wqkv_d[0, :, j * 512:(j + 1) * 512].rearrange("(k p) n -> p k n", p=128), writes=[Rwq[j // 2]])
        wo = self.sb([128, KC, D], BF16)
        Rwo = P.R("wo")
        P.dma("pool", wo, wo_d[0].rearrange("(k p) n -> p k n", p=128), writes=[Rwo])
        browB = self.sb([1, 4 * D], BF16)
        Rbr = P.R("brow")
        P.dma("pool", browB[:, 0:3 * D], bqkv_d[0:1, :], writes=[Rbr])
        P.dma("pool", browB[:, 3 * D:4 * D], bo_d[0:1, :], writes=[Rbr])
        g_b, Rg = self.bcast_load(lng_d[1:2, :], D, "act")
        b_b, Rb_ = self.bcast_load(lnb_d[1:2, :], D, "sp")
        Rgb = P.R("at_gb")
        st = self.sb([128, 2, 6], F32)
        mv = self.sb([128, 8], F32)
        Rs = P.R("at_ln")
        P.op("pool", lambda e: e.tensor_copy(out=mv[:, 7:8], in_=g_b[:, 0:1]), reads=[Rg, Rb_], writes=[Rgb])
        KT = self.sb([128, KC, RS, 128], BF16)
        RK = P.Rs("KTr", RS)
        V = self.sb([128, RS, AH, 65], BF16)
        RV = P.Rs("Vr", RS)
        for sl in range(RS):
            P.op("pool", lambda e, sl=sl: e.memset(V[:, sl, :, 64:65], 1.0), writes=[RV[sl]])
        QT = [self.sb([128, KC, 128], BF16) for _ in range(2)]
        RQ = P.Rs("QT", 2)
        xT = [self.sb([128, KC, 128], BF16) for _ in range(2)]
        xr = [self.sb([128, D], F32) for _ in range(2)]
        Rld = P.Rs("at_ld", 2)
        NPB = 4
        PT = [self.sb([128, 5 * 128], BF16) for _ in range(NPB)]
        RPT = P.Rs("PT", NPB)
        O = self.sb([128, D], BF16)
        RO = P.R("O")
        rec = self.sb([128, 2, 4], F32)
        Rrec = P.Rs("rec", 2)
        OT = self.sb([128, KC, 128], BF16)
        ROT = P.R("OT")
        r = [self.sb([128, D], F32) for _ in range(2)]
        Rr = P.Rs("at_r", 2)
        xTb = [self.sb([128, KC, 128], BF16) for _ in range(2)]
        RxTb = P.Rs("at_xTb", 2)
        xTf = [self.sb([128, KC, 128], F32) for _ in range(2)]
        RxTf = P.Rs("at_xTf", 2)
        cb = [self.sb([128, NE], F32) for _ in range(2)]
        Rcb = P.Rs("at_cb", 2)
        hcount = [0]

        def proj(t):
            b = t % 2
            sl = t % RS
            P.dma("sp", xT[b], xT_d[t], reads=Rin, writes=[Rld[b]])
            P.dma("sp", xr[b], x_d[t * 128:(t + 1) * 128, :], reads=Rin, writes=[Rld[b]])
            for which in range(2):
                for hf in range(2):
                    bi = 6 + hf
                    pq = self.bank(bi)
                    Rpq = self.Rbank[bi]
                    for mm in range(4):
                        m = hf * 4 + mm
                        col = which * D + m * 128
                        for k in range(KC):
                            P.op("pe", lambda e, pq=pq, mm=mm, k=k, col=col: e.matmul(
                                pq[:, mm * 128:(mm + 1) * 128], wqkv[:, k, col:col + 128], xT[b][:, k, :],
                                start=(k == 0), stop=False), reads=[Rwq[col // 1024], Rld[b]], writes=[Rpq])
                        P.op("pe", lambda e, pq=pq, mm=mm, col=col: e.matmul(
                            pq[:, mm * 128:(mm + 1) * 128], browB[0:1, col:col + 128], oB[0:1, 0:128],
                            start=False, stop=True), reads=[Rbr, Rc], writes=[Rpq])
                    pv3 = pq.rearrange("p (a b) -> p a b", a=4)
                    if which == 0:
                        P.op("act", lambda e, pv3=pv3, hf=hf: e.mul(out=QT[b][:, hf * 4:(hf + 1) * 4, :], in_=pv3,
                                                                    mul=0.125),
                             reads=[Rpq], writes=[RQ[b], Rpq])
                    else:
                        P.op("dve", lambda e, pv3=pv3, hf=hf: e.tensor_copy(
                            out=KT[:, hf * 4:(hf + 1) * 4, sl, :], in_=pv3), reads=[Rpq], writes=[RK[sl], Rpq])
            for n in range(2):
                bi = 6 + n
                pv = self.bank(bi)
                Rpv = self.Rbank[bi]
                for k in range(KC):
                    P.op("pe", lambda e, pv=pv, k=k, n=n: e.matmul(
                        pv, xT[b][:, k, :], wqkv[:, k, 2 * D + n * 512:2 * D + (n + 1) * 512], start=(k == 0),
                        stop=False), reads=[Rwq[2], Rld[b]], writes=[Rpv])
                P.op("pe", lambda e, pv=pv, n=n: e.matmul(pv, oB[0:1, 0:128],
                                                          browB[0:1, 2 * D + n * 512:2 * D + (n + 1) * 512],
                                                          start=False, stop=True), reads=[Rbr, Rc], writes=[Rpv])
                if n == 0:
                    P.op("act", lambda e, pv=pv, n=n: e.copy(out=V[:, sl, n * 8:(n + 1) * 8, 0:64],
                                                            in_=pv.rearrange("p (a b) -> p a b", a=8)),
                         reads=[Rpv], writes=[RV[sl], Rpv])
                else:
                    P.op("dve", lambda e, pv=pv, n=n: e.tensor_copy(out=V[:, sl, n * 8:(n + 1) * 8, 0:64],
                                                                   in_=pv.rearrange("p (a b) -> p a b", a=8)),
                         reads=[Rpv], writes=[RV[sl], Rpv])

        def heads(t):
            b = t % 2
            nj = min(t, 4) + 1
            js = list(range(t - nj + 1, t + 1))

            def scores(h):
                m, p0 = h // 2, (h % 2) * 64
                pi = hcount[0] % 2
                pbuf = hcount[0] % NPB
                hcount[0] += 1
                pst = self.pair(pi)
                Rps = [self.Rbank[2 * pi], self.Rbank[2 * pi + 1]]
                for jj, j in enumerate(js):
                    ksl = j % RS
                    o_ = pst[:, jj * 128:(jj + 1) * 128]
                    wr = [Rps[0]] if jj < 4 else [Rps[1]]
                    P.op("pe", lambda e, o_=o_, ksl=ksl: e.matmul(
                        o_, KT[p0:p0 + 64, m, ksl, :], QT[b][p0:p0 + 64, m, :], start=True, stop=True),
                        reads=[RK[ksl], RQ[b]], writes=wr)
                wrs = Rps if nj == 5 else [Rps[0]]
                P.op("act", lambda e: e.activation(out=PT[pbuf][:, 0:nj * 128], in_=pst[:, 0:nj * 128], func=AF.Exp),
                     reads=wrs, writes=[RPT[pbuf]] + wrs)
                P.op(("pool" if (h % 2 and wsteps is None) else "dve"), lambda e: e.tensor_tensor(
                    out=PT[pbuf][:, 0:nj * 128], in0=PT[pbuf][:, 0:nj * 128],
                    in1=self.expB[:, h, 5 - nj:5, :].rearrange("p a b -> p (a b)"), op=ALU.mult),
                    reads=[RPT[pbuf], self.Rbias], writes=[RPT[pbuf]])
                return pbuf

            def pv_out(h, pbuf):
                hq, hl = h // 4, h % 4
                ob = 4 + hq % 2
                po = self.bank(ob)
                Rpo = self.Rbank[ob]
                for jj, j in enumerate(js):
                    ksl = j % RS
                    lastj = (jj == len(js) - 1)
                    P.op("pe", lambda e, jj=jj, ksl=ksl, lastj=lastj: e.matmul(
                        po[:, hl * 128:hl * 128 + 65], PT[pbuf][:, jj * 128:(jj + 1) * 128], V[:, ksl, h, :],
                        start=(jj == 0), stop=lastj), reads=[RPT[pbuf], RV[ksl]], writes=[Rpo])
                if hl == 3:
                    po3 = po.rearrange("p (a b) -> p a b", a=4)
                    rb = hq % 2
                    P.op("dve", lambda e: e.reciprocal(out=rec[:, rb, :].unsqueeze(2), in_=po3[:, :, 64:65]),
                         reads=[Rpo], writes=[Rrec[rb], Rpo])
                    P.op("dve", lambda e: e.tensor_tensor(
                        out=O[:, hq * 256:(hq + 1) * 256].rearrange("p (a b) -> p a b", a=4), in0=po3[:, :, 0:64],
                        in1=rec[:, rb, :].unsqueeze(2).to_broadcast([128, 4, 64]), op=ALU.mult),
                        reads=[Rpo, Rrec[rb]], writes=[RO, Rpo])

            LAG = 2
            pend = []
            for h in range(AH):
                pend.append((h, scores(h)))
                if len(pend) > LAG:
                    pv_out(*pend.pop(0))
            while pend:
                pv_out(*pend.pop(0))
            pt = self.bank(4, BF16)
            Rpt = self.Rbank[4]
            for k in range(KC):
                P.op("pe", lambda e, k=k: e.transpose(pt[:, k * 128:(k + 1) * 128], O[:, k * 128:(k + 1) * 128], iB),
                     reads=[RO, Rc], writes=[Rpt])
            P.op("act", lambda e: e.copy(out=OT, in_=pt.rearrange("p (a b) -> p a b", a=8)), reads=[Rpt],
                 writes=[ROT, Rpt])
            for n in range(2):
                pm = self.bank(n)
                Rpm = self.Rbank[n]
                ns = slice(n * 512, (n + 1) * 512)
                for k in range(KC):
                    P.op("pe", lambda e, pm=pm, k=k, ns=ns: e.matmul(pm, OT[:, k, :], wo[:, k, ns], start=(k == 0),
                                                                      stop=False), reads=[ROT, Rwo], writes=[Rpm])
                P.op("pe", lambda e, pm=pm, n=n: e.matmul(pm, oB[0:1, 0:128],
                                                          browB[0:1, 3 * D + n * 512:3 * D + (n + 1) * 512],
                                                          start=False, stop=True), reads=[Rbr, Rc], writes=[Rpm])
                P.op("dve", lambda e, pm=pm, ns=ns: e.scalar_tensor_tensor(
                    out=r[b][:, ns], in0=xr[b][:, ns], scalar=ALPHA, in1=pm, op0=ALU.mult, op1=ALU.add),
                    reads=[Rld[b], Rpm], writes=[Rr[b], Rpm])
            self.layer_norm(r[b], Rr[b], st, mv, Rs, g_b, b_b, Rgb)
            P.store(self.dq(), x3_d[t * 128:(t + 1) * 128, :], r[b], Rr[b], Rx3_d)
            self.make_xT(r[b], Rr[b], 2, xTb[b], RxTb[b], xTf[b], RxTf[b])
            P.store(self.dq(), x3T_d[t], xTb[b], RxTb[b], Rx3T_d)
            self.router(xTf[b], RxTf[b], 5, cb[b], Rcb[b])
            P.store(self.dq(), comb_d[t], cb[b], Rcb[b], Rcomb_d)

        proj(0)
        for t in range(NT):
            if wsteps is not None:
                for _ in range(2 if t % 2 == 0 else 1):
                    st_ = next(wsteps, None)
                    if st_ is not None:
                        st_()
            m0 = P.mark()
            if t + 1 < NT:
                proj(t + 1)
            m1 = P.mark()
            heads(t)
            P.interleave(m0, m1)
        if wsteps is not None:
            for st_ in wsteps:
                st_()
        P.barrier()
        self.off = mark


    def phase_moe_sparse(self, layer, x_d, comb_d, Rin, wg_d, wu_d, wd_d, lng_d, lnb_d, out_d, Rout,
                         outT_d, RoutT, xsort_d, ysort_d, Rzero, stop_after=9, dbg=None, wb=None, Rwb=None):
        P = self.P
        NT = self.NT
        mark = self.off
        ST = 512
        NSL = (2 * self.T) // ST + NE
        I32 = mybir.dt.int32
        iB, mU, oF, Rc = self.identB, self.maskU, self.onesF, self.Rc
        Rxsort, Rysort = P.R("xsort"), P.R("ysort")
        TE = NT * NE
        comb = self.sb([128, NT, NE], F32)
        M = self.sb([128, NT, NE], F32)
        M0 = self.sb([128, NT, NE], F32)
        M1 = self.sb([128, NT, NE], F32)
        T1 = self.sb([128, NT, NE], F32)
        Cc = self.sb([128, NT, NE], F32)
        tot = self.sb([128, NT, NE], F32)
        Pfx = self.sb([128, NT, NE], F32)
        w16 = self.sb([128, NE], F32)
        sml = self.sb([128, 256], F32)
        cmpT = self.sb([128, NE, 8], F32)
        cmpE = self.sb([128, NSL, NE], F32)
        posg = self.sb([128, 4, NT], F32)
        pos_i = self.sb([128, 2, NT], I32)
        esb = self.sb([128, NSL], F32)
        sthr = self.sb([128, NSL], F32)
        widx_i = self.sb([128, 2, NSL], I32)
        widxd_i = self.sb([128, 4, NSL], I32)
        esd = self.sb([128, NSL], F32)
        Rr_ = P.R("route")
        R1 = [Rr_]
        d = lambda fn, rd=(), wr=(): P.op("dve", fn, reads=R1 + list(rd), writes=R1 + list(wr))
        pl = lambda fn, rd=(), wr=(): P.op("pool", fn, reads=R1 + list(rd), writes=R1 + list(wr))
        P.dma("sp", comb, comb_d.rearrange("t p e -> p t e"), reads=Rin, writes=[Rr_])
        Rk = []
        for e_ in range(NE):
            Rk.append(P.R("k"))
            P.op("pool", lambda e, e_=e_: e.memset(w16[:, e_:e_ + 1], float(NE - e_)), writes=[Rk[-1]])
        for j in range(8):
            Rk.append(P.R("k"))
            P.op("pool", lambda e, j=j: e.memset(sml[:, 64 + j:65 + j], float(ST * j)), writes=[Rk[-1]])
        for s_ in range(NSL):
            Rk.append(P.R("k"))
            P.op("pool", lambda e, s_=s_: e.memset(sthr[:, s_:s_ + 1], float(ST * s_)), writes=[Rk[-1]])
        P.op("pool", lambda e: e.memset(sml[:, 120:121], 0.0), reads=Rk, writes=[Rr_])
        thr = sml[:, 64:72]
        n_e, ntile, npad, off, offend = (sml[:, 0:16], sml[:, 16:32], sml[:, 32:48], sml[:, 80:96], sml[:, 96:112])
        pidx = sml[:, 112:113]

        def bt(a):
            return a.unsqueeze(1).to_broadcast([128, NT, NE])

        def be(a):
            return a.unsqueeze(2).to_broadcast([128, NT, NE])

        self._Rroute = Rr_
        self.route_batched(comb, M, M0, M1, T1, posg.rearrange("p a t -> p (a t)").rearrange("p (a t) -> p a t", a=4)
                           if False else self.sb([128, 8, NT], F32), d)
        d(lambda e: e.tensor_single_scalar(out=M, in_=comb, scalar=0.0, op=ALU.is_gt))
        d(lambda e: e.tensor_tensor(out=T1, in0=M, in1=bt(w16), op=ALU.mult))
        d(lambda e: e.tensor_reduce(out=posg[:, 0, :], in_=T1, axis=AX.X, op=ALU.max))
        d(lambda e: e.tensor_tensor(out=M0, in0=T1, in1=be(posg[:, 0, :]), op=ALU.is_equal))
        d(lambda e: e.tensor_tensor(out=M1, in0=M, in1=M0, op=ALU.subtract))
        pbC, pbT = self.bank(0), self.bank(1)
        Mf = M.rearrange("p t e -> p (t e)")
        P.op("pe", lambda e: e.matmul(pbC[:, 0:TE], mU, Mf, start=True, stop=True), reads=[Rr_, Rc],
             writes=[self.Rbank[0]])
        P.op("pe", lambda e: e.matmul(pbT[:, 0:TE], oF, Mf, start=True, stop=True), reads=[Rr_, Rc],
             writes=[self.Rbank[1]])
        d(lambda e: e.tensor_copy(out=Cc.rearrange("p t e -> p (t e)"), in_=pbC[:, 0:TE]), rd=[self.Rbank[0]],
          wr=[self.Rbank[0]])
        d(lambda e: e.tensor_copy(out=tot.rearrange("p t e -> p (t e)"), in_=pbT[:, 0:TE]), rd=[self.Rbank[1]],
          wr=[self.Rbank[1]])
        d(lambda e: e.memset(Pfx[:, 0, :], 0.0))
        for t in range(1, NT):
            d(lambda e, t=t: e.tensor_tensor(out=Pfx[:, t, :], in0=Pfx[:, t - 1, :], in1=tot[:, t - 1, :], op=ALU.add))
        d(lambda e: e.tensor_tensor(out=n_e, in0=Pfx[:, NT - 1, :], in1=tot[:, NT - 1, :], op=ALU.add))
        d(lambda e: e.tensor_tensor(out=cmpT, in0=n_e.unsqueeze(2).to_broadcast([128, NE, 8]),
                                    in1=thr.unsqueeze(1).to_broadcast([128, NE, 8]), op=ALU.is_gt))
        d(lambda e: e.tensor_reduce(out=ntile, in_=cmpT, axis=AX.X, op=ALU.add))
        d(lambda e: e.tensor_scalar(out=npad, in0=ntile, scalar1=float(ST), scalar2=None, op0=ALU.mult))
        d(lambda e: e.memset(off[:, 0:1], 0.0))
        for e_ in range(1, NE):
            d(lambda e, e_=e_: e.tensor_tensor(out=off[:, e_:e_ + 1], in0=off[:, e_ - 1:e_], in1=npad[:, e_ - 1:e_],
                                               op=ALU.add))
        d(lambda e: e.tensor_tensor(out=offend, in0=off, in1=npad, op=ALU.add))
        d(lambda e: e.tensor_tensor(out=T1, in0=Cc, in1=M, op=ALU.subtract))
        d(lambda e: e.tensor_tensor(out=T1, in0=T1, in1=Pfx, op=ALU.add))
        d(lambda e: e.tensor_tensor(out=T1, in0=T1, in1=bt(off), op=ALU.add))
        d(lambda e: e.tensor_tensor(out=Cc, in0=T1, in1=M0, op=ALU.mult))
        d(lambda e: e.tensor_reduce(out=posg[:, 0, :], in_=Cc, axis=AX.X, op=ALU.add))
        d(lambda e: e.tensor_tensor(out=Cc, in0=T1, in1=M1, op=ALU.mult))
        d(lambda e: e.tensor_reduce(out=posg[:, 1, :], in_=Cc, axis=AX.X, op=ALU.add))
        d(lambda e: e.tensor_tensor(out=Cc, in0=comb, in1=M0, op=ALU.mult))
        d(lambda e: e.tensor_reduce(out=posg[:, 2, :], in_=Cc, axis=AX.X, op=ALU.add))
        d(lambda e: e.tensor_tensor(out=Cc, in0=comb, in1=M1, op=ALU.mult))
        d(lambda e: e.tensor_reduce(out=posg[:, 3, :], in_=Cc, axis=AX.X, op=ALU.add))
        d(lambda e: e.tensor_copy(out=pos_i, in_=posg[:, 0:2, :]))
        d(lambda e: e.tensor_tensor(out=cmpE, in0=offend.unsqueeze(1).to_broadcast([128, NSL, NE]),
                                    in1=sthr.unsqueeze(2).to_broadcast([128, NSL, NE]), op=ALU.is_le))
        d(lambda e: e.tensor_reduce(out=esb, in_=cmpE, axis=AX.X, op=ALU.add))
        d(lambda e: e.tensor_scalar(out=sthr, in0=esb, scalar1=float(NE) - 0.5, scalar2=65536.0, op0=ALU.is_gt,
                                    op1=ALU.mult))
        d(lambda e: e.tensor_scalar(out=esb, in0=esb, scalar1=float(NE - 1), scalar2=None, op0=ALU.min))
        d(lambda e: e.tensor_reduce(out=pidx, in_=mU, axis=AX.X, op=ALU.add), rd=[Rc])
        d(lambda e: e.tensor_scalar(out=pidx, in0=pidx, scalar1=-2.0, scalar2=float(2 * 128),
                                    op0=ALU.mult, op1=ALU.add))
        d(lambda e: e.tensor_scalar(out=esd, in0=esb, scalar1=512.0, scalar2=None, op0=ALU.mult))
        d(lambda e: e.tensor_tensor(out=esd, in0=esd, in1=sthr, op=ALU.add))
        d(lambda e: e.tensor_scalar(out=sml[:, 113:114], in0=pidx, scalar1=0.5, scalar2=None, op0=ALU.mult))
        for k in range(4):
            d(lambda e, k=k: e.tensor_scalar(out=cmpE[:, :, 0], in0=esd, scalar1=sml[:, 113:114], scalar2=float(k * 128),
                                             op0=ALU.add, op1=ALU.add))
            d(lambda e, k=k: e.tensor_copy(out=widxd_i[:, k, :], in_=cmpE[:, :, 0]))
        d(lambda e: e.tensor_scalar(out=esb, in0=esb, scalar1=128.0, scalar2=sml[:, 113:114], op0=ALU.mult,
                                    op1=ALU.add))
        d(lambda e: e.tensor_tensor(out=esb, in0=esb, in1=sthr, op=ALU.add))
        d(lambda e: e.tensor_copy(out=widx_i[:, 0, :], in_=esb))

        if dbg is not None:
            P.store("sp", dbg["pos"], pos_i, Rr_, dbg["R"])
            P.store("sp", dbg["widx"], widx_i, Rr_, dbg["R"])
            P.store("sp", dbg["posg"], posg, Rr_, dbg["R"])
        if stop_after < 2:
            P.barrier()
            self.off = mark
            return
        mark4 = self.off
        xf = [self.sb([128, D], F32) for _ in range(2)]
        Rxf = P.Rs("sp_xf", 2)
        xb = [self.sb([128, D], BF16) for _ in range(2)]
        Rxb = P.Rs("sp_xb", 2)
        for t in range(NT):
            b = t % 2
            P.dma("sp", xf[b], x_d[t * 128:(t + 1) * 128, :], reads=Rin, writes=[Rxf[b]])
            P.op("act", lambda e, b=b: e.copy(out=xb[b], in_=xf[b]), reads=[Rxf[b]], writes=[Rxb[b]])
            for k in range(2):
                i = Ins("pool", (lambda e, b=b, k=k, t=t: e.indirect_dma_start(
                    out=xsort_d[:, :], out_offset=bass.IndirectOffsetOnAxis(ap=pos_i[:, k, t:t + 1], axis=0),
                    in_=xb[b], in_offset=None)), (Rxb[b], Rr_, Rzero), (Rxsort,), True)
                i.key = Rxb[b]
                i.phase = P.phase
                P.ins.append(i)

        if stop_after < 2.4:
            P.barrier()
            self.off = mark
            return
        wg_v, wu_v, wd_v = wb
        wgs = [self.sb([128, KC, DFF], BF16) for _ in range(2)]
        wus = [self.sb([128, KC, DFF], BF16) for _ in range(2)]
        wds = [self.sb([128, 4, D], BF16) for _ in range(2)]
        Rwg, Rwu, Rwd = P.Rs("s_wg", 2), P.Rs("s_wu", 2), P.Rs("s_wd", 2)
        xs_sb = [self.sb([128, 4, D], BF16) for _ in range(2)]
        Rxs = P.Rs("s_xs", 2)
        xTs = [self.sb([128, KC, ST], BF16) for _ in range(2)]
        RxTs = P.Rs("s_xTs", 2)
        sg = [self.sb([128, 512], F32) for _ in range(2)]
        Rsg = P.Rs("s_sg", 2)
        hT = [self.sb([128, 4, 512], BF16) for _ in range(2)]
        RhT = P.Rs("s_hT", 2)
        ysb = [self.sb([128, D], F32) for _ in range(2)]
        Rysb = P.Rs("s_ysb", 2)

        def wgather(dst, src_v, s_, Rw):
            d2 = dst.rearrange("p k f -> p (k f)")
            i = Ins("pool", (lambda e: e.indirect_dma_start(
                out=d2, out_offset=None, in_=src_v,
                in_offset=bass.IndirectOffsetOnAxis(ap=widx_i[:, 0, s_:s_ + 1], axis=0),
                bounds_check=self._bc_reg(e), oob_is_err=False)),
                (Rr_, Rwb), (Rw,), True)
            i.key = Rw
            i.phase = P.phase
            P.ins.append(i)

        def fetch(s_):
            wb = s_ % 2
            wgather(wgs[wb], wg_v, s_, Rwg[wb])
            wgather(wus[wb], wu_v, s_, Rwu[wb])
            for k in range(4):
                i = Ins("pool", (lambda e, k=k: e.indirect_dma_start(
                    out=wds[wb][:, k, :], out_offset=None, in_=wd_v,
                    in_offset=bass.IndirectOffsetOnAxis(ap=widxd_i[:, k, s_:s_ + 1], axis=0),
                    bounds_check=self._bc_reg2(e), oob_is_err=False)), (Rr_, Rwb), (Rwd[wb],), True)
                i.key = Rwd[wb]
                i.phase = P.phase
                P.ins.append(i)
            P.dma("sp", xs_sb[wb], xsort_d[s_ * ST:(s_ + 1) * ST, :].rearrange("(a p) d -> p a d", p=128),
                  reads=[Rxsort], writes=[Rxs[wb]])

        yc = 0
        fetch(0)
        for s_ in range(NSL):
            wb = s_ % 2
            if s_ + 1 < NSL:
                fetch(s_ + 1)
            if stop_after < 2.6:
                continue
            for a in range(4):
                pt = self.bank(a % 2, BF16)
                Rpt = self.Rbank[a % 2]
                xv = xs_sb[wb][:, a, :].rearrange("p (c k) -> p k c", k=KC)
                for k in range(KC):
                    P.op("pe", lambda e, pt=pt, xv=xv, k=k: e.transpose(pt[:, k * 128:(k + 1) * 128], xv[:, k, :], iB),
                         reads=[Rxs[wb], Rc], writes=[Rpt])
                if a % 2 == 0:
                    P.op("act", lambda e, pt=pt, a=a, wb=wb: e.copy(
                        out=xTs[wb][:, :, a * 128:(a + 1) * 128], in_=pt.rearrange("p (k c) -> p k c", k=KC)),
                        reads=[Rpt], writes=[RxTs[wb], Rpt])
                else:
                    P.op("dve", lambda e, pt=pt, a=a, wb=wb: e.tensor_copy(
                        out=xTs[wb][:, :, a * 128:(a + 1) * 128], in_=pt.rearrange("p (k c) -> p k c", k=KC)),
                        reads=[Rpt], writes=[RxTs[wb], Rpt])
            if stop_after < 2.8:
                continue
            hb = s_ % 2
            for c in range(4):
                pg = self.bank(2)
                Rpg = self.Rbank[2]
                pu = self.bank(3)
                Rpu = self.Rbank[3]
                sgb = c % 2
                wgc = wgs[wb].rearrange("p k (c q) -> p k c q", c=4)
                wuc = wus[wb].rearrange("p k (c q) -> p k c q", c=4)
                for k in range(KC):
                    P.op("pe", lambda e, pg=pg, k=k, c=c, wgc=wgc, wb=wb: e.matmul(
                        pg, wgc[:, k, c, :], xTs[wb][:, k, :], start=(k == 0), stop=(k == KC - 1)),
                        reads=[Rwg[wb], RxTs[wb]], writes=[Rpg])
                for k in range(KC):
                    P.op("pe", lambda e, pu=pu, k=k, c=c, wuc=wuc, wb=wb: e.matmul(
                        pu, wuc[:, k, c, :], xTs[wb][:, k, :], start=(k == 0), stop=(k == KC - 1)),
                        reads=[Rwu[wb], RxTs[wb]], writes=[Rpu])
                P.op("act", lambda e, pg=pg, sgb=sgb: e.activation(out=sg[sgb], in_=pg, func=AF.Silu),
                     reads=[Rpg], writes=[Rsg[sgb], Rpg])
                P.op("dve", lambda e, pu=pu, sgb=sgb, hb=hb, c=c: e.tensor_tensor(
                    out=hT[hb][:, c, :], in0=pu, in1=sg[sgb], op=ALU.mult),
                    reads=[Rpu, Rsg[sgb]], writes=[RhT[hb], Rpu])
            for a in range(4):
                yb = yc % 2
                yc += 1
                for n in range(2):
                    bi = 4 + (a % 2) * 2 + n
                    po = self.bank(bi)
                    Rpo = self.Rbank[bi]
                    for c in range(4):
                        P.op("pe", lambda e, po=po, c=c, a=a, n=n, hb=hb, wb=wb: e.matmul(
                            po, hT[hb][:, c, a * 128:(a + 1) * 128], wds[wb][:, c, n * 512:(n + 1) * 512],
                            start=(c == 0), stop=(c == 3)), reads=[RhT[hb], Rwd[wb]], writes=[Rpo])
                    if n == 0:
                        P.op("act", lambda e, po=po, yb=yb: e.copy(out=ysb[yb][:, 0:512], in_=po),
                             reads=[Rpo], writes=[Rysb[yb], Rpo])
                    else:
                        P.op("dve", lambda e, po=po, yb=yb: e.tensor_copy(out=ysb[yb][:, 512:1024], in_=po),
                             reads=[Rpo], writes=[Rysb[yb], Rpo])
                r0 = s_ * ST + a * 128
                P.store("sp", ysort_d[r0:r0 + 128, :], ysb[yb], Rysb[yb], Rysort)

        if stop_after < 4:
            P.barrier()
            self.off = mark
            return
        P.barrier()
        self.off = mark4
        NB4 = 8
        g_b, Rg = self.bcast_load(lng_d[layer:layer + 1, :], D, "sp")
        b_b, Rb_ = self.bcast_load(lnb_d[layer:layer + 1, :], D, "sp")
        Rgb = P.R("sp_gb")
        dmy = self.sb([128, 8], F32)
        P.op("pool", lambda e: e.tensor_copy(out=dmy[:, 7:8], in_=g_b[:, 0:1]), reads=[Rg, Rb_], writes=[Rgb])
        st = [self.sb([128, 2, 6], F32) for _ in range(NB4)]
        mv = [self.sb([128, 8], F32) for _ in range(NB4)]
        Rs = P.Rs("sp_ln", NB4)
        acc = [self.sb([128, D], F32) for _ in range(NB4)]
        Racc = P.Rs("sp_acc", NB4)
        yg = [[self.sb([128, D], F32) for _ in range(NB4)] for _ in range(2)]
        Ryg = [P.Rs("sp_yg%d" % k, NB4) for k in range(2)]
        xTo = [self.sb([128, KC, 128], BF16) for _ in range(NB4)]
        RxTo = P.Rs("sp_xTo", NB4)

        def fetch4(t):
            b = t % NB4
            P.dma("sp", acc[b], x_d[t * 128:(t + 1) * 128, :], reads=Rin, writes=[Racc[b]])
            for k in range(2):
                i = Ins("pool", (lambda e, k=k: e.indirect_dma_start(
                    out=yg[k][b], out_offset=None, in_=ysort_d[:, :],
                    in_offset=bass.IndirectOffsetOnAxis(ap=pos_i[:, k, t:t + 1], axis=0))),
                    (Rysort, Rr_), (Ryg[k][b],), True)
                i.key = Ryg[k][b]
                i.phase = P.phase
                P.ins.append(i)

        def comb4(t):
            b = t % NB4
            for k in range(2):
                P.op("act", lambda e, k=k: e.activation(out=yg[k][b], in_=yg[k][b], func=AF.Identity,
                                                        scale=posg[:, 2 + k, t:t + 1]),
                     reads=[Ryg[k][b], Rr_], writes=[Ryg[k][b]])
            P.op("pool", lambda e: e.tensor_tensor(out=yg[0][b], in0=yg[0][b], in1=yg[1][b], op=ALU.add),
                 reads=[Ryg[0][b], Ryg[1][b]], writes=[Ryg[0][b]])
            P.op("dve", lambda e: e.scalar_tensor_tensor(out=acc[b], in0=acc[b], scalar=ALPHA, in1=yg[0][b],
                                                         op0=ALU.mult, op1=ALU.add),
                 reads=[Ryg[0][b], Racc[b]], writes=[Racc[b]])
            self.layer_norm(acc[b], Racc[b], st[b], mv[b], Rs[b], g_b, b_b, Rgb, eng2="dve")
            if outT_d is not None:
                self.make_xT(acc[b], Racc[b], 2 * (b % 4), xTo[b], RxTo[b])
            P.store("sp", out_d[t * 128:(t + 1) * 128, :], acc[b], Racc[b], Rout)
            if outT_d is not None:
                P.store("sp", outT_d[t], xTo[b], RxTo[b], RoutT)

        GS = 4
        for t in range(min(GS, NT)):
            fetch4(t)
        for t0 in range(0, NT, GS):
            ts = list(range(t0, min(NT, t0 + GS)))
            for t in ts:
                if t + GS < NT:
                    fetch4(t + GS)
            ms = []
            for t in ts:
                ms.append(P.mark())
                comb4(t)
            if len(ts) == 4:
                P.interleave(ms[2], ms[3])
                tail = P.ins[ms[2]:]
                del P.ins[ms[2]:]
                P.interleave(ms[0], ms[1])
                mid = len(P.ins)
                P.ins.extend(tail)
                P.interleave(ms[0], mid)
            elif len(ts) >= 2:
                P.interleave(ms[0], ms[1])
        P.barrier()
        self.off = mark


W_SHAPES = {
    "ssm_w_in": [1, D, SSD_IN], "ssm_conv_w": [1, 4, 3072], "ssm_conv_b": [1, 3072], "ssm_dt_bias": [1, NH],
    "ssm_a_log": [1, NH], "ssm_d": [1, NH], "ssm_norm_w": [1, D_INNER], "ssm_w_out": [1, D_INNER, D],
    "att_w_qkv": [1, D, 3 * D], "att_b_qkv": [1, 3 * D], "att_rel_bias": [1, 257, AH], "att_w_o": [1, D, D],
    "att_b_o": [1, D], "router_w": [D, NE], "router_bias": [NE],
    "moe_w_gate": [DEPTH, NE, D, DFF], "moe_w_up": [DEPTH, NE, D, DFF], "moe_w_down": [DEPTH, NE, DFF, D],
    "ln_mix_g": [DEPTH, D], "ln_mix_b": [DEPTH, D], "ln_ffn_g": [DEPTH, D], "ln_ffn_b": [DEPTH, D],
}


def build_full(T=4096, debug=False, sparse=True):
    B = Builder(T)
    NT = B.NT
    P = B.P
    x_d = B.dt_in("x", [T, D])
    w = {k: B.dt_in(k, v) for k, v in W_SHAPES.items()}
    scr = lambda n, shp, dt=F32: B.dt_scr(n, shp, dt, debug=debug)
    xT0 = scr("xT0", [NT, 128, KC, 128], BF16)
    sz = scr("sz", [NT, 128, D_INNER], BF16)
    dt = scr("dt", [128, NT, 5, NH], F32)
    xs = scr("xs", [NT, 128, D_INNER], BF16)
    bt = scr("bt", [NT, 128, 512], BF16)
    bct = scr("bct", [NT, 128, 8, 128], BF16)
    x1 = scr("x1", [T, D])
    x1T = scr("x1T", [NT, 128, KC, 128], BF16)
    c1 = scr("comb1", [NT, 128, NE])
    x2 = scr("x2", [T, D])
    x2T = scr("x2T", [NT, 128, KC, 128], BF16)
    x3 = scr("x3", [T, D])
    x3T = scr("x3T", [NT, 128, KC, 128], BF16)
    c3 = scr("comb3", [NT, 128, NE])
    y = B.dt_out("y", [T, D])
    R = {n: P.R("d_" + n) for n in "xT0 sz dt xs bt bct x1 x1T c1 x2 x2T x3 x3T c3 y".split()}
    frep = B.dt_scr("frep", [AH, 128, 768], F32)
    B.consts()
    B.router_setup(w["router_w"], w["router_bias"])
    NSLOT = ((2 * T) // 512 + NE) * 512
    xsort = [B.dt_scr("xsort%d" % l, [NSLOT, D], BF16) for l in range(2)]
    ysort = B.dt_scr("ysort", [NSLOT, D], F32)
    Rz = [P.R("zero%d" % l) for l in range(2)]
    expb = B.dt_scr("expb", [128, AH, 5, 128], BF16)
    Rexpb = P.R("d_expb")
    wbs = [(B.dt_scr("wgb%d" % l, [NE * 128, 4096], BF16), B.dt_scr("wub%d" % l, [NE * 128, 4096], BF16),
            B.dt_scr("wdb%d" % l, [NE * DFF, D], BF16)) for l in range(2)]
    Rwbs = [P.R("d_wb%d" % l) for l in range(2)]
    mkconv = lambda l, rng=None, nb=2: (lambda: B.wconv_steps(l, w["moe_w_gate"], w["moe_w_up"], w["moe_w_down"],
                                                           wbs[l], Rwbs[l], ex_range=rng, nbuf=nb))
    B.phase_a0z(x_d, w["ssm_w_in"], w["ssm_dt_bias"], w["ssm_a_log"], xT0, R["xT0"], sz, R["sz"], dt, R["dt"],
                early=lambda: B.attn_bias_setup(w["att_rel_bias"], frep, expb, Rexpb),
                wconv=(mkconv(0, None, 4) if sparse else None))
    B.phase_a0x(w["ssm_w_in"], w["ssm_conv_w"], w["ssm_conv_b"], xT0, R["xT0"], xs, R["xs"], bt, R["bt"], bct,
                R["bct"], zfill=([(xsort[0], Rz[0]), (xsort[1], Rz[1])] if sparse else ()),
                wconv=None)
    B.phase_b0(x_d, sz, dt, xs, bt, bct, [R["sz"], R["dt"], R["xs"], R["bt"], R["bct"]], w["ssm_a_log"], w["ssm_d"],
               w["ssm_norm_w"], w["ssm_w_out"], w["ln_mix_g"], w["ln_mix_b"], x1, R["x1"], x1T, R["x1T"], c1, R["c1"])
    if sparse:
        B.phase_moe_sparse(0, x1, c1, [R["x1"], R["c1"]], w["moe_w_gate"], w["moe_w_up"], w["moe_w_down"],
                           w["ln_ffn_g"], w["ln_ffn_b"], x2, R["x2"], x2T, R["x2T"], xsort[0], ysort, Rz[0],
                           wb=wbs[0], Rwb=Rwbs[0])
    else:
        B.phase_moe(0, x1, x1T, c1, [R["x1"], R["x1T"], R["c1"]], w["moe_w_gate"], w["moe_w_up"], w["moe_w_down"],
                    w["ln_ffn_g"], w["ln_ffn_b"], x2, R["x2"], x2T, R["x2T"])
    B.phase_attn(x2, x2T, [R["x2"], R["x2T"]], w["att_w_qkv"], w["att_b_qkv"], w["att_w_o"], w["att_b_o"],
                 expb, Rexpb, w["ln_mix_g"], w["ln_mix_b"], x3, R["x3"], x3T, R["x3T"], c3, R["c3"],
                 wconv=(mkconv(1) if sparse else None))
    if sparse:
        B.phase_moe_sparse(1, x3, c3, [R["x3"], R["c3"]], w["moe_w_gate"], w["moe_w_up"], w["moe_w_down"],
                           w["ln_ffn_g"], w["ln_ffn_b"], y, R["y"], None, None, xsort[1], ysort, Rz[1],
                           wb=wbs[1], Rwb=Rwbs[1])
    else:
        B.phase_moe(1, x3, x3T, c3, [R["x3"], R["x3T"], R["c3"]], w["moe_w_gate"], w["moe_w_up"], w["moe_w_down"],
                    w["ln_ffn_g"], w["ln_ffn_b"], y, R["y"])
    fw = [R["y"]]
    if debug:
        fw = list(R.values())
    P.emit(final_wait=fw)
    B.es.close()
    return B


def kernel(**inputs):
    x = np.ascontiguousarray(np.asarray(inputs["x"], dtype=np.float32))
    nb, T, _ = x.shape
    B = build_full(T)
    ws = {k: np.ascontiguousarray(np.asarray(inputs[k], dtype=np.float32)) for k in W_SHAPES}
    in_maps = []
    for c in range(nb):
        m = {"x": x[c]}
        m.update(ws)
        in_maps.append(m)
    res = run_bass_kernel_spmd(B.nc, in_maps, core_ids=list(range(nb)))
    return np.stack([np.asarray(r["y"], dtype=np.float32) for r in res.results], axis=0)
```

```python
import numpy as np
from contextlib import ExitStack
import concourse.bass as bass
import concourse.mybir as mybir
from concourse.bass_utils import run_bass_kernel_spmd

F32 = mybir.dt.float32
BF16 = mybir.dt.bfloat16
U8 = mybir.dt.uint8
AF = mybir.ActivationFunctionType
ALU = mybir.AluOpType
AX = mybir.AxisListType

ENGS = ("pe", "act", "dve", "pool", "sp")

D = 1024
KC = 8
DEPTH = 2
ALPHA = (2.0 * DEPTH) ** 0.25
LN_EPS = 1e-5
RMS_EPS = 1e-5
NE = 16
DFF = 512
D_INNER = 2048
NH = 32
NG = 4
NST = 128
SSD_IN = 5152
AH = 16
AD = 64


class Res:
    __slots__ = ("name", "lw", "rd", "dcount", "dsem")

    def __init__(self, name):
        self.name = name
        self.lw = []
        self.rd = []
        self.dcount = 0
        self.dsem = None


class Ins:
    __slots__ = ("eng", "fn", "reads", "writes", "dma", "deps", "needed", "sem", "val", "clock", "pseudo", "key",
                 "phase")

    def __init__(self, eng, fn, reads, writes, dma, pseudo=False):
        self.eng = eng
        self.fn = fn
        self.reads = reads
        self.writes = writes
        self.dma = dma
        self.deps = []
        self.needed = False
        self.sem = None
        self.val = 0
        self.clock = None
        self.pseudo = pseudo


class Phys:
    __slots__ = ("count", "dsem")

    def __init__(self):
        self.count = 0
        self.dsem = None


class Prog:
    def __init__(self, nc):
        self.nc = nc
        self.ins = []
        self.res = []
        self.phase = 0

    def R(self, name):
        r = Res(name)
        self.res.append(r)
        return r

    def Rs(self, name, n):
        return [self.R("%s%d" % (name, i)) for i in range(n)]

    def op(self, eng, fn, reads=(), writes=()):
        i = Ins(eng, fn, tuple(reads), tuple(writes), False)
        i.phase = self.phase
        self.ins.append(i)
        return i

    def dma(self, eng, out, in_, reads=(), writes=(), key=None):
        assert len(writes) == 1
        i = Ins(eng, (lambda e, o=out, s=in_: e.dma_start(out=o, in_=s)), tuple(reads), tuple(writes), True)
        i.key = key if key is not None else writes[0]
        i.phase = self.phase
        self.ins.append(i)
        return i

    def store(self, eng, out, in_, src, dst, extra_reads=()):
        return self.dma(eng, out, in_, reads=[src] + list(extra_reads), writes=[dst], key=src)

    def mark(self):
        return len(self.ins)

    def interleave(self, start, mid):
        A = self.ins[start:mid]
        Bq = self.ins[mid:]
        if not A or not Bq:
            return
        keyed = [((i + 0.5) / len(A), 0, i, x) for i, x in enumerate(A)] + \
                [((j + 0.5) / len(Bq), 1, j, x) for j, x in enumerate(Bq)]
        keyed.sort(key=lambda z: (z[0], z[1], z[2]))
        self.ins[start:] = [z[3] for z in keyed]

    def barrier(self):
        allr = tuple(self.res)
        for e in ENGS:
            i = Ins(e, None, (), allr, False, pseudo=True)
            i.phase = self.phase
            self.ins.append(i)
        self.phase += 1

    def _analyse(self, final_wait):
        fin = Ins("sp", None, tuple(final_wait), (), False, pseudo=True)
        fin.phase = self.phase
        self.ins.append(fin)
        for i in self.ins:
            deps = []
            for r in i.reads:
                deps.extend(r.lw)
            par = {}
            for w in i.writes:
                p = bool(i.dma and w.lw and all(x.dma for x in w.lw) and not w.rd)
                par[id(w)] = p
                if not p:
                    deps.extend(w.lw)
                deps.extend(w.rd)
            seen = set()
            for d in deps:
                if d is i or id(d) in seen:
                    continue
                seen.add(id(d))
                if (not d.dma) and (not i.dma) and d.eng == "pe" and i.eng == "pe" and not i.pseudo:
                    continue
                i.deps.append(d)
                d.needed = True
            if i.pseudo:
                continue
            for r in i.reads:
                r.rd.append(i)
            for w in i.writes:
                if par[id(w)]:
                    w.lw = w.lw + [i]
                else:
                    w.lw = [i]
                w.rd = []
        cnt = {e: 0 for e in ENGS}
        dres = []
        free = []
        live = []
        cur_phase = 0
        for i in self.ins:
            if i.phase != cur_phase:
                free.extend(p for (_, p) in live)
                live = []
                cur_phase = i.phase
            if i.pseudo:
                continue
            if i.dma:
                w = i.key
                if w.dsem is None or w.dsem[0] != cur_phase:
                    if i.eng == "pool":
                        ph = Phys()
                        dres.append(ph)
                    else:
                        if free:
                            ph = free.pop()
                        else:
                            ph = Phys()
                            dres.append(ph)
                        live.append((cur_phase, ph))
                    w.dsem = (cur_phase, ph)
                ph = w.dsem[1]
                ph.count += 16
                i.sem = ph
                i.val = ph.count
            elif i.needed:
                cnt[i.eng] += 1
                i.sem = i.eng
                i.val = cnt[i.eng]
        seen = {e: {} for e in ENGS}
        per_eng = {e: [] for e in ENGS}
        nwaits = 0
        for i in self.ins:
            s = seen[i.eng]
            waits = {}
            for d in i.deps:
                kid = id(d.sem) if d.dma else d.sem
                if s.get(kid, 0) >= d.val:
                    continue
                if kid not in waits or waits[kid][1] < d.val:
                    waits[kid] = (d.sem, d.val)
            for d in i.deps:
                if d.clock:
                    for k, v in d.clock.items():
                        if s.get(k, 0) < v:
                            s[k] = v
            for kid, (key, v) in waits.items():
                if s.get(kid, 0) < v:
                    s[kid] = v
            wl = list(waits.values())
            nwaits += len(wl)
            if i.sem is not None:
                c = dict(s)
                c[id(i.sem) if i.dma else i.sem] = i.val
                i.clock = c
            per_eng[i.eng].append((i, wl))
        self.stats = dict(n_ins=len(self.ins), n_waits=nwaits, n_dma_sems=len(dres),
                          per_eng={e: len(v) for e, v in per_eng.items()})
        return per_eng, dres

    def emit(self, final_wait=()):
        nc = self.nc
        per_eng, dres = self._analyse(final_wait)
        with ExitStack() as es:
            esem = {e: es.enter_context(nc.semaphore("S_" + e)) for e in ENGS}
            for k, r in enumerate(dres):
                r.dsem = es.enter_context(nc.semaphore("D%d" % k))
            block = es.enter_context(nc.Block())

            def run(e, name):
                for (i, wl) in per_eng[name]:
                    for (key, v) in wl:
                        sem = esem[key] if isinstance(key, str) else key.dsem
                        e.wait_ge(sem, v)
                    if i.fn is None:
                        continue
                    bi = i.fn(e)
                    if i.dma:
                        bi.then_inc(i.sem.dsem, 16)
                    elif i.needed:
                        bi.then_inc(esem[name], 1)

            @block.tensor
            def _(e):
                run(e, "pe")

            @block.scalar
            def _(e):
                run(e, "act")

            @block.vector
            def _(e):
                run(e, "dve")

            @block.gpsimd
            def _(e):
                run(e, "pool")

            @block.sync
            def _(e):
                run(e, "sp")
        return nc


ARENA_BYTES = 210944


def _sz(dt):
    return 4 if dt in (F32, mybir.dt.int32, mybir.dt.uint32) else (2 if dt == BF16 else 1)


class Builder:
    def __init__(self, T=4096):
        self.T = T
        self.NT = T // 128
        self.nc = bass.Bass("TRN2", target_bir_lowering=False)
        self.P = Prog(self.nc)
        self.es = ExitStack()
        nc = self.nc
        self.arena = self.es.enter_context(nc.sbuf_tensor("arena", [128, ARENA_BYTES], U8))
        self.off = 0
        self.pairs = [self.es.enter_context(nc.psum_tensor("pair%d" % i, [128, 1024], F32)) for i in range(4)]
        self.Rbank = self.P.Rs("bank", 8)
        self.dram = {}
        self._rr = 0

    def sb(self, shape, dt):
        n = int(np.prod(shape[1:])) * _sz(dt)
        n_al = (n + 63) // 64 * 64
        off = self.off
        assert off + n_al <= ARENA_BYTES, ("SBUF arena overflow", off, n_al)
        self.off += n_al
        v = self.arena[0:shape[0], off:off + n].bitcast(dt)
        if len(shape) == 3:
            v = v.rearrange("p (a b) -> p a b", a=shape[1])
        elif len(shape) == 4:
            v = v.rearrange("p (a b c) -> p a b c", a=shape[1], b=shape[2])
        elif len(shape) == 5:
            v = v.rearrange("p (a b c d) -> p a b c d", a=shape[1], b=shape[2], c=shape[3])
        return v

    def bank(self, i, dt=F32):
        b = self.pairs[i // 2][:, (i % 2) * 512:(i % 2 + 1) * 512]
        if dt != F32:
            b = b.bitcast(dt)
        return b

    def pair(self, i):
        return self.pairs[i][:]

    def dt_in(self, name, shape, dt=F32):
        t = self.nc.dram_tensor(name, list(shape), dt, kind="ExternalInput").ap()
        self.dram[name] = t
        return t

    def dt_out(self, name, shape, dt=F32):
        t = self.nc.dram_tensor(name, list(shape), dt, kind="ExternalOutput").ap()
        self.dram[name] = t
        return t

    def dt_scr(self, name, shape, dt=F32, debug=False):
        t = self.nc.dram_tensor(name, list(shape), dt, kind=("ExternalOutput" if debug else "Internal")).ap()
        self.dram[name] = t
        return t

    def dq(self):
        self._rr += 1
        return ("sp", "act")[self._rr % 2]

    def consts(self):
        P = self.P
        self.identF = self.sb([128, 128], F32)
        self.identB = self.sb([128, 128], BF16)
        self.maskU = self.sb([128, 128], F32)
        self.maskSL = self.sb([128, 128], F32)
        self.onesF = self.sb([128, 128], F32)
        self.onesB = self.sb([128, 128], BF16)
        self.Rc = P.R("consts")
        Rc = self.Rc
        iF, iB, mU, mS, oF, oB = self.identF, self.identB, self.maskU, self.maskSL, self.onesF, self.onesB
        P.op("pool", lambda e: e.memset(iF, 0.0), writes=[Rc])
        P.op("pool", lambda e: e.affine_select(out=iF, in_=iF, pattern=[[-1, 128]], compare_op=ALU.not_equal,
                                               fill=1.0, base=0, channel_multiplier=1), reads=[Rc], writes=[Rc])
        P.op("pool", lambda e: e.tensor_copy(out=iB, in_=iF), reads=[Rc], writes=[Rc])
        P.op("pool", lambda e: e.memset(oF, 1.0), reads=[Rc], writes=[Rc])
        P.op("pool", lambda e: e.memset(oB, 1.0), reads=[Rc], writes=[Rc])
        P.op("pool", lambda e: e.affine_select(out=mU, in_=oF, pattern=[[1, 128]], compare_op=ALU.is_ge,
                                               fill=0.0, base=0, channel_multiplier=-1), reads=[Rc], writes=[Rc])
        P.op("pool", lambda e: e.affine_select(out=mS, in_=oF, pattern=[[-1, 128]], compare_op=ALU.is_gt,
                                               fill=0.0, base=0, channel_multiplier=1), reads=[Rc], writes=[Rc])

    def wconv_steps(self, layer, wg_d, wu_d, wd_d, wb, Rwb, ex_range=None, nbuf=2):
        P = self.P
        bnc = [self.sb([128, 4096], BF16) for _ in range(nbuf)]
        Rbn = P.Rs("bnc", nbuf)
        RbnS = P.Rs("bncS", nbuf)
        n = 0
        pend = [None]

        def flush():
            if pend[0] is not None:
                b_, dst_ = pend[0]
                P.dma("pool", dst_, bnc[b_], reads=[Rbn[b_]], writes=[Rwb], key=RbnS[b_])
                pend[0] = None

        for ex in (ex_range if ex_range is not None else range(NE)):
            for which in range(3):
                b = n % nbuf
                n += 1
                if which < 2:
                    src = (wg_d, wu_d)[which][layer, ex].rearrange("(p k) f -> p (k f)", k=KC)
                    dst = wb[which][ex * 128:(ex + 1) * 128, :]
                else:
                    src = wd_d[layer, ex].rearrange("(p k) n -> p (k n)", k=4)
                    dst = wb[2][ex * DFF:(ex + 1) * DFF, :].rearrange("(p k) n -> p (k n)", k=4)

                def step(b=b, src=src, dst=dst):
                    P.dma("pool", bnc[b], src, writes=[Rbn[b]])
                    flush()
                    pend[0] = (b, dst)
                yield step
        yield flush

    def _bc_reg(self, e):
        if getattr(self, "_bcr", None) is None:
            self._bcr = e.to_reg(NE * 128 - 1)
        return self._bcr

    def _bc_reg2(self, e):
        if getattr(self, "_bcr2", None) is None:
            self._bcr2 = e.to_reg(NE * DFF - 1)
        return self._bcr2

    def zero_fill(self, dram2d, Rz):
        P = self.P
        if getattr(self, "zt", None) is None:
            self.zt = self.sb([128, 4, D], BF16)
            self.Rzt = P.R("zt")
            P.op("pool", lambda e, zt=self.zt: e.memset(zt, 0.0), writes=[self.Rzt])
        n = dram2d.shape[0] // 512
        for i in range(n):
            P.dma(self.dq(), dram2d[i * 512:(i + 1) * 512, :].rearrange("(a p) d -> p a d", p=128), self.zt,
                  reads=[self.Rzt], writes=[Rz], key=self.Rzt)

    def bcast_load(self, dram_row, n, eng="sp"):
        t = self.sb([128, n], F32)
        r = self.P.R("bc")
        self.P.dma(eng, t, dram_row.partition_broadcast(128), writes=[r])
        return t, r

    def layer_norm(self, xt, Rx, st, mv, Rs, g_b, b_b, Rgb, eng2="pool"):
        P = self.P
        for c in range(2):
            P.op("dve", lambda e, c=c: e.bn_stats(out=st[:, c, :], in_=xt[:, c * 512:(c + 1) * 512]),
                 reads=[Rx], writes=[Rs])
        P.op("dve", lambda e: e.bn_aggr(out=mv[:, 0:2], in_=st), reads=[Rs], writes=[Rs])
        P.op("dve", lambda e: e.tensor_scalar(out=mv[:, 2:3], in0=mv[:, 1:2], scalar1=LN_EPS, scalar2=None,
                                              op0=ALU.add), reads=[Rs], writes=[Rs])
        P.op("act", lambda e: e.activation(out=mv[:, 3:4], in_=mv[:, 2:3], func=AF.Sqrt), reads=[Rs], writes=[Rs])
        P.op("dve", lambda e: e.reciprocal(out=mv[:, 4:5], in_=mv[:, 3:4]), reads=[Rs], writes=[Rs])
        P.op("dve", lambda e: e.scalar_tensor_tensor(out=mv[:, 5:6], in0=mv[:, 0:1], scalar=-1.0, in1=mv[:, 4:5],
                                                     op0=ALU.mult, op1=ALU.mult), reads=[Rs], writes=[Rs])
        P.op("act", lambda e: e.activation(out=xt, in_=xt, func=AF.Identity, bias=mv[:, 5:6], scale=mv[:, 4:5]),
             reads=[Rs, Rx], writes=[Rx])
        P.op("dve", lambda e: e.tensor_tensor(out=xt, in0=xt, in1=g_b, op=ALU.mult), reads=[Rx, Rgb], writes=[Rx])
        P.op(eng2, lambda e: e.tensor_tensor(out=xt, in0=xt, in1=b_b, op=ALU.add), reads=[Rx, Rgb], writes=[Rx])

    def make_xT(self, xt, Rx, bk, xTb, RxT, xTf=None, RxTf=None):
        P = self.P
        iF = self.identF
        for h in range(2):
            pb = self.bank(bk + h)
            Rb = self.Rbank[bk + h]
            for k in range(4):
                kk = h * 4 + k
                P.op("pe", lambda e, pb=pb, k=k, kk=kk: e.transpose(pb[:, k * 128:(k + 1) * 128],
                                                                    xt[:, kk * 128:(kk + 1) * 128], iF),
                     reads=[Rx, self.Rc], writes=[Rb])
            P.op("act", lambda e, pb=pb, h=h: e.copy(out=xTb[:, h * 4:(h + 1) * 4, :],
                                                     in_=pb.rearrange("p (a b) -> p a b", a=4)),
                 reads=[Rb], writes=[RxT, Rb])
            if xTf is not None:
                P.op("dve", lambda e, pb=pb, h=h: e.tensor_copy(out=xTf[:, h * 4:(h + 1) * 4, :],
                                                                in_=pb.rearrange("p (a b) -> p a b", a=4)),
                     reads=[Rb], writes=[RxTf, Rb])

    def router_setup(self, router_w, router_bias):
        P = self.P
        self.rw = self.sb([128, KC, NE], F32)
        self.Rrw = P.R("rw")
        P.dma("sp", self.rw, router_w.rearrange("(k p) e -> p k e", p=128), writes=[self.Rrw])
        self.rb_b, self.Rrb = self.bcast_load(router_bias.rearrange("(o e) -> o e", o=1), NE)
        self.rt = self.sb([128, 160], F32)
        self.Rrt = P.R("rt")

    def router(self, xTf, RxTf, bk, comb, Rcomb):
        P = self.P
        pb = self.bank(bk)
        Rb = self.Rbank[bk]
        rw = self.rw
        for k in range(KC):
            P.op("pe", lambda e, k=k: e.matmul(pb[:, 0:NE], xTf[:, k, :], rw[:, k, :], start=(k == 0),
                                                stop=(k == KC - 1)), reads=[RxTf, self.Rrw], writes=[Rb])
        P.op("dve", lambda e: e.tensor_copy(out=comb, in_=pb[:, 0:NE]), reads=[Rb], writes=[Rcomb, Rb])

    def route_batched(self, lg, sc, w1, w2, w3, sm4, d):
        P = self.P
        NT = self.NT
        f = lambda a: a.rearrange("p t e -> p (t e)")
        v4 = lambda a: a.rearrange("p t (g j) -> p t g j", g=4)
        be = lambda a: a.unsqueeze(2).to_broadcast([128, NT, NE])
        mx, ssum, rs, gmx, gsum = sm4[:, 0, :], sm4[:, 1, :], sm4[:, 2, :], sm4[:, 3, :], sm4[:, 4, :]
        m1 = w3[:, :, 0:4]
        m2 = w3[:, :, 4:8]
        gs = w3[:, :, 8:12]
        gm = w3[:, :, 12:16]
        b4 = lambda a: a.unsqueeze(3).to_broadcast([128, NT, 4, 4])
        d(lambda e: e.tensor_reduce(out=mx, in_=lg, axis=AX.X, op=ALU.max))
        d(lambda e: e.tensor_tensor(out=sc, in0=lg, in1=be(mx), op=ALU.subtract))
        P.op("act", lambda e: e.activation(out=f(sc), in_=f(sc), func=AF.Exp), reads=[self._Rroute],
             writes=[self._Rroute])
        d(lambda e: e.tensor_reduce(out=ssum, in_=sc, axis=AX.X, op=ALU.add))
        d(lambda e: e.reciprocal(out=rs, in_=ssum))
        d(lambda e: e.tensor_tensor(out=sc, in0=sc, in1=be(rs), op=ALU.mult))
        d(lambda e: e.tensor_tensor(out=w1, in0=sc, in1=self.rb_b.unsqueeze(1).to_broadcast([128, NT, NE]),
                                    op=ALU.add), rd=[self.Rrb])
        d(lambda e: e.tensor_reduce(out=m1, in_=v4(w1), axis=AX.X, op=ALU.max))
        d(lambda e: e.tensor_tensor(out=v4(w2), in0=v4(w1), in1=b4(m1), op=ALU.is_equal))
        d(lambda e: e.scalar_tensor_tensor(out=f(w1), in0=f(w2), scalar=-1e9, in1=f(w1), op0=ALU.mult,
                                           op1=ALU.add))
        d(lambda e: e.tensor_reduce(out=m2, in_=v4(w1), axis=AX.X, op=ALU.max))
        d(lambda e: e.tensor_tensor(out=v4(lg), in0=v4(w1), in1=b4(m2), op=ALU.is_equal))
        d(lambda e: e.tensor_tensor(out=gs, in0=m1, in1=m2, op=ALU.add))
        d(lambda e: e.tensor_reduce(out=gmx, in_=gs, axis=AX.X, op=ALU.max))
        d(lambda e: e.tensor_tensor(out=gm, in0=gs, in1=gmx.unsqueeze(2).to_broadcast([128, NT, 4]),
                                    op=ALU.is_equal))
        d(lambda e: e.tensor_tensor(out=w2, in0=w2, in1=lg, op=ALU.add))
        d(lambda e: e.tensor_tensor(out=v4(w2), in0=v4(w2), in1=b4(gm), op=ALU.mult))
        d(lambda e: e.tensor_tensor(out=w2, in0=w2, in1=sc, op=ALU.mult))
        d(lambda e: e.tensor_reduce(out=gsum, in_=w2, axis=AX.X, op=ALU.add))
        d(lambda e: e.reciprocal(out=rs, in_=gsum))
        d(lambda e: e.tensor_tensor(out=lg, in0=w2, in1=be(rs), op=ALU.mult))

    def phase_pre(self, x_d, xT_d, comb_d, RxT_d, Rcomb_d):
        P = self.P
        mark = self.off
        xt = [self.sb([128, D], F32) for _ in range(2)]
        Rx = P.Rs("pre_x", 2)
        xTb = [self.sb([128, KC, 128], BF16) for _ in range(2)]
        RxTb = P.Rs("pre_xTb", 2)
        xTf = [self.sb([128, KC, 128], F32) for _ in range(2)]
        RxTf = P.Rs("pre_xTf", 2)
        cb = [self.sb([128, NE], F32) for _ in range(2)]
        Rcb = P.Rs("pre_cb", 2)
        for t in range(self.NT):
            b = t % 2
            P.dma("sp", xt[b], x_d[t * 128:(t + 1) * 128, :], writes=[Rx[b]])
            self.make_xT(xt[b], Rx[b], 0 + 2 * b, xTb[b], RxTb[b], xTf[b], RxTf[b])
            self.router(xTf[b], RxTf[b], 4 + b, cb[b], Rcb[b])
            P.store("act", xT_d[t], xTb[b], RxTb[b], RxT_d)
            P.store("act", comb_d[t], cb[b], Rcb[b], Rcomb_d)
        P.barrier()
        self.off = mark

    def phase_moe(self, layer, x_d, xT_d, comb_d, Rin, wg_d, wu_d, wd_d, lng_d, lnb_d,
                  out_d, Rout, outT_d=None, RoutT=None):
        P = self.P
        T, NT = self.T, self.NT
        mark = self.off
        HT = min(16, NT)
        NHALF = NT // HT
        NBLK = HT // 4
        acc = self.sb([128, HT, D], F32)
        Racc = P.Rs("acc", HT)
        xT = self.sb([128, HT, KC, 128], BF16)
        RxT = P.R("moe_xT")
        comb = self.sb([128, HT, NE], F32)
        Rcomb = P.R("moe_comb")
        wg = [self.sb([128, KC, DFF], BF16) for _ in range(2)]
        wu = [self.sb([128, KC, DFF], BF16) for _ in range(2)]
        wd = [self.sb([128, 4, D], BF16) for _ in range(2)]
        Rwg = P.Rs("wg", 2)
        Rwu = P.Rs("wu", 2)
        Rwd = P.Rs("wd", 2)
        sg = [self.sb([128, 512], F32) for _ in range(2)]
        Rsg = P.Rs("sg", 2)
        hT = [self.sb([128, 4, 512], BF16) for _ in range(2)]
        RhT = P.Rs("hT", 2)
        g_b, Rg = self.bcast_load(lng_d[layer:layer + 1, :], D, "sp")
        b_b, Rb_ = self.bcast_load(lnb_d[layer:layer + 1, :], D, "act")
        st = self.sb([128, 2, 6], F32)
        mv = self.sb([128, 8], F32)
        Rs = P.R("moe_ln")
        xTo = [self.sb([128, KC, 128], BF16) for _ in range(2)]
        RxTo = P.Rs("moe_xTo", 2)
        Rgb = P.R("moe_gb")
        P.op("pool", lambda e: e.tensor_copy(out=mv[:, 7:8], in_=g_b[:, 0:1]), reads=[Rg, Rb_], writes=[Rgb])

        it = 0
        for half in range(NHALF):
            t0 = half * HT
            for j in range(HT):
                P.dma(self.dq(), acc[:, j, :], x_d[(t0 + j) * 128:(t0 + j + 1) * 128, :], reads=Rin,
                      writes=[Racc[j]])
                P.op("act", lambda e, j=j: e.mul(out=acc[:, j, :], in_=acc[:, j, :], mul=ALPHA),
                     reads=[Racc[j]], writes=[Racc[j]])
            P.dma("sp", xT, xT_d[t0:t0 + HT].rearrange("t p k c -> p t k c"), reads=Rin, writes=[RxT])
            P.dma("act", comb, comb_d[t0:t0 + HT].rearrange("t p e -> p t e"), reads=Rin, writes=[Rcomb])
            for ex in range(NE):
                wb = (half * NE + ex) % 2
                P.dma("pool", wg[wb], wg_d[layer, ex].rearrange("(k p) f -> p k f", p=128), writes=[Rwg[wb]])
                P.dma("pool", wu[wb], wu_d[layer, ex].rearrange("(k p) f -> p k f", p=128), writes=[Rwu[wb]])
                P.dma("pool", wd[wb], wd_d[layer, ex].rearrange("(k p) f -> p k f", p=128), writes=[Rwd[wb]])
                for blk in range(NBLK):
                    hb = it % 2
                    it += 1
                    rhs_x = lambda k, blk=blk: xT[:, blk * 4:(blk + 1) * 4, k, :]
                    for f in range(4):
                        pg = self.bank(f % 2)
                        Rpg = self.Rbank[f % 2]
                        pu = self.bank(2 + f % 2)
                        Rpu = self.Rbank[2 + f % 2]
                        sgb = f % 2
                        for k in range(KC):
                            P.op("pe", lambda e, pg=pg, k=k, f=f, wb=wb, rx=rhs_x: e.matmul(
                                pg, wg[wb][:, k, f * 128:(f + 1) * 128], rx(k), start=(k == 0), stop=(k == KC - 1)),
                                reads=[Rwg[wb], RxT], writes=[Rpg])
                        for k in range(KC):
                            P.op("pe", lambda e, pu=pu, k=k, f=f, wb=wb, rx=rhs_x: e.matmul(
                                pu, wu[wb][:, k, f * 128:(f + 1) * 128], rx(k), start=(k == 0), stop=(k == KC - 1)),
                                reads=[Rwu[wb], RxT], writes=[Rpu])
                        P.op("act", lambda e, pg=pg, sgb=sgb: e.activation(out=sg[sgb], in_=pg, func=AF.Silu),
                             reads=[Rpg], writes=[Rsg[sgb]])
                        P.op("dve", lambda e, pu=pu, sgb=sgb, hb=hb, f=f: e.tensor_tensor(
                            out=hT[hb][:, f, :], in0=pu, in1=sg[sgb], op=ALU.mult),
                            reads=[Rpu, Rsg[sgb]], writes=[RhT[hb]])
                    for s in range(4):
                        j = blk * 4 + s
                        for n in range(2):
                            bi = 4 + (s % 2) * 2 + n
                            po = self.bank(bi)
                            Rpo = self.Rbank[bi]
                            for f in range(4):
                                P.op("pe", lambda e, po=po, f=f, s=s, n=n, hb=hb, wb=wb: e.matmul(
                                    po, hT[hb][:, f, s * 128:(s + 1) * 128], wd[wb][:, f, n * 512:(n + 1) * 512],
                                    start=(f == 0), stop=(f == 3)), reads=[RhT[hb], Rwd[wb]], writes=[Rpo])
                            P.op("dve", lambda e, po=po, j=j, n=n, ex=ex: e.scalar_tensor_tensor(
                                out=acc[:, j, n * 512:(n + 1) * 512], in0=po, scalar=comb[:, j, ex:ex + 1],
                                in1=acc[:, j, n * 512:(n + 1) * 512], op0=ALU.mult, op1=ALU.add),
                                reads=[Rpo, Rcomb, Racc[j]], writes=[Racc[j]])
            for j in range(HT):
                t = t0 + j
                self.layer_norm(acc[:, j, :], Racc[j], st, mv, Rs, g_b, b_b, Rgb)
                P.store(self.dq(), out_d[t * 128:(t + 1) * 128, :], acc[:, j, :], Racc[j], Rout)
                if outT_d is not None:
                    b = j % 2
                    self.make_xT(acc[:, j, :], Racc[j], 0 + 2 * b, xTo[b], RxTo[b])
                    P.store(self.dq(), outT_d[t], xTo[b], RxTo[b], RoutT)
        P.barrier()
        self.off = mark


    def phase_a0z(self, x_d, win_d, dtb_d, alog_d, xT_d, RxT_d, sz_d, Rsz_d, dt_d, Rdt_d, early=None, wconv=None):
        P = self.P
        NT = self.NT
        mark = self.off
        wsteps = wconv() if wconv is not None else None
        winz = self.sb([128, KC, 2048], BF16)
        Rwz = P.Rs("winz", 4)
        for n in range(4):
            P.dma("pool", winz[:, :, n * 512:(n + 1) * 512],
                  win_d[0, :, n * 512:(n + 1) * 512].rearrange("(k p) n -> p k n", p=128), writes=[Rwz[n]])
        windt = self.sb([128, KC, 32], F32)
        Rwdt = P.R("windt")
        P.dma("sp", windt, win_d[0, :, 5120:5152].rearrange("(k p) n -> p k n", p=128), writes=[Rwdt])
        dtb_b, Rdtb = self.bcast_load(dtb_d[0:1, :], 32)
        vall = self.sb([128, NT, 32], F32)
        av = self.sb([128, NT, 32], F32)
        Rv = P.R("vall")
        xt = [self.sb([128, D], F32) for _ in range(2)]
        Rx = P.Rs("a0_x", 2)
        xTb = [self.sb([128, KC, 128], BF16) for _ in range(2)]
        RxTb = P.Rs("a0_xTb", 2)
        xTf = [self.sb([128, KC, 128], F32) for _ in range(2)]
        RxTf = P.Rs("a0_xTf", 2)
        sz = [self.sb([128, 2048], BF16) for _ in range(2)]
        Rsz = P.Rs("a0_sz", 2)
        for t in range(NT):
            b = t % 2
            if early is not None and t == min(3, NT - 1):
                early()
            if wsteps is not None and t >= 2:
                for _ in range(2 if t % 2 == 0 else 1):
                    st_ = next(wsteps, None)
                    if st_ is not None:
                        st_()
            P.dma("sp", xt[b], x_d[t * 128:(t + 1) * 128, :], writes=[Rx[b]])
            self.make_xT(xt[b], Rx[b], 2 * b, xTb[b], RxTb[b], xTf[b], RxTf[b])
            P.store("act", xT_d[t], xTb[b], RxTb[b], RxT_d)
            for n in range(4):
                bi = 4 + n % 2
                pz = self.bank(bi)
                Rpz = self.Rbank[bi]
                for k in range(KC):
                    P.op("pe", lambda e, pz=pz, k=k, n=n, b=b: e.matmul(
                        pz, xTb[b][:, k, :], winz[:, k, n * 512:(n + 1) * 512], start=(k == 0), stop=(k == KC - 1)),
                        reads=[RxTb[b], Rwz[n]], writes=[Rpz])
                P.op("act", lambda e, pz=pz, n=n, b=b: e.activation(out=sz[b][:, n * 512:(n + 1) * 512], in_=pz,
                                                                     func=AF.Silu),
                     reads=[Rpz], writes=[Rsz[b], Rpz])
            P.store(self.dq(), sz_d[t], sz[b], Rsz[b], Rsz_d)
            pdt = self.bank(6 + b)
            Rpdt = self.Rbank[6 + b]
            for k in range(KC):
                P.op("pe", lambda e, pdt=pdt, k=k, b=b: e.matmul(pdt[:, 0:32], xTf[b][:, k, :], windt[:, k, :],
                                                                  start=(k == 0), stop=(k == KC - 1)),
                     reads=[RxTf[b], Rwdt], writes=[Rpdt])
            P.op("dve", lambda e, pdt=pdt, t=t: e.tensor_tensor(out=vall[:, t, :], in0=pdt[:, 0:32], in1=dtb_b,
                                                                 op=ALU.add),
                 reads=[Rpdt, Rdtb], writes=[Rv, Rpdt])
        if wsteps is not None:
            for st_ in wsteps:
                st_()
        P.op("dve", lambda e: e.scalar_tensor_tensor(out=av, in0=vall, scalar=-1.0, in1=vall, op0=ALU.mult,
                                                     op1=ALU.max), reads=[Rv], writes=[Rv])
        P.op("act", lambda e: e.activation(out=av, in_=av, func=AF.Exp, scale=-1.0), reads=[Rv], writes=[Rv])
        P.op("act", lambda e: e.activation(out=av, in_=av, func=AF.Ln, bias=1.0), reads=[Rv], writes=[Rv])
        P.op("dve", lambda e: e.scalar_tensor_tensor(out=vall, in0=vall, scalar=0.0, in1=av, op0=ALU.max,
                                                     op1=ALU.add), reads=[Rv], writes=[Rv])
        a_b, Ra = self.bcast_load(alog_d[0:1, :], NH, "act")
        P.op("act", lambda e: e.activation(out=a_b, in_=a_b, func=AF.Exp), reads=[Ra], writes=[Ra])
        P.op("dve", lambda e: e.tensor_scalar(out=a_b, in0=a_b, scalar1=-1.0, scalar2=None, op0=ALU.mult),
             reads=[Ra], writes=[Ra])
        dtx = self.sb([128, NT, 5, NH], F32)
        cumt = self.sb([128, 2, NT, NH], F32)
        Rdx = P.R("dtx")
        NTH = NT * NH
        P.op("dve", lambda e: e.tensor_copy(out=dtx[:, :, 0, :], in_=vall), reads=[Rv], writes=[Rdx])
        P.op("dve", lambda e: e.tensor_tensor(out=av, in0=vall, in1=a_b.unsqueeze(1).to_broadcast([128, NT, NH]),
                                              op=ALU.mult), reads=[Rv, Ra], writes=[Rv])
        P.op("dve", lambda e: e.tensor_copy(out=dtx[:, :, 1, :], in_=av), reads=[Rv], writes=[Rdx])
        avf = av.rearrange("p t h -> p (t h)")
        for which, lhs in enumerate((self.maskU, self.onesF)):
            for c0 in range(0, NTH, 512):
                c1 = min(NTH, c0 + 512)
                bi = 4 + which
                pb = self.bank(bi)
                P.op("pe", lambda e, pb=pb, lhs=lhs, c0=c0, c1=c1: e.matmul(pb[:, 0:c1 - c0], lhs, avf[:, c0:c1],
                                                                           start=True, stop=True),
                     reads=[Rv, self.Rc], writes=[self.Rbank[bi]])
                P.op("dve", lambda e, pb=pb, which=which, c0=c0, c1=c1: e.tensor_copy(
                    out=cumt[:, which].rearrange("p t h -> p (t h)")[:, c0:c1], in_=pb[:, 0:c1 - c0]),
                    reads=[self.Rbank[bi]], writes=[Rdx, self.Rbank[bi]])
        P.op("act", lambda e: e.activation(out=dtx[:, :, 2, :], in_=cumt[:, 0], func=AF.Exp), reads=[Rdx],
             writes=[Rdx])
        P.op("act", lambda e: e.activation(out=dtx[:, :, 3, :], in_=cumt[:, 1], func=AF.Exp), reads=[Rdx],
             writes=[Rdx])
        P.op("dve", lambda e: e.tensor_tensor(out=cumt[:, 1], in0=cumt[:, 1], in1=cumt[:, 0], op=ALU.subtract),
             reads=[Rdx], writes=[Rdx])
        P.op("act", lambda e: e.activation(out=dtx[:, :, 4, :], in_=cumt[:, 1], func=AF.Exp), reads=[Rdx],
             writes=[Rdx])
        P.store("sp", dt_d, dtx, Rdx, Rdt_d)
        P.barrier()
        self.off = mark

    def phase_a0x(self, win_d, convw_d, convb_d, xT_d, RxT_d, xs_d, Rxs_d, bt_d, Rbt_d, bct_d, Rbct_d, zfill=(),
                  wconv=None):
        P = self.P
        self.zt = None
        wsteps = wconv() if wconv is not None else None
        NT = self.NT
        NB = NT // 4
        mark = self.off
        NCH = 24
        winx = self.sb([128, KC, 3072], BF16)
        Rwx = P.Rs("winx", 3)
        for j in range(6):
            P.dma("pool", winx[:, :, j * 512:(j + 1) * 512],
                  win_d[0, :, 2048 + j * 512:2048 + (j + 1) * 512].rearrange("(k p) n -> p k n", p=128),
                  writes=[Rwx[j // 2]])
        cw4 = self.sb([4, 3072], F32)
        cbrow = self.sb([1, 3072], F32)
        cbrowB = self.sb([1, 3072], BF16)
        cwT = self.sb([128, NCH, 4], F32)
        Rcw = P.R("convw")
        P.dma("sp", cw4, convw_d[0], writes=[Rcw])
        Rcb = P.R("convb")
        P.dma("act", cbrow, convb_d[0:1, :], writes=[Rcb])
        P.op("dve", lambda e: e.tensor_copy(out=cbrowB, in_=cbrow), reads=[Rcb], writes=[Rcb])
        pb0 = self.bank(0)
        for c in range(NCH):
            P.op("pe", lambda e, c=c: e.matmul(pb0[:, c * 4:(c + 1) * 4], cw4[0:4, c * 128:(c + 1) * 128],
                                                self.identF[0:4, 0:4], start=True, stop=True),
                 reads=[Rcw, self.Rc], writes=[self.Rbank[0]])
        P.op("dve", lambda e: e.tensor_copy(out=cwT, in_=pb0[:, 0:NCH * 4].rearrange("p (c k) -> p c k", k=4)),
             reads=[self.Rbank[0]], writes=[Rcw, self.Rbank[0]])
        diagW = self.sb([128, NCH, 4, 128], BF16)
        Rdg = P.R("diagW")
        for c in range(NCH):
            P.op("dve", lambda e, c=c: e.tensor_tensor(
                out=diagW[:, c, :, :], in0=self.identF.unsqueeze(1).to_broadcast([128, 4, 128]),
                in1=cwT[:, c, :].unsqueeze(2).to_broadcast([128, 4, 128]), op=ALU.mult),
                reads=[Rcw, self.Rc], writes=[Rdg])
        xT = [self.sb([128, 4, KC, 128], BF16) for _ in range(2)]
        RxTl = P.Rs("a0x_xT", 2)
        uT = [self.sb([128, NCH, 515], BF16) for _ in range(2)]
        RuT = P.Rs("a0x_uT", 2)
        xs_tok = [self.sb([128, 2048], BF16) for _ in range(2)]
        Rxs = P.Rs("a0x_xs", 2)
        b_tok = [self.sb([128, 512], BF16) for _ in range(2)]
        Rbt = P.Rs("a0x_bt", 2)
        bct = [self.sb([128, 8, 128], BF16) for _ in range(2)]
        Rbct = P.Rs("a0x_bct", 2)
        oB = self.onesB
        for nb in range(NB):
            ub = nb % 2
            P.dma("sp", xT[ub], xT_d[nb * 4:(nb + 1) * 4].rearrange("t p k c -> p t k c"), reads=[RxT_d],
                  writes=[RxTl[ub]])
            for zi, z in enumerate(zfill):
                if min(zi, NB - 1) == nb:
                    self.zero_fill(*z)
            if nb == 0:
                P.op("pool", lambda e, ub=ub: e.memset(uT[ub][:, :, 0:3], 0.0), writes=[RuT[ub]])
            else:
                P.op("pool", lambda e, ub=ub: e.tensor_copy(out=uT[ub][:, :, 0:3], in_=uT[1 - ub][:, :, 512:515]),
                     reads=[RuT[1 - ub]], writes=[RuT[ub]])
            for c in range(NCH):
                bi = c % 2
                pu = self.bank(bi)
                Rpu = self.Rbank[bi]
                for k in range(KC):
                    P.op("pe", lambda e, pu=pu, k=k, c=c, ub=ub: e.matmul(
                        pu, winx[:, k, c * 128:(c + 1) * 128], xT[ub][:, :, k, :], start=(k == 0),
                        stop=(k == KC - 1)), reads=[Rwx[c // 8], RxTl[ub]], writes=[Rpu])
                if c % 2 == 0:
                    P.op("act", lambda e, pu=pu, c=c, ub=ub: e.copy(out=uT[ub][:, c, 3:515], in_=pu),
                         reads=[Rpu], writes=[RuT[ub], Rpu])
                else:
                    P.op("dve", lambda e, pu=pu, c=c, ub=ub: e.tensor_copy(out=uT[ub][:, c, 3:515], in_=pu),
                         reads=[Rpu], writes=[RuT[ub], Rpu])
            for s in range(4):
                t = nb * 4 + s
                ob = t % 2
                if wsteps is not None:
                    st_ = next(wsteps, None)
                    if st_ is not None:
                        st_()
                for q in range(5):
                    bi = 2 + q % 2
                    pc = self.bank(bi)
                    Rpc = self.Rbank[bi]
                    for cc in range(4):
                        c = q * 4 + cc
                        for k in range(4):
                            P.op("pe", lambda e, pc=pc, cc=cc, c=c, k=k, s=s, ub=ub: e.matmul(
                                pc[:, cc * 128:(cc + 1) * 128], uT[ub][:, c, s * 128 + k:s * 128 + k + 128],
                                diagW[:, c, k, :], start=(k == 0), stop=False),
                                reads=[RuT[ub], Rdg], writes=[Rpc])
                        P.op("pe", lambda e, pc=pc, cc=cc, c=c: e.matmul(
                            pc[:, cc * 128:(cc + 1) * 128], oB[0:1, 0:128], cbrowB[0:1, c * 128:(c + 1) * 128],
                            start=False, stop=True), reads=[Rcb, self.Rc], writes=[Rpc])
                    if q < 4:
                        P.op("act", lambda e, pc=pc, q=q, ob=ob: e.activation(
                            out=xs_tok[ob][:, q * 512:(q + 1) * 512], in_=pc, func=AF.Silu),
                            reads=[Rpc], writes=[Rxs[ob], Rpc])
                    else:
                        P.op("act", lambda e, pc=pc, ob=ob: e.activation(out=b_tok[ob], in_=pc, func=AF.Silu),
                             reads=[Rpc], writes=[Rbt[ob], Rpc])
                P.store(self.dq(), xs_d[t], xs_tok[ob], Rxs[ob], Rxs_d)
                P.store(self.dq(), bt_d[t], b_tok[ob], Rbt[ob], Rbt_d)
                for q2 in range(2):
                    bi = 4 + q2
                    pf = self.bank(bi)
                    Rpf = self.Rbank[bi]
                    for cc in range(4):
                        c = 16 + q2 * 4 + cc
                        for k in range(4):
                            P.op("pe", lambda e, pf=pf, cc=cc, c=c, k=k, s=s, ub=ub: e.matmul(
                                pf[:, cc * 128:(cc + 1) * 128], diagW[:, c, k, :],
                                uT[ub][:, c, s * 128 + k:s * 128 + k + 128], start=(k == 0), stop=False),
                                reads=[RuT[ub], Rdg], writes=[Rpf])
                        P.op("pe", lambda e, pf=pf, cc=cc, c=c: e.matmul(
                            pf[:, cc * 128:(cc + 1) * 128], cbrowB[0:1, c * 128:(c + 1) * 128], oB[0:1, 0:128],
                            start=False, stop=True), reads=[Rcb, self.Rc], writes=[Rpf])
                    P.op("act", lambda e, pf=pf, q2=q2, ob=ob: e.activation(
                        out=bct[ob][:, q2 * 4:(q2 + 1) * 4, :], in_=pf.rearrange("p (a b) -> p a b", a=4),
                        func=AF.Silu), reads=[Rpf], writes=[Rbct[ob], Rpf])
                P.store(self.dq(), bct_d[t], bct[ob], Rbct[ob], Rbct_d)
        if wsteps is not None:
            for st_ in wsteps:
                st_()
        P.barrier()
        self.off = mark

    def phase_b0(self, x_d, sz_d, dt_d, xs_d, bt_d, bct_d, Rin, alog_d, dskip_d, normw_d, wout_d, lng_d, lnb_d,
                 x1_d, Rx1_d, x1T_d, Rx1T_d, comb_d, Rcomb_d):
        P = self.P
        NT = self.NT
        mark = self.off
        iB, mU, mS, oF = self.identB, self.maskU, self.maskSL, self.onesF
        Rc = self.Rc
        wout = self.sb([128, 16, D], BF16)
        Rwo = P.R("wout")
        for q in range(4):
            P.dma("pool", wout[:, q * 4:(q + 1) * 4, :],
                  wout_d[0, q * 512:(q + 1) * 512, :].rearrange("(k p) n -> p k n", p=128), writes=[Rwo])
        normw_b, Rnw = self.bcast_load(normw_d[0:1, :], D_INNER, "sp")
        g_b, Rg = self.bcast_load(lng_d[0:1, :], D, "act")
        b_b, Rb_ = self.bcast_load(lnb_d[0:1, :], D, "sp")
        dsk_b, Rdsk = self.bcast_load(dskip_d[0:1, :], NH, "sp")
        Rgb = P.R("b0_gb")
        st = self.sb([128, 2, 6], F32)
        mv = self.sb([128, 8], F32)
        Rs = P.R("b0_ln")
        P.op("pool", lambda e: e.tensor_copy(out=mv[:, 7:8], in_=g_b[:, 0:1]), reads=[Rg, Rb_], writes=[Rgb])
        hT = self.sb([128, D_INNER], F32)
        hTb = [self.sb([128, D_INNER], BF16) for _ in range(3)]
        RhT = P.R("hT")
        RhTb = P.Rs("hTb", 3)
        P.op("pool", lambda e: e.memset(hT, 0.0), writes=[RhT])
        P.op("pool", lambda e: e.memset(hTb[0], 0.0), writes=[RhTb[0]])
        NLB = 3
        xs = [self.sb([128, D_INNER], BF16) for _ in range(2)]
        btok = [self.sb([128, 512], BF16) for _ in range(2)]
        RldF = P.Rs("b0_ldf", 2)
        bct = [self.sb([128, 8, 128], BF16) for _ in range(NLB)]
        sz = [self.sb([128, D_INNER], BF16) for _ in range(NLB)]
        xr = [self.sb([128, D], F32) for _ in range(NLB)]
        Rld = P.Rs("b0_ld", NLB)
        sm = [self.sb([128, 256], F32) for _ in range(NLB)]
        Rsm = P.Rs("b0_sm", NLB)
        xsdt = [self.sb([128, D_INNER], BF16) for _ in range(2)]
        xsD = [self.sb([128, D_INNER], BF16) for _ in range(2)]
        Rxsdt, RxsD = P.Rs("xsdt", 2), P.Rs("xsD", 2)
        xw = self.sb([128, D_INNER], BF16)
        Rxw = P.R("xw")
        rhsS = self.sb([128, NH // 2, 128], F32)
        RrhsS = P.R("rhsS")
        E = self.sb([128, NH, 128], BF16)
        RE = P.R("E")
        cbm = self.sb([128, NG, 128], BF16)
        Rcbm = P.R("cbm")
        MT = [self.sb([128, NH, 128], BF16) for _ in range(2)]
        RMT = P.Rs("MT", 2)
        y = self.sb([128, D_INNER], F32)
        Ry = P.Rs("y", NG)
        junk = self.sb([128, 512], BF16)
        Rjunk = P.R("junk")
        yn = self.sb([128, D_INNER], BF16)
        Ryn = P.R("yn")
        ynT = self.sb([128, 16, 128], BF16)
        RynT = P.R("ynT")
        r1 = self.sb([128, D], F32)
        r = [r1, r1]
        Rr1 = P.R("b0_r")
        Rr = [Rr1, Rr1]
        xTb = [self.sb([128, KC, 128], BF16) for _ in range(2)]
        RxTb = P.Rs("b0_xTb", 2)
        xTf1 = self.sb([128, KC, 128], F32)
        xTf = [xTf1, xTf1]
        RxTf1 = P.R("b0_xTf")
        RxTf = [RxTf1, RxTf1]
        cb = [self.sb([128, NE], F32) for _ in range(2)]
        Rcb = P.Rs("b0_cb", 2)

        def v64(a):
            return a.rearrange("p (h q) -> p h q", q=64)

        def bh(a, n=NH):
            return a.unsqueeze(2).to_broadcast([128, n, 64])

        def smv(b):
            s_ = sm[b]
            return dict(s=s_, dt=s_[:, 0:32], da=s_[:, 32:64], ecum=s_[:, 64:96], etot=s_[:, 96:128],
                        wexp=s_[:, 128:160], ss=s_[:, 192:196], rstd=s_[:, 196:200], tmp=s_[:, 200:204])

        def loads(t):
            lb = t % NLB
            P.dma("sp", xs[t % 2], xs_d[t], reads=Rin, writes=[RldF[t % 2]])
            P.dma("sp", btok[t % 2], bt_d[t], reads=Rin, writes=[RldF[t % 2]])
            P.dma("sp", bct[lb], bct_d[t], reads=Rin, writes=[Rld[lb]])
            P.dma("sp", sz[lb], sz_d[t], reads=Rin, writes=[Rld[lb]])
            P.dma("sp", xr[lb], x_d[t * 128:(t + 1) * 128, :], writes=[Rld[lb]])
            P.dma("sp", sm[lb][:, 0:160], dt_d[:, t].rearrange("p a h -> p (a h)"), reads=Rin, writes=[Rsm[lb]])

        def front(t):
            b = t % 2
            lb = t % NLB
            v = smv(lb)
            s_, da, etot, wexp, dtt = v["s"], v["da"], v["etot"], v["wexp"], v["dt"]
            Rdt = Rsm[lb]
            P.op("dve", lambda e: e.tensor_tensor(out=v64(xsdt[b]), in0=v64(xs[b]), in1=bh(dtt), op=ALU.mult),
                 reads=[RldF[b], Rdt], writes=[Rxsdt[b]])
            P.op("pool", lambda e: e.tensor_tensor(out=v64(xsD[b]), in0=v64(xs[b]), in1=bh(dsk_b), op=ALU.mult),
                 reads=[RldF[b], Rdsk], writes=[RxsD[b]])
            P.op("dve", lambda e: e.tensor_tensor(out=v64(xw), in0=v64(xsdt[b]), in1=bh(wexp), op=ALU.mult),
                 reads=[Rxsdt[b], Rsm[lb]], writes=[Rxw])
            for q in range(8):
                if q % 4 == 0:
                    hh0 = (q // 4) * 16
                    P.op("dve", lambda e, hh0=hh0: e.tensor_tensor(
                        out=rhsS, in0=mU.unsqueeze(1).to_broadcast([128, NH // 2, 128]),
                        in1=da[:, hh0:hh0 + 16].unsqueeze(2).to_broadcast([128, NH // 2, 128]), op=ALU.mult),
                        reads=[Rsm[lb], Rc], writes=[RrhsS])
                bi = 1 + q % 2
                pq = self.bank(bi)
                Rpq = self.Rbank[bi]
                P.op("pe", lambda e, pq=pq, q=q: e.matmul(pq, mS, rhsS[:, 4 * (q % 4):4 * (q % 4) + 4, :],
                                                           start=True, stop=True),
                     reads=[RrhsS, Rc], writes=[Rpq])
                P.op("act", lambda e, pq=pq, q=q: e.activation(
                    out=E[:, 4 * q:4 * q + 4, :], in_=pq.rearrange("p (a b) -> p a b", a=4), func=AF.Exp),
                    reads=[Rpq], writes=[RE, Rpq])
            b3 = self.bank(3)
            Rb3 = self.Rbank[3]
            for g in range(NG):
                P.op("pe", lambda e, g=g: e.matmul(b3[:, g * 128:(g + 1) * 128], bct[lb][:, g, :],
                                                    bct[lb][:, 4 + g, :], start=True, stop=True),
                     reads=[Rld[lb]], writes=[Rb3])
            P.op("dve", lambda e: e.tensor_tensor(out=cbm, in0=b3.rearrange("p (a b) -> p a b", a=4),
                                                  in1=mU.unsqueeze(1).to_broadcast([128, NG, 128]), op=ALU.mult),
                 reads=[Rb3, Rc], writes=[Rcbm, Rb3])
            for g in range(NG):
                eng = "dve" if g != 3 else "pool"
                P.op(eng, lambda e, g=g: e.tensor_tensor(
                    out=MT[b][:, g * 8:(g + 1) * 8, :], in0=E[:, g * 8:(g + 1) * 8, :],
                    in1=cbm[:, g:g + 1, :].to_broadcast([128, 8, 128]), op=ALU.mult),
                    reads=[RE, Rcbm], writes=[RMT[b]])
            hn = (t + 1) % 3
            for g in range(NG):
                gs = slice(g * 512, (g + 1) * 512)
                ph = self.bank(0)
                Rph = self.Rbank[0]
                P.op("pe", lambda e, g=g, gs=gs: e.matmul(ph, btok[b][:, g * 128:(g + 1) * 128], xw[:, gs],
                                                           start=True, stop=True),
                     reads=[RldF[b], Rxw], writes=[Rph])
                P.op("dve", lambda e, gs=gs, g=g: e.tensor_tensor(
                    out=v64(hT[:, gs]), in0=v64(hT[:, gs]), in1=bh(etot[:, g * 8:(g + 1) * 8], 8), op=ALU.mult),
                    reads=[RhT, Rsm[lb]], writes=[RhT])
                P.op("dve", lambda e, gs=gs: e.tensor_tensor(out=hT[:, gs], in0=ph, in1=hT[:, gs], op=ALU.add),
                     reads=[RhT, Rph], writes=[RhT, Rph])
                P.op("act", lambda e, gs=gs: e.copy(out=hTb[hn][:, gs], in_=hT[:, gs]), reads=[RhT],
                     writes=[RhTb[hn]])

        def back(t):
            b = t % 2
            lb = t % NLB
            hc = t % 3
            v = smv(lb)
            ecum, ss, rstd, tmp = v["ecum"], v["ss"], v["rstd"], v["tmp"]
            for g in range(NG):
                py = self.bank(4 + 2 * (g % 2))
                Rpy = self.Rbank[4 + 2 * (g % 2)]
                pys = self.bank(5 + 2 * (g % 2))
                Rpys = self.Rbank[5 + 2 * (g % 2)]
                gs = slice(g * 512, (g + 1) * 512)
                P.op("pe", lambda e, py=py, gs=gs: e.matmul(py, iB, xsD[b][:, gs], start=True, stop=False),
                     reads=[RxsD[b], Rc], writes=[Rpy])
                for hl in range(8):
                    h = g * 8 + hl
                    P.op("pe", lambda e, py=py, hl=hl, h=h: e.matmul(
                        py[:, hl * 64:(hl + 1) * 64], MT[b][:, h, :], xsdt[b][:, h * 64:(h + 1) * 64], start=False,
                        stop=(hl == 7)), reads=[RMT[b], Rxsdt[b]], writes=[Rpy])
                P.op("pe", lambda e, pys=pys, g=g, gs=gs: e.matmul(pys, bct[lb][:, 4 + g, :], hTb[hc][:, gs],
                                                                    start=True, stop=True),
                     reads=[Rld[lb], RhTb[hc]], writes=[Rpys])
                P.op("dve", lambda e, pys=pys, gs=gs, g=g: e.tensor_tensor(
                    out=v64(y[:, gs]), in0=v64(pys), in1=bh(ecum[:, g * 8:(g + 1) * 8], 8), op=ALU.mult),
                    reads=[Rpys, Rsm[lb]], writes=[Ry[g], Rpys])
                P.op("dve", lambda e, py=py, gs=gs: e.tensor_tensor(out=y[:, gs], in0=py, in1=y[:, gs], op=ALU.add),
                     reads=[Rpy, Ry[g]], writes=[Ry[g], Rpy])
                P.op("dve", lambda e, gs=gs: e.tensor_tensor(out=y[:, gs], in0=y[:, gs], in1=sz[lb][:, gs],
                                                              op=ALU.mult),
                     reads=[Ry[g], Rld[lb]], writes=[Ry[g]])
                P.op("act", lambda e, gs=gs, g=g: e.activation(out=junk, in_=y[:, gs], func=AF.Square,
                                                                accum_out=ss[:, g:g + 1]),
                     reads=[Ry[g]], writes=[Rjunk, Rsm[lb]])
            P.op("dve", lambda e: e.tensor_scalar(out=tmp, in0=ss, scalar1=1.0 / 512.0, scalar2=RMS_EPS,
                                                  op0=ALU.mult, op1=ALU.add), reads=[Rsm[lb]], writes=[Rsm[lb]])
            P.op("act", lambda e: e.activation(out=tmp, in_=tmp, func=AF.Sqrt), reads=[Rsm[lb]], writes=[Rsm[lb]])
            P.op("dve", lambda e: e.reciprocal(out=rstd, in_=tmp), reads=[Rsm[lb]], writes=[Rsm[lb]])
            for g in range(NG):
                gs = slice(g * 512, (g + 1) * 512)
                P.op("dve", lambda e, gs=gs, g=g: e.scalar_tensor_tensor(
                    out=yn[:, gs], in0=y[:, gs], scalar=rstd[:, g:g + 1], in1=normw_b[:, gs], op0=ALU.mult,
                    op1=ALU.mult), reads=[Ry[g], Rsm[lb], Rnw], writes=[Ryn])
            for hh in range(2):
                pt = self.bank(4 + hh, BF16)
                Rpt = self.Rbank[4 + hh]
                for k in range(8):
                    kk = hh * 8 + k
                    P.op("pe", lambda e, pt=pt, k=k, kk=kk: e.transpose(pt[:, k * 128:(k + 1) * 128],
                                                                        yn[:, kk * 128:(kk + 1) * 128], iB),
                         reads=[Ryn, Rc], writes=[Rpt])
                P.op("act", lambda e, pt=pt, hh=hh: e.copy(out=ynT[:, hh * 8:(hh + 1) * 8, :],
                                                           in_=pt.rearrange("p (a b) -> p a b", a=8)),
                     reads=[Rpt], writes=[RynT, Rpt])
            for n in range(2):
                pm = self.bank(6 + n)
                Rpm = self.Rbank[6 + n]
                ns = slice(n * 512, (n + 1) * 512)
                for k in range(16):
                    P.op("pe", lambda e, pm=pm, k=k, ns=ns: e.matmul(pm, ynT[:, k, :], wout[:, k, ns],
                                                                      start=(k == 0), stop=(k == 15)),
                         reads=[RynT, Rwo], writes=[Rpm])
                P.op("dve", lambda e, pm=pm, ns=ns: e.scalar_tensor_tensor(
                    out=r[b][:, ns], in0=xr[lb][:, ns], scalar=ALPHA, in1=pm, op0=ALU.mult, op1=ALU.add),
                    reads=[Rld[lb], Rpm], writes=[Rr[b], Rpm])
            self.layer_norm(r[b], Rr[b], st, mv, Rs, g_b, b_b, Rgb)
            P.store("sp", x1_d[t * 128:(t + 1) * 128, :], r[b], Rr[b], Rx1_d)
            self.make_xT(r[b], Rr[b], 4, xTb[b], RxTb[b], xTf[b], RxTf[b])
            P.store("sp", x1T_d[t], xTb[b], RxTb[b], Rx1T_d)
            self.router(xTf[b], RxTf[b], 6, cb[b], Rcb[b])
            P.store("sp", comb_d[t], cb[b], Rcb[b], Rcomb_d)

        loads(0)
        if NT > 1:
            loads(1)
        front(0)
        for t in range(NT):
            if t + 2 < NT:
                loads(t + 2)
            m0 = P.mark()
            if t + 1 < NT:
                front(t + 1)
            m1 = P.mark()
            back(t)
            P.interleave(m0, m1)
        P.barrier()
        self.off = mark

    def attn_bias_setup(self, rel_d, frep_d, expb_d, Rexpb_d):
        P = self.P
        expB = self.sb([128, AH, 5, 128], BF16)
        Rbias = P.R("bias_build")
        tab = self.sb([128, 2, AH], F32)
        Rtab = P.R("tab")
        P.dma("sp", tab, rel_d[0, 1:257, :].rearrange("(a p) h -> p a h", p=128), writes=[Rtab])
        FT = self.sb([AH, 768], F32)
        RFT = P.R("FT")
        pb = self.bank(0)
        Rpb = self.Rbank[0]
        for a in range(2):
            P.op("pe", lambda e, a=a: e.transpose(pb[0:AH, a * 128:(a + 1) * 128], tab[:, a, :], self.identF),
                 reads=[Rtab, self.Rc], writes=[Rpb])
        P.op("dve", lambda e: e.tensor_copy(out=FT[:, 0:256], in_=pb[0:AH, 0:256]), reads=[Rpb], writes=[RFT, Rpb])
        P.op("dve", lambda e: e.tensor_copy(out=FT[:, 256:768], in_=FT[:, 255:256].to_broadcast([AH, 512])),
             reads=[RFT], writes=[RFT])
        Rfrep = P.R("frep")
        P.store("sp", frep_d, FT.unsqueeze(1).to_broadcast([AH, 128, 768]), RFT, Rfrep)
        stage = self.sb([128, AH, 5, 128], F32)
        Rst = P.R("bstage")
        for h in range(AH):
            src = bass.AP(frep_d.tensor, h * 128 * 768 + 127, [[767, 128], [128, 5], [1, 128]])
            P.dma(("sp", "act")[h % 2], stage[:, h, :, :], src, reads=[Rfrep], writes=[Rst])
        P.op("pool", lambda e: e.memset(stage[0:64, :, 4, 64:128], -30000.0), reads=[Rst], writes=[Rst])
        P.op("pool", lambda e: e.memset(stage[64:128, :, 0, 0:64], -30000.0), reads=[Rst], writes=[Rst])
        for jr in range(5):
            P.op("act", lambda e, jr=jr: e.activation(out=expB[:, :, 4 - jr, :], in_=stage[:, :, jr, :],
                                                      func=AF.Exp), reads=[Rst], writes=[Rbias])
        P.store("sp", expb_d, expB, Rbias, Rexpb_d)

    def phase_attn(self, x_d, xT_d, Rin, wqkv_d, bqkv_d, wo_d, bo_d, rel_d, frep_d, lng_d, lnb_d,
                   x3_d, Rx3_d, x3T_d, Rx3T_d, comb_d, Rcomb_d, wconv=None):
        P = self.P
        NT = self.NT
        mark = self.off
        wsteps = wconv() if wconv is not None else None
        self.expB = self.sb([128, AH, 5, 128], BF16)
        self.Rbias = P.R("bias")
        P.dma("sp", self.expB, rel_d, reads=[frep_d], writes=[self.Rbias])
        iB, oB, Rc = self.identB, self.onesB, self.Rc
        RS = 6
        wqkv = self.sb([128, KC, 3 * D], BF16)
        Rwq = P.Rs("wqkv", 3)
        for j in range(6):
            P.dma("pool", wqkv[:, :, j * 512:(j + 1) * 512],
                  wqkv_d[0, :, j * 512:(j + 1) * 512].rearrange("(k p) n -> p k n", p=128), writes=[Rwq[j // 2]])
        wo = self.sb([128, KC, D], BF16)
        Rwo = P.R("wo")
        P.dma("pool", wo, wo_d[0].rearrange("(k p) n -> p k n", p=128), writes=[Rwo])
        browB = self.sb([1, 4 * D], BF16)
        Rbr = P.R("brow")
        P.dma("pool", browB[:, 0:3 * D], bqkv_d[0:1, :], writes=[Rbr])
        P.dma("pool", browB[:, 3 * D:4 * D], bo_d[0:1, :], writes=[Rbr])
        g_b, Rg = self.bcast_load(lng_d[1:2, :], D, "act")
        b_b, Rb_ = self.bcast_load(lnb_d[1:2, :], D, "sp")
        Rgb = P.R("at_gb")
        st = self.sb([128, 2, 6], F32)
        mv = self.sb([128, 8], F32)
        Rs = P.R("at_ln")
        P.op("pool", lambda e: e.tensor_copy(out=mv[:, 7:8], in_=g_b[:, 0:1]), reads=[Rg, Rb_], writes=[Rgb])
        KT = self.sb([128, KC, RS, 128], BF16)
        RK = P.Rs("KTr", RS)
        V = self.sb([128, RS, AH, 65], BF16)
        RV = P.Rs("Vr", RS)
        for sl in range(RS):
            P.op("pool", lambda e, sl=sl: e.memset(V[:, sl, :, 64:65], 1.0), writes=[RV[sl]])
        QT = [self.sb([128, KC, 128], BF16) for _ in range(2)]
        RQ = P.Rs("QT", 2)
        xT = [self.sb([128, KC, 128], BF16) for _ in range(2)]
        xr = [self.sb([128, D], F32) for _ in range(2)]
        Rld = P.Rs("at_ld", 2)
        NPB = 4
        PT = [self.sb([128, 5 * 128], BF16) for _ in range(NPB)]
        RPT = P.Rs("PT", NPB)
        O = self.sb([128, D], BF16)
        RO = P.R("O")
        rec = self.sb([128, 2, 4], F32)
        Rrec = P.Rs("rec", 2)
        OT = self.sb([128, KC, 128], BF16)
        ROT = P.R("OT")
        r = [self.sb([128, D], F32) for _ in range(2)]
        Rr = P.Rs("at_r", 2)
        xTb = [self.sb([128, KC, 128], BF16) for _ in range(2)]
        RxTb = P.Rs("at_xTb", 2)
        xTf = [self.sb([128, KC, 128], F32) for _ in range(2)]
        RxTf = P.Rs("at_xTf", 2)
        cb = [self.sb([128, NE], F32) for _ in range(2)]
        Rcb = P.Rs("at_cb", 2)
        hcount = [0]

        def proj(t):
            b = t % 2
            sl = t % RS
            P.dma("sp", xT[b], xT_d[t], reads=Rin, writes=[Rld[b]])
            P.dma("sp", xr[b], x_d[t * 128:(t + 1) * 128, :], reads=Rin, writes=[Rld[b]])
            for which in range(2):
                for hf in range(2):
                    bi = 6 + hf
                    pq = self.bank(bi)
                    Rpq = self.Rbank[bi]
                    for mm in range(4):
                        m = hf * 4 + mm
                        col = which * D + m * 128
                        for k in range(KC):
                            P.op("pe", lambda e, pq=pq, mm=mm, k=k, col=col: e.matmul(
                                pq[:, mm * 128:(mm + 1) * 128], wqkv[:, k, col:col + 128], xT[b][:, k, :],
                                start=(k == 0), stop=False), reads=[Rwq[col // 1024], Rld[b]], writes=[Rpq])
                        P.op("pe", lambda e, pq=pq, mm=mm, col=col: e.matmul(
                            pq[:, mm * 128:(mm + 1) * 128], browB[0:1, col:col + 128], oB[0:1, 0:128],
                            start=False, stop=True), reads=[Rbr, Rc], writes=[Rpq])
                    pv3 = pq.rearrange("p (a b) -> p a b", a=4)
                    if which == 0:
                        P.op("act", lambda e, pv3=pv3, hf=hf: e.mul(out=QT[b][:, hf * 4:(hf + 1) * 4, :], in_=pv3,
                                                                    mul=0.125),
                             reads=[Rpq], writes=[RQ[b], Rpq])
                    else:
                        P.op("dve", lambda e, pv3=pv3, hf=hf: e.tensor_copy(
                            out=KT[:, hf * 4:(hf + 1) * 4, sl, :], in_=pv3), reads=[Rpq], writes=[RK[sl], Rpq])
            for n in range(2):
                bi = 6 + n
                pv = self.bank(bi)
                Rpv = self.Rbank[bi]
                for k in range(KC):
                    P.op("pe", lambda e, pv=pv, k=k, n=n: e.matmul(
                        pv, xT[b][:, k, :], wqkv[:, k, 2 * D + n * 512:2 * D + (n + 1) * 512], start=(k == 0),
                        stop=False), reads=[Rwq[2], Rld[b]], writes=[Rpv])
                P.op("pe", lambda e, pv=pv, n=n: e.matmul(pv, oB[0:1, 0:128],
                                                          browB[0:1, 2 * D + n * 512:2 * D + (n + 1) * 512],
                                                          start=False, stop=True), reads=[Rbr, Rc], writes=[Rpv])
                if n == 0:
                    P.op("act", lambda e, pv=pv, n=n: e.copy(out=V[:, sl, n * 8:(n + 1) * 8, 0:64],
                                                            in_=pv.rearrange("p (a b) -> p a b", a=8)),
                         reads=[Rpv], writes=[RV[sl], Rpv])
                else:
                    P.op("dve", lambda e, pv=pv, n=n: e.tensor_copy(out=V[:, sl, n * 8:(n + 1) * 8, 0:64],
                                                                   in_=pv.rearrange("p (a b) -> p a b", a=8)),
                         reads=[Rpv], writes=[RV[sl], Rpv])

        def heads(t):
            b = t % 2
            nj = min(t, 4) + 1
            js = list(range(t - nj + 1, t + 1))

            def scores(h):
                m, p0 = h // 2, (h % 2) * 64
                pi = hcount[0] % 2
                pbuf = hcount[0] % NPB
                hcount[0] += 1
                pst = self.pair(pi)
                Rps = [self.Rbank[2 * pi], self.Rbank[2 * pi + 1]]
                for jj, j in enumerate(js):
                    ksl = j % RS
                    o_ = pst[:, jj * 128:(jj + 1) * 128]
                    wr = [Rps[0]] if jj < 4 else [Rps[1]]
                    P.op("pe", lambda e, o_=o_, ksl=ksl: e.matmul(
                        o_, KT[p0:p0 + 64, m, ksl, :], QT[b][p0:p0 + 64, m, :], start=True, stop=True),
                        reads=[RK[ksl], RQ[b]], writes=wr)
                wrs = Rps if nj == 5 else [Rps[0]]
                P.op("act", lambda e: e.activation(out=PT[pbuf][:, 0:nj * 128], in_=pst[:, 0:nj * 128], func=AF.Exp),
                     reads=wrs, writes=[RPT[pbuf]] + wrs)
                P.op(("pool" if (h % 2 and wsteps is None) else "dve"), lambda e: e.tensor_tensor(
                    out=PT[pbuf][:, 0:nj * 128], in0=PT[pbuf][:, 0:nj * 128],
                    in1=self.expB[:, h, 5 - nj:5, :].rearrange("p a b -> p (a b)"), op=ALU.mult),
                    reads=[RPT[pbuf], self.Rbias], writes=[RPT[pbuf]])
                return pbuf

            def pv_out(h, pbuf):
                hq, hl = h // 4, h % 4
                ob = 4 + hq % 2
                po = self.bank(ob)
                Rpo = self.Rbank[ob]
                for jj, j in enumerate(js):
                    ksl = j % RS
                    lastj = (jj == len(js) - 1)
                    P.op("pe", lambda e, jj=jj, ksl=ksl, lastj=lastj: e.matmul(
                        po[:, hl * 128:hl * 128 + 65], PT[pbuf][:, jj * 128:(jj + 1) * 128], V[:, ksl, h, :],
                        start=(jj == 0), stop=lastj), reads=[RPT[pbuf], RV[ksl]], writes=[Rpo])
                if hl == 3:
                    po3 = po.rearrange("p (a b) -> p a b", a=4)
                    rb = hq % 2
                    P.op("dve", lambda e: e.reciprocal(out=rec[:, rb, :].unsqueeze(2), in_=po3[:, :, 64:65]),
                         reads=[Rpo], writes=[Rrec[rb], Rpo])
                    P.op("dve", lambda e: e.tensor_tensor(
                        out=O[:, hq * 256:(hq + 1) * 256].rearrange("p (a b) -> p a b", a=4), in0=po3[:, :, 0:64],
                        in1=rec[:, rb, :].unsqueeze(2).to_broadcast([128, 4, 64]), op=ALU.mult),
                        reads=[Rpo, Rrec[rb]], writes=[RO, Rpo])

            LAG = 2
            pend = []
            for h in range(AH):
                pend.append((h, scores(h)))
                if len(pend) > LAG:
                    pv_out(*pend.pop(0))
            while pend:
                pv_out(*pend.pop(0))
            pt = self.bank(4, BF16)
            Rpt = self.Rbank[4]
            for k in range(KC):
                P.op("pe", lambda e, k=k: e.transpose(pt[:, k * 128:(k + 1) * 128], O[:, k * 128:(k + 1) * 128], iB),
                     reads=[RO, Rc], writes=[Rpt])
            P.op("act", lambda e: e.copy(out=OT, in_=pt.rearrange("p (a b) -> p a b", a=8)), reads=[Rpt],
                 writes=[ROT, Rpt])
            for n in range(2):
                pm = self.bank(n)
                Rpm = self.Rbank[n]
                ns = slice(n * 512, (n + 1) * 512)
                for k in range(KC):
                    P.op("pe", lambda e, pm=pm, k=k, ns=ns: e.matmul(pm, OT[:, k, :], wo[:, k, ns], start=(k == 0),
                                                                      stop=False), reads=[ROT, Rwo], writes=[Rpm])
                P.op("pe", lambda e, pm=pm, n=n: e.matmul(pm, oB[0:1, 0:128],
                                                          browB[0:1, 3 * D + n * 512:3 * D + (n + 1) * 512],
                                                          start=False, stop=True), reads=[Rbr, Rc], writes=[Rpm])
                P.op("dve", lambda e, pm=pm, ns=ns: e.scalar_tensor_tensor(
                    out=r[b][:, ns], in0=xr[b][:, ns], scalar=ALPHA, in1=pm, op0=ALU.mult, op1=ALU.add),
                    reads=[Rld[b], Rpm], writes=[Rr[b], Rpm])
            self.layer_norm(r[b], Rr[b], st, mv, Rs, g_b, b_b, Rgb)
            P.store(self.dq(), x3_d[t * 128:(t + 1) * 128, :], r[b], Rr[b], Rx3_d)
            self.make_xT(r[b], Rr[b], 2, xTb[b], RxTb[b], xTf[b], RxTf[b])
            P.store(self.dq(), x3T_d[t], xTb[b], RxTb[b], Rx3T_d)
            self.router(xTf[b], RxTf[b], 5, cb[b], Rcb[b])
            P.store(self.dq(), comb_d[t], cb[b], Rcb[b], Rcomb_d)

        proj(0)
        for t in range(NT):
            if wsteps is not None:
                for _ in range(2 if t % 2 == 0 else 1):
                    st_ = next(wsteps, None)
                    if st_ is not None:
                        st_()
            m0 = P.mark()
            if t + 1 < NT:
                proj(t + 1)
            m1 = P.mark()
            heads(t)
            P.interleave(m0, m1)
        if wsteps is not None:
            for st_ in wsteps:
                st_()
        P.barrier()
        self.off = mark


    def phase_moe_sparse(self, layer, x_d, comb_d, Rin, wg_d, wu_d, wd_d, lng_d, lnb_d, out_d, Rout,
                         outT_d, RoutT, xsort_d, ysort_d, Rzero, stop_after=9, dbg=None, wb=None, Rwb=None):
        P = self.P
        NT = self.NT
        mark = self.off
        ST = 512
        NSL = (2 * self.T) // ST + NE
        I32 = mybir.dt.int32
        iB, mU, oF, Rc = self.identB, self.maskU, self.onesF, self.Rc
        Rxsort, Rysort = P.R("xsort"), P.R("ysort")
        TE = NT * NE
        comb = self.sb([128, NT, NE], F32)
        M = self.sb([128, NT, NE], F32)
        M0 = self.sb([128, NT, NE], F32)
        M1 = self.sb([128, NT, NE], F32)
        T1 = self.sb([128, NT, NE], F32)
        Cc = self.sb([128, NT, NE], F32)
        tot = self.sb([128, NT, NE], F32)
        Pfx = self.sb([128, NT, NE], F32)
        w16 = self.sb([128, NE], F32)
        sml = self.sb([128, 256], F32)
        cmpT = self.sb([128, NE, 8], F32)
        cmpE = self.sb([128, NSL, NE], F32)
        posg = self.sb([128, 4, NT], F32)
        pos_i = self.sb([128, 2, NT], I32)
        esb = self.sb([128, NSL], F32)
        sthr = self.sb([128, NSL], F32)
        widx_i = self.sb([128, 2, NSL], I32)
        widxd_i = self.sb([128, 4, NSL], I32)
        esd = self.sb([128, NSL], F32)
        Rr_ = P.R("route")
        R1 = [Rr_]
        d = lambda fn, rd=(), wr=(): P.op("dve", fn, reads=R1 + list(rd), writes=R1 + list(wr))
        pl = lambda fn, rd=(), wr=(): P.op("pool", fn, reads=R1 + list(rd), writes=R1 + list(wr))
        P.dma("sp", comb, comb_d.rearrange("t p e -> p t e"), reads=Rin, writes=[Rr_])
        Rk = []
        for e_ in range(NE):
            Rk.append(P.R("k"))
            P.op("pool", lambda e, e_=e_: e.memset(w16[:, e_:e_ + 1], float(NE - e_)), writes=[Rk[-1]])
        for j in range(8):
            Rk.append(P.R("k"))
            P.op("pool", lambda e, j=j: e.memset(sml[:, 64 + j:65 + j], float(ST * j)), writes=[Rk[-1]])
        for s_ in range(NSL):
            Rk.append(P.R("k"))
            P.op("pool", lambda e, s_=s_: e.memset(sthr[:, s_:s_ + 1], float(ST * s_)), writes=[Rk[-1]])
        P.op("pool", lambda e: e.memset(sml[:, 120:121], 0.0), reads=Rk, writes=[Rr_])
        thr = sml[:, 64:72]
        n_e, ntile, npad, off, offend = (sml[:, 0:16], sml[:, 16:32], sml[:, 32:48], sml[:, 80:96], sml[:, 96:112])
        pidx = sml[:, 112:113]

        def bt(a):
            return a.unsqueeze(1).to_broadcast([128, NT, NE])

        def be(a):
            return a.unsqueeze(2).to_broadcast([128, NT, NE])

        self._Rroute = Rr_
        self.route_batched(comb, M, M0, M1, T1, posg.rearrange("p a t -> p (a t)").rearrange("p (a t) -> p a t", a=4)
                           if False else self.sb([128, 8, NT], F32), d)
        d(lambda e: e.tensor_single_scalar(out=M, in_=comb, scalar=0.0, op=ALU.is_gt))
        d(lambda e: e.tensor_tensor(out=T1, in0=M, in1=bt(w16), op=ALU.mult))
        d(lambda e: e.tensor_reduce(out=posg[:, 0, :], in_=T1, axis=AX.X, op=ALU.max))
        d(lambda e: e.tensor_tensor(out=M0, in0=T1, in1=be(posg[:, 0, :]), op=ALU.is_equal))
        d(lambda e: e.tensor_tensor(out=M1, in0=M, in1=M0, op=ALU.subtract))
        pbC, pbT = self.bank(0), self.bank(1)
        Mf = M.rearrange("p t e -> p (t e)")
        P.op("pe", lambda e: e.matmul(pbC[:, 0:TE], mU, Mf, start=True, stop=True), reads=[Rr_, Rc],
             writes=[self.Rbank[0]])
        P.op("pe", lambda e: e.matmul(pbT[:, 0:TE], oF, Mf, start=True, stop=True), reads=[Rr_, Rc],
             writes=[self.Rbank[1]])
        d(lambda e: e.tensor_copy(out=Cc.rearrange("p t e -> p (t e)"), in_=pbC[:, 0:TE]), rd=[self.Rbank[0]],
          wr=[self.Rbank[0]])
        d(lambda e: e.tensor_copy(out=tot.rearrange("p t e -> p (t e)"), in_=pbT[:, 0:TE]), rd=[self.Rbank[1]],
          wr=[self.Rbank[1]])
        d(lambda e: e.memset(Pfx[:, 0, :], 0.0))
        for t in range(1, NT):
            d(lambda e, t=t: e.tensor_tensor(out=Pfx[:, t, :], in0=Pfx[:, t - 1, :], in1=tot[:, t - 1, :], op=ALU.add))
        d(lambda e: e.tensor_tensor(out=n_e, in0=Pfx[:, NT - 1, :], in1=tot[:, NT - 1, :], op=ALU.add))
        d(lambda e: e.tensor_tensor(out=cmpT, in0=n_e.unsqueeze(2).to_broadcast([128, NE, 8]),
                                    in1=thr.unsqueeze(1).to_broadcast([128, NE, 8]), op=ALU.is_gt))
        d(lambda e: e.tensor_reduce(out=ntile, in_=cmpT, axis=AX.X, op=ALU.add))
        d(lambda e: e.tensor_scalar(out=npad, in0=ntile, scalar1=float(ST), scalar2=None, op0=ALU.mult))
        d(lambda e: e.memset(off[:, 0:1], 0.0))
        for e_ in range(1, NE):
            d(lambda e, e_=e_: e.tensor_tensor(out=off[:, e_:e_ + 1], in0=off[:, e_ - 1:e_], in1=npad[:, e_ - 1:e_],
                                               op=ALU.add))
        d(lambda e: e.tensor_tensor(out=offend, in0=off, in1=npad, op=ALU.add))
        d(lambda e: e.tensor_tensor(out=T1, in0=Cc, in1=M, op=ALU.subtract))
        d(lambda e: e.tensor_tensor(out=T1, in0=T1, in1=Pfx, op=ALU.add))
        d(lambda e: e.tensor_tensor(out=T1, in0=T1, in1=bt(off), op=ALU.add))
        d(lambda e: e.tensor_tensor(out=Cc, in0=T1, in1=M0, op=ALU.mult))
        d(lambda e: e.tensor_reduce(out=posg[:, 0, :], in_=Cc, axis=AX.X, op=ALU.add))
        d(lambda e: e.tensor_tensor(out=Cc, in0=T1, in1=M1, op=ALU.mult))
        d(lambda e: e.tensor_reduce(out=posg[:, 1, :], in_=Cc, axis=AX.X, op=ALU.add))
        d(lambda e: e.tensor_tensor(out=Cc, in0=comb, in1=M0, op=ALU.mult))
        d(lambda e: e.tensor_reduce(out=posg[:, 2, :], in_=Cc, axis=AX.X, op=ALU.add))
        d(lambda e: e.tensor_tensor(out=Cc, in0=comb, in1=M1, op=ALU.mult))
        d(lambda e: e.tensor_reduce(out=posg[:, 3, :], in_=Cc, axis=AX.X, op=ALU.add))
        d(lambda e: e.tensor_copy(out=pos_i, in_=posg[:, 0:2, :]))
        d(lambda e: e.tensor_tensor(out=cmpE, in0=offend.unsqueeze(1).to_broadcast([128, NSL, NE]),
                                    in1=sthr.unsqueeze(2).to_broadcast([128, NSL, NE]), op=ALU.is_le))
        d(lambda e: e.tensor_reduce(out=esb, in_=cmpE, axis=AX.X, op=ALU.add))
        d(lambda e: e.tensor_scalar(out=sthr, in0=esb, scalar1=float(NE) - 0.5, scalar2=65536.0, op0=ALU.is_gt,
                                    op1=ALU.mult))
        d(lambda e: e.tensor_scalar(out=esb, in0=esb, scalar1=float(NE - 1), scalar2=None, op0=ALU.min))
        d(lambda e: e.tensor_reduce(out=pidx, in_=mU, axis=AX.X, op=ALU.add), rd=[Rc])
        d(lambda e: e.tensor_scalar(out=pidx, in0=pidx, scalar1=-2.0, scalar2=float(2 * 128),
                                    op0=ALU.mult, op1=ALU.add))
        d(lambda e: e.tensor_scalar(out=esd, in0=esb, scalar1=512.0, scalar2=None, op0=ALU.mult))
        d(lambda e: e.tensor_tensor(out=esd, in0=esd, in1=sthr, op=ALU.add))
        d(lambda e: e.tensor_scalar(out=sml[:, 113:114], in0=pidx, scalar1=0.5, scalar2=None, op0=ALU.mult))
        for k in range(4):
            d(lambda e, k=k: e.tensor_scalar(out=cmpE[:, :, 0], in0=esd, scalar1=sml[:, 113:114], scalar2=float(k * 128),
                                             op0=ALU.add, op1=ALU.add))
            d(lambda e, k=k: e.tensor_copy(out=widxd_i[:, k, :], in_=cmpE[:, :, 0]))
        d(lambda e: e.tensor_scalar(out=esb, in0=esb, scalar1=128.0, scalar2=sml[:, 113:114], op0=ALU.mult,
                                    op1=ALU.add))
        d(lambda e: e.tensor_tensor(out=esb, in0=esb, in1=sthr, op=ALU.add))
        d(lambda e: e.tensor_copy(out=widx_i[:, 0, :], in_=esb))

        if dbg is not None:
            P.store("sp", dbg["pos"], pos_i, Rr_, dbg["R"])
            P.store("sp", dbg["widx"], widx_i, Rr_, dbg["R"])
            P.store("sp", dbg["posg"], posg, Rr_, dbg["R"])
        if stop_after < 2:
            P.barrier()
            self.off = mark
            return
        mark4 = self.off
        xf = [self.sb([128, D], F32) for _ in range(4)]
        Rxf = P.Rs("sp_xf", 4)
        xb = [self.sb([128, D], BF16) for _ in range(4)]
        Rxb = P.Rs("sp_xb", 4)
        for t in range(NT):
            b = t % 4
            P.dma("sp", xf[b], x_d[t * 128:(t + 1) * 128, :], reads=Rin, writes=[Rxf[b]])
            P.op("act", lambda e, b=b: e.copy(out=xb[b], in_=xf[b]), reads=[Rxf[b]], writes=[Rxb[b]])
            for k in range(2):
                i = Ins("pool", (lambda e, b=b, k=k, t=t: e.indirect_dma_start(
                    out=xsort_d[:, :], out_offset=bass.IndirectOffsetOnAxis(ap=pos_i[:, k, t:t + 1], axis=0),
                    in_=xb[b], in_offset=None)), (Rxb[b], Rr_, Rzero), (Rxsort,), True)
                i.key = Rxb[b]
                i.phase = P.phase
                P.ins.append(i)

        if stop_after < 2.4:
            P.barrier()
            self.off = mark
            return
        wg_v, wu_v, wd_v = wb
        wgs = [self.sb([128, KC, DFF], BF16) for _ in range(2)]
        wus = [self.sb([128, KC, DFF], BF16) for _ in range(2)]
        wds = [self.sb([128, 4, D], BF16) for _ in range(2)]
        Rwg, Rwu, Rwd = P.Rs("s_wg", 2), P.Rs("s_wu", 2), P.Rs("s_wd", 2)
        xs_sb = [self.sb([128, 4, D], BF16) for _ in range(2)]
        Rxs = P.Rs("s_xs", 2)
        xTs = [self.sb([128, KC, ST], BF16) for _ in range(2)]
        RxTs = P.Rs("s_xTs", 2)
        sg = [self.sb([128, 512], F32) for _ in range(2)]
        Rsg = P.Rs("s_sg", 2)
        hT = [self.sb([128, 4, 512], BF16) for _ in range(2)]
        RhT = P.Rs("s_hT", 2)
        ysb = [self.sb([128, D], F32) for _ in range(2)]
        Rysb = P.Rs("s_ysb", 2)

        def wgather(dst, src_v, s_, Rw):
            d2 = dst.rearrange("p k f -> p (k f)")
            i = Ins("pool", (lambda e: e.indirect_dma_start(
                out=d2, out_offset=None, in_=src_v,
                in_offset=bass.IndirectOffsetOnAxis(ap=widx_i[:, 0, s_:s_ + 1], axis=0),
                bounds_check=self._bc_reg(e), oob_is_err=False)),
                (Rr_, Rwb), (Rw,), True)
            i.key = Rw
            i.phase = P.phase
            P.ins.append(i)

        def fetch(s_):
            wb = s_ % 2
            wgather(wgs[wb], wg_v, s_, Rwg[wb])
            wgather(wus[wb], wu_v, s_, Rwu[wb])
            for k in range(4):
                i = Ins("pool", (lambda e, k=k: e.indirect_dma_start(
                    out=wds[wb][:, k, :], out_offset=None, in_=wd_v,
                    in_offset=bass.IndirectOffsetOnAxis(ap=widxd_i[:, k, s_:s_ + 1], axis=0),
                    bounds_check=self._bc_reg2(e), oob_is_err=False)), (Rr_, Rwb), (Rwd[wb],), True)
                i.key = Rwd[wb]
                i.phase = P.phase
                P.ins.append(i)
            P.dma("sp", xs_sb[wb], xsort_d[s_ * ST:(s_ + 1) * ST, :].rearrange("(a p) d -> p a d", p=128),
                  reads=[Rxsort], writes=[Rxs[wb]])

        yc = 0
        fetch(0)
        for s_ in range(NSL):
            wb = s_ % 2
            if s_ + 1 < NSL:
                fetch(s_ + 1)
            if stop_after < 2.6:
                continue
            for a in range(4):
                pt = self.bank(a % 2, BF16)
                Rpt = self.Rbank[a % 2]
                xv = xs_sb[wb][:, a, :].rearrange("p (c k) -> p k c", k=KC)
                for k in range(KC):
                    P.op("pe", lambda e, pt=pt, xv=xv, k=k: e.transpose(pt[:, k * 128:(k + 1) * 128], xv[:, k, :], iB),
                         reads=[Rxs[wb], Rc], writes=[Rpt])
                if a % 2 == 0:
                    P.op("act", lambda e, pt=pt, a=a, wb=wb: e.copy(
                        out=xTs[wb][:, :, a * 128:(a + 1) * 128], in_=pt.rearrange("p (k c) -> p k c", k=KC)),
                        reads=[Rpt], writes=[RxTs[wb], Rpt])
                else:
                    P.op("dve", lambda e, pt=pt, a=a, wb=wb: e.tensor_copy(
                        out=xTs[wb][:, :, a * 128:(a + 1) * 128], in_=pt.rearrange("p (k c) -> p k c", k=KC)),
                        reads=[Rpt], writes=[RxTs[wb], Rpt])
            if stop_after < 2.8:
                continue
            hb = s_ % 2
            for c in range(4):
                pg = self.bank(2)
                Rpg = self.Rbank[2]
                pu = self.bank(3)
                Rpu = self.Rbank[3]
                sgb = c % 2
                wgc = wgs[wb].rearrange("p k (c q) -> p k c q", c=4)
                wuc = wus[wb].rearrange("p k (c q) -> p k c q", c=4)
                for k in range(KC):
                    P.op("pe", lambda e, pg=pg, k=k, c=c, wgc=wgc, wb=wb: e.matmul(
                        pg, wgc[:, k, c, :], xTs[wb][:, k, :], start=(k == 0), stop=(k == KC - 1)),
                        reads=[Rwg[wb], RxTs[wb]], writes=[Rpg])
                for k in range(KC):
                    P.op("pe", lambda e, pu=pu, k=k, c=c, wuc=wuc, wb=wb: e.matmul(
                        pu, wuc[:, k, c, :], xTs[wb][:, k, :], start=(k == 0), stop=(k == KC - 1)),
                        reads=[Rwu[wb], RxTs[wb]], writes=[Rpu])
                P.op("act", lambda e, pg=pg, sgb=sgb: e.activation(out=sg[sgb], in_=pg, func=AF.Silu),
                     reads=[Rpg], writes=[Rsg[sgb], Rpg])
                P.op("dve", lambda e, pu=pu, sgb=sgb, hb=hb, c=c: e.tensor_tensor(
                    out=hT[hb][:, c, :], in0=pu, in1=sg[sgb], op=ALU.mult),
                    reads=[Rpu, Rsg[sgb]], writes=[RhT[hb], Rpu])
            for a in range(4):
                yb = yc % 2
                yc += 1
                for n in range(2):
                    bi = 4 + (a % 2) * 2 + n
                    po = self.bank(bi)
                    Rpo = self.Rbank[bi]
                    for c in range(4):
                        P.op("pe", lambda e, po=po, c=c, a=a, n=n, hb=hb, wb=wb: e.matmul(
                            po, hT[hb][:, c, a * 128:(a + 1) * 128], wds[wb][:, c, n * 512:(n + 1) * 512],
                            start=(c == 0), stop=(c == 3)), reads=[RhT[hb], Rwd[wb]], writes=[Rpo])
                    if n == 0:
                        P.op("act", lambda e, po=po, yb=yb: e.copy(out=ysb[yb][:, 0:512], in_=po),
                             reads=[Rpo], writes=[Rysb[yb], Rpo])
                    else:
                        P.op("dve", lambda e, po=po, yb=yb: e.tensor_copy(out=ysb[yb][:, 512:1024], in_=po),
                             reads=[Rpo], writes=[Rysb[yb], Rpo])
                r0 = s_ * ST + a * 128
                P.store("sp", ysort_d[r0:r0 + 128, :], ysb[yb], Rysb[yb], Rysort)

        if stop_after < 4:
            P.barrier()
            self.off = mark
            return
        P.barrier()
        self.off = mark4
        NB4 = 8
        g_b, Rg = self.bcast_load(lng_d[layer:layer + 1, :], D, "sp")
        b_b, Rb_ = self.bcast_load(lnb_d[layer:layer + 1, :], D, "sp")
        Rgb = P.R("sp_gb")
        dmy = self.sb([128, 8], F32)
        P.op("pool", lambda e: e.tensor_copy(out=dmy[:, 7:8], in_=g_b[:, 0:1]), reads=[Rg, Rb_], writes=[Rgb])
        st = [self.sb([128, 2, 6], F32) for _ in range(NB4)]
        mv = [self.sb([128, 8], F32) for _ in range(NB4)]
        Rs = P.Rs("sp_ln", NB4)
        acc = [self.sb([128, D], F32) for _ in range(NB4)]
        Racc = P.Rs("sp_acc", NB4)
        yg = [[self.sb([128, D], F32) for _ in range(NB4)] for _ in range(2)]
        Ryg = [P.Rs("sp_yg%d" % k, NB4) for k in range(2)]
        xTo = [self.sb([128, KC, 128], BF16) for _ in range(NB4)]
        RxTo = P.Rs("sp_xTo", NB4)

        def fetch4(t):
            b = t % NB4
            P.dma("sp", acc[b], x_d[t * 128:(t + 1) * 128, :], reads=Rin, writes=[Racc[b]])
            for k in range(2):
                i = Ins("pool", (lambda e, k=k: e.indirect_dma_start(
                    out=yg[k][b], out_offset=None, in_=ysort_d[:, :],
                    in_offset=bass.IndirectOffsetOnAxis(ap=pos_i[:, k, t:t + 1], axis=0))),
                    (Rysort, Rr_), (Ryg[k][b],), True)
                i.key = Ryg[k][b]
                i.phase = P.phase
                P.ins.append(i)

        def comb4(t):
            b = t % NB4
            for k in range(2):
                P.op("act", lambda e, k=k: e.activation(out=yg[k][b], in_=yg[k][b], func=AF.Identity,
                                                        scale=posg[:, 2 + k, t:t + 1]),
                     reads=[Ryg[k][b], Rr_], writes=[Ryg[k][b]])
            P.op("pool", lambda e: e.tensor_tensor(out=yg[0][b], in0=yg[0][b], in1=yg[1][b], op=ALU.add),
                 reads=[Ryg[0][b], Ryg[1][b]], writes=[Ryg[0][b]])
            P.op("dve", lambda e: e.scalar_tensor_tensor(out=acc[b], in0=acc[b], scalar=ALPHA, in1=yg[0][b],
                                                         op0=ALU.mult, op1=ALU.add),
                 reads=[Ryg[0][b], Racc[b]], writes=[Racc[b]])
            self.layer_norm(acc[b], Racc[b], st[b], mv[b], Rs[b], g_b, b_b, Rgb, eng2="dve")
            if outT_d is not None:
                self.make_xT(acc[b], Racc[b], 2 * (b % 4), xTo[b], RxTo[b])
            P.store("sp", out_d[t * 128:(t + 1) * 128, :], acc[b], Racc[b], Rout)
            if outT_d is not None:
                P.store("sp", outT_d[t], xTo[b], RxTo[b], RoutT)

        GS = 4
        for t in range(min(GS, NT)):
            fetch4(t)
        for t0 in range(0, NT, GS):
            ts = list(range(t0, min(NT, t0 + GS)))
            for t in ts:
                if t + GS < NT:
                    fetch4(t + GS)
            ms = []
            for t in ts:
                ms.append(P.mark())
                comb4(t)
            if len(ts) == 4:
                P.interleave(ms[2], ms[3])
                tail = P.ins[ms[2]:]
                del P.ins[ms[2]:]
                P.interleave(ms[0], ms[1])
                mid = len(P.ins)
                P.ins.extend(tail)
                P.interleave(ms[0], mid)
            elif len(ts) >= 2:
                P.interleave(ms[0], ms[1])
        P.barrier()
        self.off = mark


W_SHAPES = {
    "ssm_w_in": [1, D, SSD_IN], "ssm_conv_w": [1, 4, 3072], "ssm_conv_b": [1, 3072], "ssm_dt_bias": [1, NH],
    "ssm_a_log": [1, NH], "ssm_d": [1, NH], "ssm_norm_w": [1, D_INNER], "ssm_w_out": [1, D_INNER, D],
    "att_w_qkv": [1, D, 3 * D], "att_b_qkv": [1, 3 * D], "att_rel_bias": [1, 257, AH], "att_w_o": [1, D, D],
    "att_b_o": [1, D], "router_w": [D, NE], "router_bias": [NE],
    "moe_w_gate": [DEPTH, NE, D, DFF], "moe_w_up": [DEPTH, NE, D, DFF], "moe_w_down": [DEPTH, NE, DFF, D],
    "ln_mix_g": [DEPTH, D], "ln_mix_b": [DEPTH, D], "ln_ffn_g": [DEPTH, D], "ln_ffn_b": [DEPTH, D],
}


def build_full(T=4096, debug=False, sparse=True):
    B = Builder(T)
    NT = B.NT
    P = B.P
    x_d = B.dt_in("x", [T, D])
    w = {k: B.dt_in(k, v) for k, v in W_SHAPES.items()}
    scr = lambda n, shp, dt=F32: B.dt_scr(n, shp, dt, debug=debug)
    xT0 = scr("xT0", [NT, 128, KC, 128], BF16)
    sz = scr("sz", [NT, 128, D_INNER], BF16)
    dt = scr("dt", [128, NT, 5, NH], F32)
    xs = scr("xs", [NT, 128, D_INNER], BF16)
    bt = scr("bt", [NT, 128, 512], BF16)
    bct = scr("bct", [NT, 128, 8, 128], BF16)
    x1 = scr("x1", [T, D])
    x1T = scr("x1T", [NT, 128, KC, 128], BF16)
    c1 = scr("comb1", [NT, 128, NE])
    x2 = scr("x2", [T, D])
    x2T = scr("x2T", [NT, 128, KC, 128], BF16)
    x3 = scr("x3", [T, D])
    x3T = scr("x3T", [NT, 128, KC, 128], BF16)
    c3 = scr("comb3", [NT, 128, NE])
    y = B.dt_out("y", [T, D])
    R = {n: P.R("d_" + n) for n in "xT0 sz dt xs bt bct x1 x1T c1 x2 x2T x3 x3T c3 y".split()}
    frep = B.dt_scr("frep", [AH, 128, 768], F32)
    B.consts()
    B.router_setup(w["router_w"], w["router_bias"])
    NSLOT = ((2 * T) // 512 + NE) * 512
    xsort = [B.dt_scr("xsort%d" % l, [NSLOT, D], BF16) for l in range(2)]
    ysort = B.dt_scr("ysort", [NSLOT, D], F32)
    Rz = [P.R("zero%d" % l) for l in range(2)]
    expb = B.dt_scr("expb", [128, AH, 5, 128], BF16)
    Rexpb = P.R("d_expb")
    wbs = [(B.dt_scr("wgb%d" % l, [NE * 128, 4096], BF16), B.dt_scr("wub%d" % l, [NE * 128, 4096], BF16),
            B.dt_scr("wdb%d" % l, [NE * DFF, D], BF16)) for l in range(2)]
    Rwbs = [P.R("d_wb%d" % l) for l in range(2)]
    mkconv = lambda l, rng=None, nb=2: (lambda: B.wconv_steps(l, w["moe_w_gate"], w["moe_w_up"], w["moe_w_down"],
                                                           wbs[l], Rwbs[l], ex_range=rng, nbuf=nb))
    B.phase_a0z(x_d, w["ssm_w_in"], w["ssm_dt_bias"], w["ssm_a_log"], xT0, R["xT0"], sz, R["sz"], dt, R["dt"],
                early=lambda: B.attn_bias_setup(w["att_rel_bias"], frep, expb, Rexpb),
                wconv=(mkconv(0, None, 4) if sparse else None))
    B.phase_a0x(w["ssm_w_in"], w["ssm_conv_w"], w["ssm_conv_b"], xT0, R["xT0"], xs, R["xs"], bt, R["bt"], bct,
                R["bct"], zfill=([(xsort[0], Rz[0]), (xsort[1], Rz[1])] if sparse else ()),
                wconv=None)
    B.phase_b0(x_d, sz, dt, xs, bt, bct, [R["sz"], R["dt"], R["xs"], R["bt"], R["bct"]], w["ssm_a_log"], w["ssm_d"],
               w["ssm_norm_w"], w["ssm_w_out"], w["ln_mix_g"], w["ln_mix_b"], x1, R["x1"], x1T, R["x1T"], c1, R["c1"])
    if sparse:
        B.phase_moe_sparse(0, x1, c1, [R["x1"], R["c1"]], w["moe_w_gate"], w["moe_w_up"], w["moe_w_down"],
                           w["ln_ffn_g"], w["ln_ffn_b"], x2, R["x2"], x2T, R["x2T"], xsort[0], ysort, Rz[0],
                           wb=wbs[0], Rwb=Rwbs[0])
    else:
        B.phase_moe(0, x1, x1T, c1, [R["x1"], R["x1T"], R["c1"]], w["moe_w_gate"], w["moe_w_up"], w["moe_w_down"],
                    w["ln_ffn_g"], w["ln_ffn_b"], x2, R["x2"], x2T, R["x2T"])
    B.phase_attn(x2, x2T, [R["x2"], R["x2T"]], w["att_w_qkv"], w["att_b_qkv"], w["att_w_o"], w["att_b_o"],
                 expb, Rexpb, w["ln_mix_g"], w["ln_mix_b"], x3, R["x3"], x3T, R["x3T"], c3, R["c3"],
                 wconv=(mkconv(1) if sparse else None))
    if sparse:
        B.phase_moe_sparse(1, x3, c3, [R["x3"], R["c3"]], w["moe_w_gate"], w["moe_w_up"], w["moe_w_down"],
                           w["ln_ffn_g"], w["ln_ffn_b"], y, R["y"], None, None, xsort[1], ysort, Rz[1],
                           wb=wbs[1], Rwb=Rwbs[1])
    else:
        B.phase_moe(1, x3, x3T, c3, [R["x3"], R["x3T"], R["c3"]], w["moe_w_gate"], w["moe_w_up"], w["moe_w_down"],
                    w["ln_ffn_g"], w["ln_ffn_b"], y, R["y"])
    fw = [R["y"]]
    if debug:
        fw = list(R.values())
    P.emit(final_wait=fw)
    B.es.close()
    return B


def kernel(**inputs):
    x = np.ascontiguousarray(np.asarray(inputs["x"], dtype=np.float32))
    nb, T, _ = x.shape
    B = build_full(T)
    ws = {k: np.ascontiguousarray(np.asarray(inputs[k], dtype=np.float32)) for k in W_SHAPES}
    in_maps = []
    for c in range(nb):
        m = {"x": x[c]}
        m.update(ws)
        in_maps.append(m)
    res = run_bass_kernel_spmd(B.nc, in_maps, core_ids=list(range(nb)))
    return np.stack([np.asarray(r["y"], dtype=np.float32) for r in res.results], axis=0)
```

```python
import numpy as np
from contextlib import ExitStack
import concourse.bass as bass
import concourse.mybir as mybir
from concourse.bass_utils import run_bass_kernel_spmd

F32 = mybir.dt.float32
BF16 = mybir.dt.bfloat16
U8 = mybir.dt.uint8
AF = mybir.ActivationFunctionType
ALU = mybir.AluOpType
AX = mybir.AxisListType

ENGS = ("pe", "act", "dve", "pool", "sp")

D = 1024
KC = 8
DEPTH = 2
ALPHA = (2.0 * DEPTH) ** 0.25
LN_EPS = 1e-5
RMS_EPS = 1e-5
NE = 16
DFF = 512
D_INNER = 2048
NH = 32
NG = 4
NST = 128
SSD_IN = 5152
AH = 16
AD = 64


class Res:
    __slots__ = ("name", "lw", "rd", "dcount", "dsem")

    def __init__(self, name):
        self.name = name
        self.lw = []
        self.rd = []
        self.dcount = 0
        self.dsem = None


class Ins:
    __slots__ = ("eng", "fn", "reads", "writes", "dma", "deps", "needed", "sem", "val", "clock", "pseudo", "key",
                 "phase")

    def __init__(self, eng, fn, reads, writes, dma, pseudo=False):
        self.eng = eng
        self.fn = fn
        self.reads = reads
        self.writes = writes
        self.dma = dma
        self.deps = []
        self.needed = False
        self.sem = None
        self.val = 0
        self.clock = None
        self.pseudo = pseudo


class Phys:
    __slots__ = ("count", "dsem")

    def __init__(self):
        self.count = 0
        self.dsem = None


class Prog:
    def __init__(self, nc):
        self.nc = nc
        self.ins = []
        self.res = []
        self.phase = 0

    def R(self, name):
        r = Res(name)
        self.res.append(r)
        return r

    def Rs(self, name, n):
        return [self.R("%s%d" % (name, i)) for i in range(n)]

    def op(self, eng, fn, reads=(), writes=()):
        i = Ins(eng, fn, tuple(reads), tuple(writes), False)
        i.phase = self.phase
        self.ins.append(i)
        return i

    def dma(self, eng, out, in_, reads=(), writes=(), key=None):
        assert len(writes) == 1
        i = Ins(eng, (lambda e, o=out, s=in_: e.dma_start(out=o, in_=s)), tuple(reads), tuple(writes), True)
        i.key = key if key is not None else writes[0]
        i.phase = self.phase
        self.ins.append(i)
        return i

    def store(self, eng, out, in_, src, dst, extra_reads=()):
        return self.dma(eng, out, in_, reads=[src] + list(extra_reads), writes=[dst], key=src)

    def mark(self):
        return len(self.ins)

    def interleave(self, start, mid):
        A = self.ins[start:mid]
        Bq = self.ins[mid:]
        if not A or not Bq:
            return
        keyed = [((i + 0.5) / len(A), 0, i, x) for i, x in enumerate(A)] + \
                [((j + 0.5) / len(Bq), 1, j, x) for j, x in enumerate(Bq)]
        keyed.sort(key=lambda z: (z[0], z[1], z[2]))
        self.ins[start:] = [z[3] for z in keyed]

    def barrier(self):
        allr = tuple(self.res)
        for e in ENGS:
            i = Ins(e, None, (), allr, False, pseudo=True)
            i.phase = self.phase
            self.ins.append(i)
        self.phase += 1

    def _analyse(self, final_wait):
        fin = Ins("sp", None, tuple(final_wait), (), False, pseudo=True)
        fin.phase = self.phase
        self.ins.append(fin)
        for i in self.ins:
            deps = []
            for r in i.reads:
                deps.extend(r.lw)
            par = {}
            for w in i.writes:
                p = bool(i.dma and w.lw and all(x.dma for x in w.lw) and not w.rd)
                par[id(w)] = p
                if not p:
                    deps.extend(w.lw)
                deps.extend(w.rd)
            seen = set()
            for d in deps:
                if d is i or id(d) in seen:
                    continue
                seen.add(id(d))
                if (not d.dma) and (not i.dma) and d.eng == "pe" and i.eng == "pe" and not i.pseudo:
                    continue
                i.deps.append(d)
                d.needed = True
            if i.pseudo:
                continue
            for r in i.reads:
                r.rd.append(i)
            for w in i.writes:
                if par[id(w)]:
                    w.lw = w.lw + [i]
                else:
                    w.lw = [i]
                w.rd = []
        cnt = {e: 0 for e in ENGS}
        dres = []
        free = []
        live = []
        cur_phase = 0
        for i in self.ins:
            if i.phase != cur_phase:
                free.extend(p for (_, p) in live)
                live = []
                cur_phase = i.phase
            if i.pseudo:
                continue
            if i.dma:
                w = i.key
                if w.dsem is None or w.dsem[0] != cur_phase:
                    if i.eng == "pool":
                        ph = Phys()
                        dres.append(ph)
                    else:
                        if free:
                            ph = free.pop()
                        else:
                            ph = Phys()
                            dres.append(ph)
                        live.append((cur_phase, ph))
                    w.dsem = (cur_phase, ph)
                ph = w.dsem[1]
                ph.count += 16
                i.sem = ph
                i.val = ph.count
            elif i.needed:
                cnt[i.eng] += 1
                i.sem = i.eng
                i.val = cnt[i.eng]
        seen = {e: {} for e in ENGS}
        per_eng = {e: [] for e in ENGS}
        nwaits = 0
        for i in self.ins:
            s = seen[i.eng]
            waits = {}
            for d in i.deps:
                kid = id(d.sem) if d.dma else d.sem
                if s.get(kid, 0) >= d.val:
                    continue
                if kid not in waits or waits[kid][1] < d.val:
                    waits[kid] = (d.sem, d.val)
            for d in i.deps:
                if d.clock:
                    for k, v in d.clock.items():
                        if s.get(k, 0) < v:
                            s[k] = v
            for kid, (key, v) in waits.items():
                if s.get(kid, 0) < v:
                    s[kid] = v
            wl = list(waits.values())
            nwaits += len(wl)
            if i.sem is not None:
                c = dict(s)
                c[id(i.sem) if i.dma else i.sem] = i.val
                i.clock = c
            per_eng[i.eng].append((i, wl))
        self.stats = dict(n_ins=len(self.ins), n_waits=nwaits, n_dma_sems=len(dres),
                          per_eng={e: len(v) for e, v in per_eng.items()})
        return per_eng, dres

    def emit(self, final_wait=()):
        nc = self.nc
        per_eng, dres = self._analyse(final_wait)
        with ExitStack() as es:
            esem = {e: es.enter_context(nc.semaphore("S_" + e)) for e in ENGS}
            for k, r in enumerate(dres):
                r.dsem = es.enter_context(nc.semaphore("D%d" % k))
            block = es.enter_context(nc.Block())

            def run(e, name):
                for (i, wl) in per_eng[name]:
                    for (key, v) in wl:
                        sem = esem[key] if isinstance(key, str) else key.dsem
                        e.wait_ge(sem, v)
                    if i.fn is None:
                        continue
                    bi = i.fn(e)
                    if i.dma:
                        bi.then_inc(i.sem.dsem, 16)
                    elif i.needed:
                        bi.then_inc(esem[name], 1)

            @block.tensor
            def _(e):
                run(e, "pe")

            @block.scalar
            def _(e):
                run(e, "act")

            @block.vector
            def _(e):
                run(e, "dve")

            @block.gpsimd
            def _(e):
                run(e, "pool")

            @block.sync
            def _(e):
                run(e, "sp")
        return nc


ARENA_BYTES = 210944


def _sz(dt):
    return 4 if dt in (F32, mybir.dt.int32, mybir.dt.uint32) else (2 if dt == BF16 else 1)


class Builder:
    def __init__(self, T=4096):
        self.T = T
        self.NT = T // 128
        self.nc = bass.Bass("TRN2", target_bir_lowering=False)
        self.P = Prog(self.nc)
        self.es = ExitStack()
        nc = self.nc
        self.arena = self.es.enter_context(nc.sbuf_tensor("arena", [128, ARENA_BYTES], U8))
        self.off = 0
        self.pairs = [self.es.enter_context(nc.psum_tensor("pair%d" % i, [128, 1024], F32)) for i in range(4)]
        self.Rbank = self.P.Rs("bank", 8)
        self.dram = {}
        self._rr = 0

    def sb(self, shape, dt):
        n = int(np.prod(shape[1:])) * _sz(dt)
        n_al = (n + 63) // 64 * 64
        off = self.off
        assert off + n_al <= ARENA_BYTES, ("SBUF arena overflow", off, n_al)
        self.off += n_al
        v = self.arena[0:shape[0], off:off + n].bitcast(dt)
        if len(shape) == 3:
            v = v.rearrange("p (a b) -> p a b", a=shape[1])
        elif len(shape) == 4:
            v = v.rearrange("p (a b c) -> p a b c", a=shape[1], b=shape[2])
        elif len(shape) == 5:
            v = v.rearrange("p (a b c d) -> p a b c d", a=shape[1], b=shape[2], c=shape[3])
        return v

    def bank(self, i, dt=F32):
        b = self.pairs[i // 2][:, (i % 2) * 512:(i % 2 + 1) * 512]
        if dt != F32:
            b = b.bitcast(dt)
        return b

    def pair(self, i):
        return self.pairs[i][:]

    def dt_in(self, name, shape, dt=F32):
        t = self.nc.dram_tensor(name, list(shape), dt, kind="ExternalInput").ap()
        self.dram[name] = t
        return t

    def dt_out(self, name, shape, dt=F32):
        t = self.nc.dram_tensor(name, list(shape), dt, kind="ExternalOutput").ap()
        self.dram[name] = t
        return t

    def dt_scr(self, name, shape, dt=F32, debug=False):
        t = self.nc.dram_tensor(name, list(shape), dt, kind=("ExternalOutput" if debug else "Internal")).ap()
        self.dram[name] = t
        return t

    def dq(self):
        self._rr += 1
        return ("sp", "act")[self._rr % 2]

    def consts(self):
        P = self.P
        self.identF = self.sb([128, 128], F32)
        self.identB = self.sb([128, 128], BF16)
        self.maskU = self.sb([128, 128], F32)
        self.maskSL = self.sb([128, 128], F32)
        self.onesF = self.sb([128, 128], F32)
        self.onesB = self.sb([128, 128], BF16)
        self.Rc = P.R("consts")
        Rc = self.Rc
        iF, iB, mU, mS, oF, oB = self.identF, self.identB, self.maskU, self.maskSL, self.onesF, self.onesB
        P.op("pool", lambda e: e.memset(iF, 0.0), writes=[Rc])
        P.op("pool", lambda e: e.affine_select(out=iF, in_=iF, pattern=[[-1, 128]], compare_op=ALU.not_equal,
                                               fill=1.0, base=0, channel_multiplier=1), reads=[Rc], writes=[Rc])
        P.op("pool", lambda e: e.tensor_copy(out=iB, in_=iF), reads=[Rc], writes=[Rc])
        P.op("pool", lambda e: e.memset(oF, 1.0), reads=[Rc], writes=[Rc])
        P.op("pool", lambda e: e.memset(oB, 1.0), reads=[Rc], writes=[Rc])
        P.op("pool", lambda e: e.affine_select(out=mU, in_=oF, pattern=[[1, 128]], compare_op=ALU.is_ge,
                                               fill=0.0, base=0, channel_multiplier=-1), reads=[Rc], writes=[Rc])
        P.op("pool", lambda e: e.affine_select(out=mS, in_=oF, pattern=[[-1, 128]], compare_op=ALU.is_gt,
                                               fill=0.0, base=0, channel_multiplier=1), reads=[Rc], writes=[Rc])

    def wconv_steps(self, layer, wg_d, wu_d, wd_d, wb, Rwb, ex_range=None, nbuf=2):
        P = self.P
        bnc = [self.sb([128, 4096], BF16) for _ in range(nbuf)]
        Rbn = P.Rs("bnc", nbuf)
        RbnS = P.Rs("bncS", nbuf)
        n = 0
        pend = [None]

        def flush():
            if pend[0] is not None:
                b_, dst_ = pend[0]
                P.dma("pool", dst_, bnc[b_], reads=[Rbn[b_]], writes=[Rwb], key=RbnS[b_])
                pend[0] = None

        for ex in (ex_range if ex_range is not None else range(NE)):
            for which in range(3):
                b = n % nbuf
                n += 1
                if which < 2:
                    src = (wg_d, wu_d)[which][layer, ex].rearrange("(p k) f -> p (k f)", k=KC)
                    dst = wb[which][ex * 128:(ex + 1) * 128, :]
                else:
                    src = wd_d[layer, ex].rearrange("(p k) n -> p (k n)", k=4)
                    dst = wb[2][ex * DFF:(ex + 1) * DFF, :].rearrange("(p k) n -> p (k n)", k=4)

                def step(b=b, src=src, dst=dst):
                    P.dma("pool", bnc[b], src, writes=[Rbn[b]])
                    flush()
                    pend[0] = (b, dst)
                yield step
        yield flush

    def _bc_reg(self, e):
        if getattr(self, "_bcr", None) is None:
            self._bcr = e.to_reg(NE * 128 - 1)
        return self._bcr

    def _bc_reg2(self, e):
        if getattr(self, "_bcr2", None) is None:
            self._bcr2 = e.to_reg(NE * DFF - 1)
        return self._bcr2

    def zero_fill(self, dram2d, Rz):
        P = self.P
        if getattr(self, "zt", None) is None:
            self.zt = self.sb([128, 4, D], BF16)
            self.Rzt = P.R("zt")
            P.op("pool", lambda e, zt=self.zt: e.memset(zt, 0.0), writes=[self.Rzt])
        n = dram2d.shape[0] // 512
        for i in range(n):
            P.dma(self.dq(), dram2d[i * 512:(i + 1) * 512, :].rearrange("(a p) d -> p a d", p=128), self.zt,
                  reads=[self.Rzt], writes=[Rz], key=self.Rzt)

    def bcast_load(self, dram_row, n, eng="sp"):
        t = self.sb([128, n], F32)
        r = self.P.R("bc")
        self.P.dma(eng, t, dram_row.partition_broadcast(128), writes=[r])
        return t, r

    def layer_norm(self, xt, Rx, st, mv, Rs, g_b, b_b, Rgb, eng2="pool"):
        P = self.P
        for c in range(2):
            P.op("dve", lambda e, c=c: e.bn_stats(out=st[:, c, :], in_=xt[:, c * 512:(c + 1) * 512]),
                 reads=[Rx], writes=[Rs])
        P.op("dve", lambda e: e.bn_aggr(out=mv[:, 0:2], in_=st), reads=[Rs], writes=[Rs])
        P.op("dve", lambda e: e.tensor_scalar(out=mv[:, 2:3], in0=mv[:, 1:2], scalar1=LN_EPS, scalar2=None,
                                              op0=ALU.add), reads=[Rs], writes=[Rs])
        P.op("act", lambda e: e.activation(out=mv[:, 3:4], in_=mv[:, 2:3], func=AF.Sqrt), reads=[Rs], writes=[Rs])
        P.op("dve", lambda e: e.reciprocal(out=mv[:, 4:5], in_=mv[:, 3:4]), reads=[Rs], writes=[Rs])
        P.op("dve", lambda e: e.scalar_tensor_tensor(out=mv[:, 5:6], in0=mv[:, 0:1], scalar=-1.0, in1=mv[:, 4:5],
                                                     op0=ALU.mult, op1=ALU.mult), reads=[Rs], writes=[Rs])
        P.op("act", lambda e: e.activation(out=xt, in_=xt, func=AF.Identity, bias=mv[:, 5:6], scale=mv[:, 4:5]),
             reads=[Rs, Rx], writes=[Rx])
        P.op("dve", lambda e: e.tensor_tensor(out=xt, in0=xt, in1=g_b, op=ALU.mult), reads=[Rx, Rgb], writes=[Rx])
        P.op(eng2, lambda e: e.tensor_tensor(out=xt, in0=xt, in1=b_b, op=ALU.add), reads=[Rx, Rgb], writes=[Rx])

    def make_xT(self, xt, Rx, bk, xTb, RxT, xTf=None, RxTf=None):
        P = self.P
        iF = self.identF
        for h in range(2):
            pb = self.bank(bk + h)
            Rb = self.Rbank[bk + h]
            for k in range(4):
                kk = h * 4 + k
                P.op("pe", lambda e, pb=pb, k=k, kk=kk: e.transpose(pb[:, k * 128:(k + 1) * 128],
                                                                    xt[:, kk * 128:(kk + 1) * 128], iF),
                     reads=[Rx, self.Rc], writes=[Rb])
            P.op("act", lambda e, pb=pb, h=h: e.copy(out=xTb[:, h * 4:(h + 1) * 4, :],
                                                     in_=pb.rearrange("p (a b) -> p a b", a=4)),
                 reads=[Rb], writes=[RxT, Rb])
            if xTf is not None:
                P.op("dve", lambda e, pb=pb, h=h: e.tensor_copy(out=xTf[:, h * 4:(h + 1) * 4, :],
                                                                in_=pb.rearrange("p (a b) -> p a b", a=4)),
                     reads=[Rb], writes=[RxTf, Rb])

    def router_setup(self, router_w, router_bias):
        P = self.P
        self.rw = self.sb([128, KC, NE], F32)
        self.Rrw = P.R("rw")
        P.dma("sp", self.rw, router_w.rearrange("(k p) e -> p k e", p=128), writes=[self.Rrw])
        self.rb_b, self.Rrb = self.bcast_load(router_bias.rearrange("(o e) -> o e", o=1), NE)
        self.rt = self.sb([128, 160], F32)
        self.Rrt = P.R("rt")

    def router(self, xTf, RxTf, bk, comb, Rcomb):
        P = self.P
        pb = self.bank(bk)
        Rb = self.Rbank[bk]
        rw = self.rw
        for k in range(KC):
            P.op("pe", lambda e, k=k: e.matmul(pb[:, 0:NE], xTf[:, k, :], rw[:, k, :], start=(k == 0),
                                                stop=(k == KC - 1)), reads=[RxTf, self.Rrw], writes=[Rb])
        P.op("dve", lambda e: e.tensor_copy(out=comb, in_=pb[:, 0:NE]), reads=[Rb], writes=[Rcomb, Rb])

    def route_batched(self, lg, sc, w1, w2, w3, sm4, d):
        P = self.P
        NT = self.NT
        f = lambda a: a.rearrange("p t e -> p (t e)")
        v4 = lambda a: a.rearrange("p t (g j) -> p t g j", g=4)
        be = lambda a: a.unsqueeze(2).to_broadcast([128, NT, NE])
        mx, ssum, rs, gmx, gsum = sm4[:, 0, :], sm4[:, 1, :], sm4[:, 2, :], sm4[:, 3, :], sm4[:, 4, :]
        m1 = w3[:, :, 0:4]
        m2 = w3[:, :, 4:8]
        gs = w3[:, :, 8:12]
        gm = w3[:, :, 12:16]
        b4 = lambda a: a.unsqueeze(3).to_broadcast([128, NT, 4, 4])
        d(lambda e: e.tensor_reduce(out=mx, in_=lg, axis=AX.X, op=ALU.max))
        d(lambda e: e.tensor_tensor(out=sc, in0=lg, in1=be(mx), op=ALU.subtract))
        P.op("act", lambda e: e.activation(out=f(sc), in_=f(sc), func=AF.Exp), reads=[self._Rroute],
             writes=[self._Rroute])
        d(lambda e: e.tensor_reduce(out=ssum, in_=sc, axis=AX.X, op=ALU.add))
        d(lambda e: e.reciprocal(out=rs, in_=ssum))
        d(lambda e: e.tensor_tensor(out=sc, in0=sc, in1=be(rs), op=ALU.mult))
        d(lambda e: e.tensor_tensor(out=w1, in0=sc, in1=self.rb_b.unsqueeze(1).to_broadcast([128, NT, NE]),
                                    op=ALU.add), rd=[self.Rrb])
        d(lambda e: e.tensor_reduce(out=m1, in_=v4(w1), axis=AX.X, op=ALU.max))
        d(lambda e: e.tensor_tensor(out=v4(w2), in0=v4(w1), in1=b4(m1), op=ALU.is_equal))
        d(lambda e: e.scalar_tensor_tensor(out=f(w1), in0=f(w2), scalar=-1e9, in1=f(w1), op0=ALU.mult,
                                           op1=ALU.add))
        d(lambda e: e.tensor_reduce(out=m2, in_=v4(w1), axis=AX.X, op=ALU.max))
        d(lambda e: e.tensor_tensor(out=v4(lg), in0=v4(w1), in1=b4(m2), op=ALU.is_equal))
        d(lambda e: e.tensor_tensor(out=gs, in0=m1, in1=m2, op=ALU.add))
        d(lambda e: e.tensor_reduce(out=gmx, in_=gs, axis=AX.X, op=ALU.max))
        d(lambda e: e.tensor_tensor(out=gm, in0=gs, in1=gmx.unsqueeze(2).to_broadcast([128, NT, 4]),
                                    op=ALU.is_equal))
        d(lambda e: e.tensor_tensor(out=w2, in0=w2, in1=lg, op=ALU.add))
        d(lambda e: e.tensor_tensor(out=v4(w2), in0=v4(w2), in1=b4(gm), op=ALU.mult))
        d(lambda e: e.tensor_tensor(out=w2, in0=w2, in1=sc, op=ALU.mult))
        d(lambda e: e.tensor_reduce(out=gsum, in_=w2, axis=AX.X, op=ALU.add))
        d(lambda e: e.reciprocal(out=rs, in_=gsum))
        d(lambda e: e.tensor_tensor(out=lg, in0=w2, in1=be(rs), op=ALU.mult))

    def phase_pre(self, x_d, xT_d, comb_d, RxT_d, Rcomb_d):
        P = self.P
        mark = self.off
        xt = [self.sb([128, D], F32) for _ in range(2)]
        Rx = P.Rs("pre_x", 2)
        xTb = [self.sb([128, KC, 128], BF16) for _ in range(2)]
        RxTb = P.Rs("pre_xTb", 2)
        xTf = [self.sb([128, KC, 128], F32) for _ in range(2)]
        RxTf = P.Rs("pre_xTf", 2)
        cb = [self.sb([128, NE], F32) for _ in range(2)]
        Rcb = P.Rs("pre_cb", 2)
        for t in range(self.NT):
            b = t % 2
            P.dma("sp", xt[b], x_d[t * 128:(t + 1) * 128, :], writes=[Rx[b]])
            self.make_xT(xt[b], Rx[b], 0 + 2 * b, xTb[b], RxTb[b], xTf[b], RxTf[b])
            self.router(xTf[b], RxTf[b], 4 + b, cb[b], Rcb[b])
            P.store("act", xT_d[t], xTb[b], RxTb[b], RxT_d)
            P.store("act", comb_d[t], cb[b], Rcb[b], Rcomb_d)
        P.barrier()
        self.off = mark

    def phase_moe(self, layer, x_d, xT_d, comb_d, Rin, wg_d, wu_d, wd_d, lng_d, lnb_d,
                  out_d, Rout, outT_d=None, RoutT=None):
        P = self.P
        T, NT = self.T, self.NT
        mark = self.off
        HT = min(16, NT)
        NHALF = NT // HT
        NBLK = HT // 4
        acc = self.sb([128, HT, D], F32)
        Racc = P.Rs("acc", HT)
        xT = self.sb([128, HT, KC, 128], BF16)
        RxT = P.R("moe_xT")
        comb = self.sb([128, HT, NE], F32)
        Rcomb = P.R("moe_comb")
        wg = [self.sb([128, KC, DFF], BF16) for _ in range(2)]
        wu = [self.sb([128, KC, DFF], BF16) for _ in range(2)]
        wd = [self.sb([128, 4, D], BF16) for _ in range(2)]
        Rwg = P.Rs("wg", 2)
        Rwu = P.Rs("wu", 2)
        Rwd = P.Rs("wd", 2)
        sg = [self.sb([128, 512], F32) for _ in range(2)]
        Rsg = P.Rs("sg", 2)
        hT = [self.sb([128, 4, 512], BF16) for _ in range(2)]
        RhT = P.Rs("hT", 2)
        g_b, Rg = self.bcast_load(lng_d[layer:layer + 1, :], D, "sp")
        b_b, Rb_ = self.bcast_load(lnb_d[layer:layer + 1, :], D, "act")
        st = self.sb([128, 2, 6], F32)
        mv = self.sb([128, 8], F32)
        Rs = P.R("moe_ln")
        xTo = [self.sb([128, KC, 128], BF16) for _ in range(2)]
        RxTo = P.Rs("moe_xTo", 2)
        Rgb = P.R("moe_gb")
        P.op("pool", lambda e: e.tensor_copy(out=mv[:, 7:8], in_=g_b[:, 0:1]), reads=[Rg, Rb_], writes=[Rgb])

        it = 0
        for half in range(NHALF):
            t0 = half * HT
            for j in range(HT):
                P.dma(self.dq(), acc[:, j, :], x_d[(t0 + j) * 128:(t0 + j + 1) * 128, :], reads=Rin,
                      writes=[Racc[j]])
                P.op("act", lambda e, j=j: e.mul(out=acc[:, j, :], in_=acc[:, j, :], mul=ALPHA),
                     reads=[Racc[j]], writes=[Racc[j]])
            P.dma("sp", xT, xT_d[t0:t0 + HT].rearrange("t p k c -> p t k c"), reads=Rin, writes=[RxT])
            P.dma("act", comb, comb_d[t0:t0 + HT].rearrange("t p e -> p t e"), reads=Rin, writes=[Rcomb])
            for ex in range(NE):
                wb = (half * NE + ex) % 2
                P.dma("pool", wg[wb], wg_d[layer, ex].rearrange("(k p) f -> p k f", p=128), writes=[Rwg[wb]])
                P.dma("pool", wu[wb], wu_d[layer, ex].rearrange("(k p) f -> p k f", p=128), writes=[Rwu[wb]])
                P.dma("pool", wd[wb], wd_d[layer, ex].rearrange("(k p) f -> p k f", p=128), writes=[Rwd[wb]])
                for blk in range(NBLK):
                    hb = it % 2
                    it += 1
                    rhs_x = lambda k, blk=blk: xT[:, blk * 4:(blk + 1) * 4, k, :]
                    for f in range(4):
                        pg = self.bank(f % 2)
                        Rpg = self.Rbank[f % 2]
                        pu = self.bank(2 + f % 2)
                        Rpu = self.Rbank[2 + f % 2]
                        sgb = f % 2
                        for k in range(KC):
                            P.op("pe", lambda e, pg=pg, k=k, f=f, wb=wb, rx=rhs_x: e.matmul(
                                pg, wg[wb][:, k, f * 128:(f + 1) * 128], rx(k), start=(k == 0), stop=(k == KC - 1)),
                                reads=[Rwg[wb], RxT], writes=[Rpg])
                        for k in range(KC):
                            P.op("pe", lambda e, pu=pu, k=k, f=f, wb=wb, rx=rhs_x: e.matmul(
                                pu, wu[wb][:, k, f * 128:(f + 1) * 128], rx(k), start=(k == 0), stop=(k == KC - 1)),
                                reads=[Rwu[wb], RxT], writes=[Rpu])
                        P.op("act", lambda e, pg=pg, sgb=sgb: e.activation(out=sg[sgb], in_=pg, func=AF.Silu),
                             reads=[Rpg], writes=[Rsg[sgb]])
                        P.op("dve", lambda e, pu=pu, sgb=sgb, hb=hb, f=f: e.tensor_tensor(
                            out=hT[hb][:, f, :], in0=pu, in1=sg[sgb], op=ALU.mult),
                            reads=[Rpu, Rsg[sgb]], writes=[RhT[hb]])
                    for s in range(4):
                        j = blk * 4 + s
                        for n in range(2):
                            bi = 4 + (s % 2) * 2 + n
                            po = self.bank(bi)
                            Rpo = self.Rbank[bi]
                            for f in range(4):
                                P.op("pe", lambda e, po=po, f=f, s=s, n=n, hb=hb, wb=wb: e.matmul(
                                    po, hT[hb][:, f, s * 128:(s + 1) * 128], wd[wb][:, f, n * 512:(n + 1) * 512],
                                    start=(f == 0), stop=(f == 3)), reads=[RhT[hb], Rwd[wb]], writes=[Rpo])
                            P.op("dve", lambda e, po=po, j=j, n=n, ex=ex: e.scalar_tensor_tensor(
                                out=acc[:, j, n * 512:(n + 1) * 512], in0=po, scalar=comb[:, j, ex:ex + 1],
                                in1=acc[:, j, n * 512:(n + 1) * 512], op0=ALU.mult, op1=ALU.add),
                                reads=[Rpo, Rcomb, Racc[j]], writes=[Racc[j]])
            for j in range(HT):
                t = t0 + j
                self.layer_norm(acc[:, j, :], Racc[j], st, mv, Rs, g_b, b_b, Rgb)
                P.store(self.dq(), out_d[t * 128:(t + 1) * 128, :], acc[:, j, :], Racc[j], Rout)
                if outT_d is not None:
                    b = j % 2
                    self.make_xT(acc[:, j, :], Racc[j], 0 + 2 * b, xTo[b], RxTo[b])
                    P.store(self.dq(), outT_d[t], xTo[b], RxTo[b], RoutT)
        P.barrier()
        self.off = mark


    def phase_a0z(self, x_d, win_d, dtb_d, alog_d, xT_d, RxT_d, sz_d, Rsz_d, dt_d, Rdt_d, early=None, wconv=None):
        P = self.P
        NT = self.NT
        mark = self.off
        wsteps = wconv() if wconv is not None else None
        winz = self.sb([128, KC, 2048], BF16)
        Rwz = P.Rs("winz", 4)
        for n in range(4):
            P.dma("pool", winz[:, :, n * 512:(n + 1) * 512],
                  win_d[0, :, n * 512:(n + 1) * 512].rearrange("(k p) n -> p k n", p=128), writes=[Rwz[n]])
        windt = self.sb([128, KC, 32], F32)
        Rwdt = P.R("windt")
        P.dma("sp", windt, win_d[0, :, 5120:5152].rearrange("(k p) n -> p k n", p=128), writes=[Rwdt])
        dtb_b, Rdtb = self.bcast_load(dtb_d[0:1, :], 32)
        vall = self.sb([128, NT, 32], F32)
        av = self.sb([128, NT, 32], F32)
        Rv = P.R("vall")
        xt = [self.sb([128, D], F32) for _ in range(2)]
        Rx = P.Rs("a0_x", 2)
        xTb = [self.sb([128, KC, 128], BF16) for _ in range(2)]
        RxTb = P.Rs("a0_xTb", 2)
        xTf = [self.sb([128, KC, 128], F32) for _ in range(2)]
        RxTf = P.Rs("a0_xTf", 2)
        sz = [self.sb([128, 2048], BF16) for _ in range(2)]
        Rsz = P.Rs("a0_sz", 2)
        for t in range(NT):
            b = t % 2
            if early is not None and t == min(3, NT - 1):
                early()
            if wsteps is not None and t >= 2:
                for _ in range(2 if t % 2 == 0 else 1):
                    st_ = next(wsteps, None)
                    if st_ is not None:
                        st_()
            P.dma("sp", xt[b], x_d[t * 128:(t + 1) * 128, :], writes=[Rx[b]])
            self.make_xT(xt[b], Rx[b], 2 * b, xTb[b], RxTb[b], xTf[b], RxTf[b])
            P.store("act", xT_d[t], xTb[b], RxTb[b], RxT_d)
            for n in range(4):
                bi = 4 + n % 2
                pz = self.bank(bi)
                Rpz = self.Rbank[bi]
                for k in range(KC):
                    P.op("pe", lambda e, pz=pz, k=k, n=n, b=b: e.matmul(
                        pz, xTb[b][:, k, :], winz[:, k, n * 512:(n + 1) * 512], start=(k == 0), stop=(k == KC - 1)),
                        reads=[RxTb[b], Rwz[n]], writes=[Rpz])
                P.op("act", lambda e, pz=pz, n=n, b=b: e.activation(out=sz[b][:, n * 512:(n + 1) * 512], in_=pz,
                                                                     func=AF.Silu),
                     reads=[Rpz], writes=[Rsz[b], Rpz])
            P.store(self.dq(), sz_d[t], sz[b], Rsz[b], Rsz_d)
            pdt = self.bank(6 + b)
            Rpdt = self.Rbank[6 + b]
            for k in range(KC):
                P.op("pe", lambda e, pdt=pdt, k=k, b=b: e.matmul(pdt[:, 0:32], xTf[b][:, k, :], windt[:, k, :],
                                                                  start=(k == 0), stop=(k == KC - 1)),
                     reads=[RxTf[b], Rwdt], writes=[Rpdt])
            P.op("dve", lambda e, pdt=pdt, t=t: e.tensor_tensor(out=vall[:, t, :], in0=pdt[:, 0:32], in1=dtb_b,
                                                                 op=ALU.add),
                 reads=[Rpdt, Rdtb], writes=[Rv, Rpdt])
        if wsteps is not None:
            for st_ in wsteps:
                st_()
        P.op("dve", lambda e: e.scalar_tensor_tensor(out=av, in0=vall, scalar=-1.0, in1=vall, op0=ALU.mult,
                                                     op1=ALU.max), reads=[Rv], writes=[Rv])
        P.op("act", lambda e: e.activation(out=av, in_=av, func=AF.Exp, scale=-1.0), reads=[Rv], writes=[Rv])
        P.op("act", lambda e: e.activation(out=av, in_=av, func=AF.Ln, bias=1.0), reads=[Rv], writes=[Rv])
        P.op("dve", lambda e: e.scalar_tensor_tensor(out=vall, in0=vall, scalar=0.0, in1=av, op0=ALU.max,
                                                     op1=ALU.add), reads=[Rv], writes=[Rv])
        a_b, Ra = self.bcast_load(alog_d[0:1, :], NH, "act")
        P.op("act", lambda e: e.activation(out=a_b, in_=a_b, func=AF.Exp), reads=[Ra], writes=[Ra])
        P.op("dve", lambda e: e.tensor_scalar(out=a_b, in0=a_b, scalar1=-1.0, scalar2=None, op0=ALU.mult),
             reads=[Ra], writes=[Ra])
        dtx = self.sb([128, NT, 5, NH], F32)
        cumt = self.sb([128, 2, NT, NH], F32)
        Rdx = P.R("dtx")
        NTH = NT * NH
        P.op("dve", lambda e: e.tensor_copy(out=dtx[:, :, 0, :], in_=vall), reads=[Rv], writes=[Rdx])
        P.op("dve", lambda e: e.tensor_tensor(out=av, in0=vall, in1=a_b.unsqueeze(1).to_broadcast([128, NT, NH]),
                                              op=ALU.mult), reads=[Rv, Ra], writes=[Rv])
        P.op("dve", lambda e: e.tensor_copy(out=dtx[:, :, 1, :], in_=av), reads=[Rv], writes=[Rdx])
        avf = av.rearrange("p t h -> p (t h)")
        for which, lhs in enumerate((self.maskU, self.onesF)):
            for c0 in range(0, NTH, 512):
                c1 = min(NTH, c0 + 512)
                bi = 4 + which
                pb = self.bank(bi)
                P.op("pe", lambda e, pb=pb, lhs=lhs, c0=c0, c1=c1: e.matmul(pb[:, 0:c1 - c0], lhs, avf[:, c0:c1],
                                                                           start=True, stop=True),
                     reads=[Rv, self.Rc], writes=[self.Rbank[bi]])
                P.op("dve", lambda e, pb=pb, which=which, c0=c0, c1=c1: e.tensor_copy(
                    out=cumt[:, which].rearrange("p t h -> p (t h)")[:, c0:c1], in_=pb[:, 0:c1 - c0]),
                    reads=[self.Rbank[bi]], writes=[Rdx, self.Rbank[bi]])
        P.op("act", lambda e: e.activation(out=dtx[:, :, 2, :], in_=cumt[:, 0], func=AF.Exp), reads=[Rdx],
             writes=[Rdx])
        P.op("act", lambda e: e.activation(out=dtx[:, :, 3, :], in_=cumt[:, 1], func=AF.Exp), reads=[Rdx],
             writes=[Rdx])
        P.op("dve", lambda e: e.tensor_tensor(out=cumt[:, 1], in0=cumt[:, 1], in1=cumt[:, 0], op=ALU.subtract),
             reads=[Rdx], writes=[Rdx])
        P.op("act", lambda e: e.activation(out=dtx[:, :, 4, :], in_=cumt[:, 1], func=AF.Exp), reads=[Rdx],
             writes=[Rdx])
        P.store("sp", dt_d, dtx, Rdx, Rdt_d)
        P.barrier()
        self.off = mark

    def phase_a0x(self, win_d, convw_d, convb_d, xT_d, RxT_d, xs_d, Rxs_d, bt_d, Rbt_d, bct_d, Rbct_d, zfill=(),
                  wconv=None):
        P = self.P
        self.zt = None
        wsteps = wconv() if wconv is not None else None
        NT = self.NT
        NB = NT // 4
        mark = self.off
        NCH = 24
        winx = self.sb([128, KC, 3072], BF16)
        Rwx = P.Rs("winx", 3)
        for j in range(6):
            P.dma("pool", winx[:, :, j * 512:(j + 1) * 512],
                  win_d[0, :, 2048 + j * 512:2048 + (j + 1) * 512].rearrange("(k p) n -> p k n", p=128),
                  writes=[Rwx[j // 2]])
        cw4 = self.sb([4, 3072], F32)
        cbrow = self.sb([1, 3072], F32)
        cbrowB = self.sb([1, 3072], BF16)
        cwT = self.sb([128, NCH, 4], F32)
        Rcw = P.R("convw")
        P.dma("sp", cw4, convw_d[0], writes=[Rcw])
        Rcb = P.R("convb")
        P.dma("act", cbrow, convb_d[0:1, :], writes=[Rcb])
        P.op("dve", lambda e: e.tensor_copy(out=cbrowB, in_=cbrow), reads=[Rcb], writes=[Rcb])
        pb0 = self.bank(0)
        for c in range(NCH):
            P.op("pe", lambda e, c=c: e.matmul(pb0[:, c * 4:(c + 1) * 4], cw4[0:4, c * 128:(c + 1) * 128],
                                                self.identF[0:4, 0:4], start=True, stop=True),
                 reads=[Rcw, self.Rc], writes=[self.Rbank[0]])
        P.op("dve", lambda e: e.tensor_copy(out=cwT, in_=pb0[:, 0:NCH * 4].rearrange("p (c k) -> p c k", k=4)),
             reads=[self.Rbank[0]], writes=[Rcw, self.Rbank[0]])
        diagW = self.sb([128, NCH, 4, 128], BF16)
        Rdg = P.R("diagW")
        for c in range(NCH):
            P.op("dve", lambda e, c=c: e.tensor_tensor(
                out=diagW[:, c, :, :], in0=self.identF.unsqueeze(1).to_broadcast([128, 4, 128]),
                in1=cwT[:, c, :].unsqueeze(2).to_broadcast([128, 4, 128]), op=ALU.mult),
                reads=[Rcw, self.Rc], writes=[Rdg])
        xT = [self.sb([128, 4, KC, 128], BF16) for _ in range(2)]
        RxTl = P.Rs("a0x_xT", 2)
        uT = [self.sb([128, NCH, 515], BF16) for _ in range(2)]
        RuT = P.Rs("a0x_uT", 2)
        xs_tok = [self.sb([128, 2048], BF16) for _ in range(2)]
        Rxs = P.Rs("a0x_xs", 2)
        b_tok = [self.sb([128, 512], BF16) for _ in range(2)]
        Rbt = P.Rs("a0x_bt", 2)
        bct = [self.sb([128, 8, 128], BF16) for _ in range(2)]
        Rbct = P.Rs("a0x_bct", 2)
        oB = self.onesB
        for nb in range(NB):
            ub = nb % 2
            P.dma("sp", xT[ub], xT_d[nb * 4:(nb + 1) * 4].rearrange("t p k c -> p t k c"), reads=[RxT_d],
                  writes=[RxTl[ub]])
            for zi, z in enumerate(zfill):
                if min(zi, NB - 1) == nb:
                    self.zero_fill(*z)
            if nb == 0:
                P.op("pool", lambda e, ub=ub: e.memset(uT[ub][:, :, 0:3], 0.0), writes=[RuT[ub]])
            else:
                P.op("pool", lambda e, ub=ub: e.tensor_copy(out=uT[ub][:, :, 0:3], in_=uT[1 - ub][:, :, 512:515]),
                     reads=[RuT[1 - ub]], writes=[RuT[ub]])
            for c in range(NCH):
                bi = c % 2
                pu = self.bank(bi)
                Rpu = self.Rbank[bi]
                for k in range(KC):
                    P.op("pe", lambda e, pu=pu, k=k, c=c, ub=ub: e.matmul(
                        pu, winx[:, k, c * 128:(c + 1) * 128], xT[ub][:, :, k, :], start=(k == 0),
                        stop=(k == KC - 1)), reads=[Rwx[c // 8], RxTl[ub]], writes=[Rpu])
                if c % 2 == 0:
                    P.op("act", lambda e, pu=pu, c=c, ub=ub: e.copy(out=uT[ub][:, c, 3:515], in_=pu),
                         reads=[Rpu], writes=[RuT[ub], Rpu])
                else:
                    P.op("dve", lambda e, pu=pu, c=c, ub=ub: e.tensor_copy(out=uT[ub][:, c, 3:515], in_=pu),
                         reads=[Rpu], writes=[RuT[ub], Rpu])
            for s in range(4):
                t = nb * 4 + s
                ob = t % 2
                if wsteps is not None:
                    st_ = next(wsteps, None)
                    if st_ is not None:
                        st_()
                for q in range(5):
                    bi = 2 + q % 2
                    pc = self.bank(bi)
                    Rpc = self.Rbank[bi]
                    for cc in range(4):
                        c = q * 4 + cc
                        for k in range(4):
                            P.op("pe", lambda e, pc=pc, cc=cc, c=c, k=k, s=s, ub=ub: e.matmul(
                                pc[:, cc * 128:(cc + 1) * 128], uT[ub][:, c, s * 128 + k:s * 128 + k + 128],
                                diagW[:, c, k, :], start=(k == 0), stop=False),
                                reads=[RuT[ub], Rdg], writes=[Rpc])
                        P.op("pe", lambda e, pc=pc, cc=cc, c=c: e.matmul(
                            pc[:, cc * 128:(cc + 1) * 128], oB[0:1, 0:128], cbrowB[0:1, c * 128:(c + 1) * 128],
                            start=False, stop=True), reads=[Rcb, self.Rc], writes=[Rpc])
                    if q < 4:
                        P.op("act", lambda e, pc=pc, q=q, ob=ob: e.activation(
                            out=xs_tok[ob][:, q * 512:(q + 1) * 512], in_=pc, func=AF.Silu),
                            reads=[Rpc], writes=[Rxs[ob], Rpc])
                    else:
                        P.op("act", lambda e, pc=pc, ob=ob: e.activation(out=b_tok[ob], in_=pc, func=AF.Silu),
                             reads=[Rpc], writes=[Rbt[ob], Rpc])
                P.store(self.dq(), xs_d[t], xs_tok[ob], Rxs[ob], Rxs_d)
                P.store(self.dq(), bt_d[t], b_tok[ob], Rbt[ob], Rbt_d)
                for q2 in range(2):
                    bi = 4 + q2
                    pf = self.bank(bi)
                    Rpf = self.Rbank[bi]
                    for cc in range(4):
                        c = 16 + q2 * 4 + cc
                        for k in range(4):
                            P.op("pe", lambda e, pf=pf, cc=cc, c=c, k=k, s=s, ub=ub: e.matmul(
                                pf[:, cc * 128:(cc + 1) * 128], diagW[:, c, k, :],
                                uT[ub][:, c, s * 128 + k:s * 128 + k + 128], start=(k == 0), stop=False),
                                reads=[RuT[ub], Rdg], writes=[Rpf])
                        P.op("pe", lambda e, pf=pf, cc=cc, c=c: e.matmul(
                            pf[:, cc * 128:(cc + 1) * 128], cbrowB[0:1, c * 128:(c + 1) * 128], oB[0:1, 0:128],
                            start=False, stop=True), reads=[Rcb, self.Rc], writes=[Rpf])
                    P.op("act", lambda e, pf=pf, q2=q2, ob=ob: e.activation(
                        out=bct[ob][:, q2 * 4:(q2 + 1) * 4, :], in_=pf.rearrange("p (a b) -> p a b", a=4),
                        func=AF.Silu), reads=[Rpf], writes=[Rbct[ob], Rpf])
                P.store(self.dq(), bct_d[t], bct[ob], Rbct[ob], Rbct_d)
        if wsteps is not None:
            for st_ in wsteps:
                st_()
        P.barrier()
        self.off = mark

    def phase_b0(self, x_d, sz_d, dt_d, xs_d, bt_d, bct_d, Rin, alog_d, dskip_d, normw_d, wout_d, lng_d, lnb_d,
                 x1_d, Rx1_d, x1T_d, Rx1T_d, comb_d, Rcomb_d):
        P = self.P
        NT = self.NT
        mark = self.off
        iB, mU, mS, oF = self.identB, self.maskU, self.maskSL, self.onesF
        Rc = self.Rc
        wout = self.sb([128, 16, D], BF16)
        Rwo = P.R("wout")
        for q in range(4):
            P.dma("pool", wout[:, q * 4:(q + 1) * 4, :],
                  wout_d[0, q * 512:(q + 1) * 512, :].rearrange("(k p) n -> p k n", p=128), writes=[Rwo])
        normw_b, Rnw = self.bcast_load(normw_d[0:1, :], D_INNER, "sp")
        g_b, Rg = self.bcast_load(lng_d[0:1, :], D, "act")
        b_b, Rb_ = self.bcast_load(lnb_d[0:1, :], D, "sp")
        dsk_b, Rdsk = self.bcast_load(dskip_d[0:1, :], NH, "sp")
        Rgb = P.R("b0_gb")
        st = self.sb([128, 2, 6], F32)
        mv = self.sb([128, 8], F32)
        Rs = P.R("b0_ln")
        P.op("pool", lambda e: e.tensor_copy(out=mv[:, 7:8], in_=g_b[:, 0:1]), reads=[Rg, Rb_], writes=[Rgb])
        hT = self.sb([128, D_INNER], F32)
        hTb = [self.sb([128, D_INNER], BF16) for _ in range(3)]
        RhT = P.R("hT")
        RhTb = P.Rs("hTb", 3)
        P.op("pool", lambda e: e.memset(hT, 0.0), writes=[RhT])
        P.op("pool", lambda e: e.memset(hTb[0], 0.0), writes=[RhTb[0]])
        NLB = 3
        xs = [self.sb([128, D_INNER], BF16) for _ in range(2)]
        btok = [self.sb([128, 512], BF16) for _ in range(2)]
        RldF = P.Rs("b0_ldf", 2)
        bct = [self.sb([128, 8, 128], BF16) for _ in range(NLB)]
        sz = [self.sb([128, D_INNER], BF16) for _ in range(NLB)]
        xr = [self.sb([128, D], F32) for _ in range(NLB)]
        Rld = P.Rs("b0_ld", NLB)
        sm = [self.sb([128, 256], F32) for _ in range(NLB)]
        Rsm = P.Rs("b0_sm", NLB)
        xsdt = [self.sb([128, D_INNER], BF16) for _ in range(2)]
        xsD = [self.sb([128, D_INNER], BF16) for _ in range(2)]
        Rxsdt, RxsD = P.Rs("xsdt", 2), P.Rs("xsD", 2)
        xw = self.sb([128, D_INNER], BF16)
        Rxw = P.R("xw")
        rhsS = self.sb([128, NH // 2, 128], F32)
        RrhsS = P.R("rhsS")
        E = self.sb([128, NH, 128], BF16)
        RE = P.R("E")
        cbm = self.sb([128, NG, 128], BF16)
        Rcbm = P.R("cbm")
        MT = [self.sb([128, NH, 128], BF16) for _ in range(2)]
        RMT = P.Rs("MT", 2)
        y = self.sb([128, D_INNER], F32)
        Ry = P.Rs("y", NG)
        junk = self.sb([128, 512], BF16)
        Rjunk = P.R("junk")
        yn = self.sb([128, D_INNER], BF16)
        Ryn = P.R("yn")
        ynT = self.sb([128, 16, 128], BF16)
        RynT = P.R("ynT")
        r1 = self.sb([128, D], F32)
        r = [r1, r1]
        Rr1 = P.R("b0_r")
        Rr = [Rr1, Rr1]
        xTb = [self.sb([128, KC, 128], BF16) for _ in range(2)]
        RxTb = P.Rs("b0_xTb", 2)
        xTf1 = self.sb([128, KC, 128], F32)
        xTf = [xTf1, xTf1]
        RxTf1 = P.R("b0_xTf")
        RxTf = [RxTf1, RxTf1]
        cb = [self.sb([128, NE], F32) for _ in range(2)]
        Rcb = P.Rs("b0_cb", 2)

        def v64(a):
            return a.rearrange("p (h q) -> p h q", q=64)

        def bh(a, n=NH):
            return a.unsqueeze(2).to_broadcast([128, n, 64])

        def smv(b):
            s_ = sm[b]
            return dict(s=s_, dt=s_[:, 0:32], da=s_[:, 32:64], ecum=s_[:, 64:96], etot=s_[:, 96:128],
                        wexp=s_[:, 128:160], ss=s_[:, 192:196], rstd=s_[:, 196:200], tmp=s_[:, 200:204])

        def loads(t):
            lb = t % NLB
            P.dma("sp", xs[t % 2], xs_d[t], reads=Rin, writes=[RldF[t % 2]])
            P.dma("sp", btok[t % 2], bt_d[t], reads=Rin, writes=[RldF[t % 2]])
            P.dma("sp", bct[lb], bct_d[t], reads=Rin, writes=[Rld[lb]])
            P.dma("sp", sz[lb], sz_d[t], reads=Rin, writes=[Rld[lb]])
            P.dma("sp", xr[lb], x_d[t * 128:(t + 1) * 128, :], writes=[Rld[lb]])
            P.dma("sp", sm[lb][:, 0:160], dt_d[:, t].rearrange("p a h -> p (a h)"), reads=Rin, writes=[Rsm[lb]])

        def front(t):
            b = t % 2
            lb = t % NLB
            v = smv(lb)
            s_, da, etot, wexp, dtt = v["s"], v["da"], v["etot"], v["wexp"], v["dt"]
            Rdt = Rsm[lb]
            P.op("dve", lambda e: e.tensor_tensor(out=v64(xsdt[b]), in0=v64(xs[b]), in1=bh(dtt), op=ALU.mult),
                 reads=[RldF[b], Rdt], writes=[Rxsdt[b]])
            P.op("pool", lambda e: e.tensor_tensor(out=v64(xsD[b]), in0=v64(xs[b]), in1=bh(dsk_b), op=ALU.mult),
                 reads=[RldF[b], Rdsk], writes=[RxsD[b]])
            P.op("dve", lambda e: e.tensor_tensor(out=v64(xw), in0=v64(xsdt[b]), in1=bh(wexp), op=ALU.mult),
                 reads=[Rxsdt[b], Rsm[lb]], writes=[Rxw])
            for q in range(8):
                if q % 4 == 0:
                    hh0 = (q // 4) * 16
                    P.op("dve", lambda e, hh0=hh0: e.tensor_tensor(
                        out=rhsS, in0=mU.unsqueeze(1).to_broadcast([128, NH // 2, 128]),
                        in1=da[:, hh0:hh0 + 16].unsqueeze(2).to_broadcast([128, NH // 2, 128]), op=ALU.mult),
                        reads=[Rsm[lb], Rc], writes=[RrhsS])
                bi = 1 + q % 2
                pq = self.bank(bi)
                Rpq = self.Rbank[bi]
                P.op("pe", lambda e, pq=pq, q=q: e.matmul(pq, mS, rhsS[:, 4 * (q % 4):4 * (q % 4) + 4, :],
                                                           start=True, stop=True),
                     reads=[RrhsS, Rc], writes=[Rpq])
                P.op("act", lambda e, pq=pq, q=q: e.activation(
                    out=E[:, 4 * q:4 * q + 4, :], in_=pq.rearrange("p (a b) -> p a b", a=4), func=AF.Exp),
                    reads=[Rpq], writes=[RE, Rpq])
            b3 = self.bank(3)
            Rb3 = self.Rbank[3]
            for g in range(NG):
                P.op("pe", lambda e, g=g: e.matmul(b3[:, g * 128:(g + 1) * 128], bct[lb][:, g, :],
                                                    bct[lb][:, 4 + g, :], start=True, stop=True),
                     reads=[Rld[lb]], writes=[Rb3])
            P.op("dve", lambda e: e.tensor_tensor(out=cbm, in0=b3.rearrange("p (a b) -> p a b", a=4),
                                                  in1=mU.unsqueeze(1).to_broadcast([128, NG, 128]), op=ALU.mult),
                 reads=[Rb3, Rc], writes=[Rcbm, Rb3])
            for g in range(NG):
                eng = "dve" if g != 3 else "pool"
                P.op(eng, lambda e, g=g: e.tensor_tensor(
                    out=MT[b][:, g * 8:(g + 1) * 8, :], in0=E[:, g * 8:(g + 1) * 8, :],
                    in1=cbm[:, g:g + 1, :].to_broadcast([128, 8, 128]), op=ALU.mult),
                    reads=[RE, Rcbm], writes=[RMT[b]])
            hn = (t + 1) % 3
            for g in range(NG):
                gs = slice(g * 512, (g + 1) * 512)
                ph = self.bank(0)
                Rph = self.Rbank[0]
                P.op("pe", lambda e, g=g, gs=gs: e.matmul(ph, btok[b][:, g * 128:(g + 1) * 128], xw[:, gs],
                                                           start=True, stop=True),
                     reads=[RldF[b], Rxw], writes=[Rph])
                P.op("dve", lambda e, gs=gs, g=g: e.tensor_tensor(
                    out=v64(hT[:, gs]), in0=v64(hT[:, gs]), in1=bh(etot[:, g * 8:(g + 1) * 8], 8), op=ALU.mult),
                    reads=[RhT, Rsm[lb]], writes=[RhT])
                P.op("dve", lambda e, gs=gs: e.tensor_tensor(out=hT[:, gs], in0=ph, in1=hT[:, gs], op=ALU.add),
                     reads=[RhT, Rph], writes=[RhT, Rph])
                P.op("act", lambda e, gs=gs: e.copy(out=hTb[hn][:, gs], in_=hT[:, gs]), reads=[RhT],
                     writes=[RhTb[hn]])

        def back(t):
            b = t % 2
            lb = t % NLB
            hc = t % 3
            v = smv(lb)
            ecum, ss, rstd, tmp = v["ecum"], v["ss"], v["rstd"], v["tmp"]
            for g in range(NG):
                py = self.bank(4 + 2 * (g % 2))
                Rpy = self.Rbank[4 + 2 * (g % 2)]
                pys = self.bank(5 + 2 * (g % 2))
                Rpys = self.Rbank[5 + 2 * (g % 2)]
                gs = slice(g * 512, (g + 1) * 512)
                P.op("pe", lambda e, py=py, gs=gs: e.matmul(py, iB, xsD[b][:, gs], start=True, stop=False),
                     reads=[RxsD[b], Rc], writes=[Rpy])
                for hl in range(8):
                    h = g * 8 + hl
                    P.op("pe", lambda e, py=py, hl=hl, h=h: e.matmul(
                        py[:, hl * 64:(hl + 1) * 64], MT[b][:, h, :], xsdt[b][:, h * 64:(h + 1) * 64], start=False,
                        stop=(hl == 7)), reads=[RMT[b], Rxsdt[b]], writes=[Rpy])
                P.op("pe", lambda e, pys=pys, g=g, gs=gs: e.matmul(pys, bct[lb][:, 4 + g, :], hTb[hc][:, gs],
                                                                    start=True, stop=True),
                     reads=[Rld[lb], RhTb[hc]], writes=[Rpys])
                P.op("dve", lambda e, pys=pys, gs=gs, g=g: e.tensor_tensor(
                    out=v64(y[:, gs]), in0=v64(pys), in1=bh(ecum[:, g * 8:(g + 1) * 8], 8), op=ALU.mult),
                    reads=[Rpys, Rsm[lb]], writes=[Ry[g], Rpys])
                P.op("dve", lambda e, py=py, gs=gs: e.tensor_tensor(out=y[:, gs], in0=py, in1=y[:, gs], op=ALU.add),
                     reads=[Rpy, Ry[g]], writes=[Ry[g], Rpy])
                P.op("dve", lambda e, gs=gs: e.tensor_tensor(out=y[:, gs], in0=y[:, gs], in1=sz[lb][:, gs],
                                                              op=ALU.mult),
                     reads=[Ry[g], Rld[lb]], writes=[Ry[g]])
                P.op("act", lambda e, gs=gs, g=g: e.activation(out=junk, in_=y[:, gs], func=AF.Square,
                                                                accum_out=ss[:, g:g + 1]),
                     reads=[Ry[g]], writes=[Rjunk, Rsm[lb]])
            P.op("dve", lambda e: e.tensor_scalar(out=tmp, in0=ss, scalar1=1.0 / 512.0, scalar2=RMS_EPS,
                                                  op0=ALU.mult, op1=ALU.add), reads=[Rsm[lb]], writes=[Rsm[lb]])
            P.op("act", lambda e: e.activation(out=tmp, in_=tmp, func=AF.Sqrt), reads=[Rsm[lb]], writes=[Rsm[lb]])
            P.op("dve", lambda e: e.reciprocal(out=rstd, in_=tmp), reads=[Rsm[lb]], writes=[Rsm[lb]])
            for g in range(NG):
                gs = slice(g * 512, (g + 1) * 512)
                P.op("dve", lambda e, gs=gs, g=g: e.scalar_tensor_tensor(
                    out=yn[:, gs], in0=y[:, gs], scalar=rstd[:, g:g + 1], in1=normw_b[:, gs], op0=ALU.mult,
                    op1=ALU.mult), reads=[Ry[g], Rsm[lb], Rnw], writes=[Ryn])
            for hh in range(2):
                pt = self.bank(4 + hh, BF16)
                Rpt = self.Rbank[4 + hh]
                for k in range(8):
                    kk = hh * 8 + k
                    P.op("pe", lambda e, pt=pt, k=k, kk=kk: e.transpose(pt[:, k * 128:(k + 1) * 128],
                                                                        yn[:, kk * 128:(kk + 1) * 128], iB),
                         reads=[Ryn, Rc], writes=[Rpt])
                P.op("act", lambda e, pt=pt, hh=hh: e.copy(out=ynT[:, hh * 8:(hh + 1) * 8, :],
                                                           in_=pt.rearrange("p (a b) -> p a b", a=8)),
                     reads=[Rpt], writes=[RynT, Rpt])
            for n in range(2):
                pm = self.bank(6 + n)
                Rpm = self.Rbank[6 + n]
                ns = slice(n * 512, (n + 1) * 512)
                for k in range(16):
                    P.op("pe", lambda e, pm=pm, k=k, ns=ns: e.matmul(pm, ynT[:, k, :], wout[:, k, ns],
                                                                      start=(k == 0), stop=(k == 15)),
                         reads=[RynT, Rwo], writes=[Rpm])
                P.op("dve", lambda e, pm=pm, ns=ns: e.scalar_tensor_tensor(
                    out=r[b][:, ns], in0=xr[lb][:, ns], scalar=ALPHA, in1=pm, op0=ALU.mult, op1=ALU.add),
                    reads=[Rld[lb], Rpm], writes=[Rr[b], Rpm])
            self.layer_norm(r[b], Rr[b], st, mv, Rs, g_b, b_b, Rgb)
            P.store("sp", x1_d[t * 128:(t + 1) * 128, :], r[b], Rr[b], Rx1_d)
            self.make_xT(r[b], Rr[b], 4, xTb[b], RxTb[b], xTf[b], RxTf[b])
            P.store("sp", x1T_d[t], xTb[b], RxTb[b], Rx1T_d)
            self.router(xTf[b], RxTf[b], 6, cb[b], Rcb[b])
            P.store("sp", comb_d[t], cb[b], Rcb[b], Rcomb_d)

        loads(0)
        if NT > 1:
            loads(1)
        front(0)
        for t in range(NT):
            if t + 2 < NT:
                loads(t + 2)
            m0 = P.mark()
            if t + 1 < NT:
                front(t + 1)
            m1 = P.mark()
            back(t)
            P.interleave(m0, m1)
        P.barrier()
        self.off = mark

    def attn_bias_setup(self, rel_d, frep_d, expb_d, Rexpb_d):
        P = self.P
        expB = self.sb([128, AH, 5, 128], BF16)
        Rbias = P.R("bias_build")
        tab = self.sb([128, 2, AH], F32)
        Rtab = P.R("tab")
        P.dma("sp", tab, rel_d[0, 1:257, :].rearrange("(a p) h -> p a h", p=128), writes=[Rtab])
        FT = self.sb([AH, 768], F32)
        RFT = P.R("FT")
        pb = self.bank(0)
        Rpb = self.Rbank[0]
        for a in range(2):
            P.op("pe", lambda e, a=a: e.transpose(pb[0:AH, a * 128:(a + 1) * 128], tab[:, a, :], self.identF),
                 reads=[Rtab, self.Rc], writes=[Rpb])
        P.op("dve", lambda e: e.tensor_copy(out=FT[:, 0:256], in_=pb[0:AH, 0:256]), reads=[Rpb], writes=[RFT, Rpb])
        P.op("dve", lambda e: e.tensor_copy(out=FT[:, 256:768], in_=FT[:, 255:256].to_broadcast([AH, 512])),
             reads=[RFT], writes=[RFT])
        Rfrep = P.R("frep")
        P.store("sp", frep_d, FT.unsqueeze(1).to_broadcast([AH, 128, 768]), RFT, Rfrep)
        stage = self.sb([128, AH, 5, 128], F32)
        Rst = P.R("bstage")
        for h in range(AH):
            src = bass.AP(frep_d.tensor, h * 128 * 768 + 127, [[767, 128], [128, 5], [1, 128]])
            P.dma(("sp", "act")[h % 2], stage[:, h, :, :], src, reads=[Rfrep], writes=[Rst])
        P.op("pool", lambda e: e.memset(stage[0:64, :, 4, 64:128], -30000.0), reads=[Rst], writes=[Rst])
        P.op("pool", lambda e: e.memset(stage[64:128, :, 0, 0:64], -30000.0), reads=[Rst], writes=[Rst])
        for jr in range(5):
            P.op("act", lambda e, jr=jr: e.activation(out=expB[:, :, 4 - jr, :], in_=stage[:, :, jr, :],
                                                      func=AF.Exp), reads=[Rst], writes=[Rbias])
        P.store("sp", expb_d, expB, Rbias, Rexpb_d)

    def phase_attn(self, x_d, xT_d, Rin, wqkv_d, bqkv_d, wo_d, bo_d, rel_d, frep_d, lng_d, lnb_d,
                   x3_d, Rx3_d, x3T_d, Rx3T_d, comb_d, Rcomb_d, wconv=None):
        P = self.P
        NT = self.NT
        mark = self.off
        wsteps = wconv() if wconv is not None else None
        self.expB = self.sb([128, AH, 5, 128], BF16)
        self.Rbias = P.R("bias")
        P.dma("sp", self.expB, rel_d, reads=[frep_d], writes=[self.Rbias])
        iB, oB, Rc = self.identB, self.onesB, self.Rc
        RS = 6
        wqkv = self.sb([128, KC, 3 * D], BF16)
        Rwq = P.Rs("wqkv", 3)
        for j in range(6):
            P.dma("pool", wqkv[:, :, j * 512:(j + 1) * 512],
                  wqkv_d[0, :, j * 512:(j + 1) * 512].rearrange("(k p) n -> p k n", p=128), writes=[Rwq[j // 2]])
        wo = self.sb([128, KC, D], BF16)
        Rwo = P.R("wo")
        P.dma("pool", wo, wo_d[0].rearrange("(k p) n -> p k n", p=128), writes=[Rwo])
        browB = self.sb([1, 4 * D], BF16)
        Rbr = P.R("brow")
        P.dma("pool", browB[:, 0:3 * D], bqkv_d[0:1, :], writes=[Rbr])
        P.dma("pool", browB[:, 3 * D:4 * D], bo_d[0:1, :], writes=[Rbr])
        g_b, Rg = self.bcast_load(lng_d[1:2, :], D, "act")
        b_b, Rb_ = self.bcast_load(lnb_d[1:2, :], D, "sp")
        Rgb = P.R("at_gb")
        st = self.sb([128, 2, 6], F32)
        mv = self.sb([128, 8], F32)
        Rs = P.R("at_ln")
        P.op("pool", lambda e: e.tensor_copy(out=mv[:, 7:8], in_=g_b[:, 0:1]), reads=[Rg, Rb_], writes=[Rgb])
        KT = self.sb([128, KC, RS, 128], BF16)
        RK = P.Rs("KTr", RS)
        V = self.sb([128, RS, AH, 65], BF16)
        RV = P.Rs("Vr", RS)
        for sl in range(RS):
            P.op("pool", lambda e, sl=sl: e.memset(V[:, sl, :, 64:65], 1.0), writes=[RV[sl]])
        QT = [self.sb([128, KC, 128], BF16) for _ in range(2)]
        RQ = P.Rs("QT", 2)
        xT = [self.sb([128, KC, 128], BF16) for _ in range(2)]
        xr = [self.sb([128, D], F32) for _ in range(2)]
        Rld = P.Rs("at_ld", 2)
        NPB = 4
        PT = [self.sb([128, 5 * 128], BF16) for _ in range(NPB)]
        RPT = P.Rs("PT", NPB)
        O = self.sb([128, D], BF16)
        RO = P.R("O")
        rec = self.sb([128, 2, 4], F32)
        Rrec = P.Rs("rec", 2)
        OT = self.sb([128, KC, 128], BF16)
        ROT = P.R("OT")
        r = [self.sb([128, D], F32) for _ in range(2)]
        Rr = P.Rs("at_r", 2)
        xTb = [self.sb([128, KC, 128], BF16) for _ in range(2)]
        RxTb = P.Rs("at_xTb", 2)
        xTf = [self.sb([128, KC, 128], F32) for _ in range(2)]
        RxTf = P.Rs("at_xTf", 2)
        cb = [self.sb([128, NE], F32) for _ in range(2)]
        Rcb = P.Rs("at_cb", 2)
        hcount = [0]

        def proj(t):
            b = t % 2
            sl = t % RS
            P.dma("sp", xT[b], xT_d[t], reads=Rin, writes=[Rld[b]])
            P.dma("sp", xr[b], x_d[t * 128:(t + 1) * 128, :], reads=Rin, writes=[Rld[b]])
            for which in range(2):
                for hf in range(2):
                    bi = 6 + hf
                    pq = self.bank(bi)
                    Rpq = self.Rbank[bi]
                    for mm in range(4):
                        m = hf * 4 + mm
                        col = which * D + m * 128
                        for k in range(KC):
                            P.op("pe", lambda e, pq=pq, mm=mm, k=k, col=col: e.matmul(
                                pq[:, mm * 128:(mm + 1) * 128], wqkv[:, k, col:col + 128], xT[b][:, k, :],
                                start=(k == 0), stop=False), reads=[Rwq[col // 1024], Rld[b]], writes=[Rpq])
                        P.op("pe", lambda e, pq=pq, mm=mm, col=col: e.matmul(
                            pq[:, mm * 128:(mm + 1) * 128], browB[0:1, col:col + 128], oB[0:1, 0:128],
                            start=False, stop=True), reads=[Rbr, Rc], writes=[Rpq])
                    pv3 = pq.rearrange("p (a b) -> p a b", a=4)
                    if which == 0:
                        P.op("act", lambda e, pv3=pv3, hf=hf: e.mul(out=QT[b][:, hf * 4:(hf + 1) * 4, :], in_=pv3,
                                                                    mul=0.125),
                             reads=[Rpq], writes=[RQ[b], Rpq])
                    else:
                        P.op("dve", lambda e, pv3=pv3, hf=hf: e.tensor_copy(
                            out=KT[:, hf * 4:(hf + 1) * 4, sl, :], in_=pv3), reads=[Rpq], writes=[RK[sl], Rpq])
            for n in range(2):
                bi = 6 + n
                pv = self.bank(bi)
                Rpv = self.Rbank[bi]
                for k in range(KC):
                    P.op("pe", lambda e, pv=pv, k=k, n=n: e.matmul(
                        pv, xT[b][:, k, :], wqkv[:, k, 2 * D + n * 512:2 * D + (n + 1) * 512], start=(k == 0),
                        stop=False), reads=[Rwq[2], Rld[b]], writes=[Rpv])
                P.op("pe", lambda e, pv=pv, n=n: e.matmul(pv, oB[0:1, 0:128],
                                                          browB[0:1, 2 * D + n * 512:2 * D + (n + 1) * 512],
                                                          start=False, stop=True), reads=[Rbr, Rc], writes=[Rpv])
                if n == 0:
                    P.op("act", lambda e, pv=pv, n=n: e.copy(out=V[:, sl, n * 8:(n + 1) * 8, 0:64],
                                                            in_=pv.rearrange("p (a b) -> p a b", a=8)),
                         reads=[Rpv], writes=[RV[sl], Rpv])
                else:
                    P.op("dve", lambda e, pv=pv, n=n: e.tensor_copy(out=V[:, sl, n * 8:(n + 1) * 8, 0:64],
                                                                   in_=pv.rearrange("p (a b) -> p a b", a=8)),
                         reads=[Rpv], writes=[RV[sl], Rpv])

        def heads(t):
            b = t % 2
            nj = min(t, 4) + 1
            js = list(range(t - nj + 1, t + 1))

            def scores(h):
                m, p0 = h // 2, (h % 2) * 64
                pi = hcount[0] % 2
                pbuf = hcount[0] % NPB
                hcount[0] += 1
                pst = self.pair(pi)
                Rps = [self.Rbank[2 * pi], self.Rbank[2 * pi + 1]]
                for jj, j in enumerate(js):
                    ksl = j % RS
                    o_ = pst[:, jj * 128:(jj + 1) * 128]
                    wr = [Rps[0]] if jj < 4 else [Rps[1]]
                    P.op("pe", lambda e, o_=o_, ksl=ksl: e.matmul(
                        o_, KT[p0:p0 + 64, m, ksl, :], QT[b][p0:p0 + 64, m, :], start=True, stop=True),
                        reads=[RK[ksl], RQ[b]], writes=wr)
                wrs = Rps if nj == 5 else [Rps[0]]
                P.op("act", lambda e: e.activation(out=PT[pbuf][:, 0:nj * 128], in_=pst[:, 0:nj * 128], func=AF.Exp),
                     reads=wrs, writes=[RPT[pbuf]] + wrs)
                P.op(("pool" if (h % 2 and wsteps is None) else "dve"), lambda e: e.tensor_tensor(
                    out=PT[pbuf][:, 0:nj * 128], in0=PT[pbuf][:, 0:nj * 128],
                    in1=self.expB[:, h, 5 - nj:5, :].rearrange("p a b -> p (a b)"), op=ALU.mult),
                    reads=[RPT[pbuf], self.Rbias], writes=[RPT[pbuf]])
                return pbuf

            def pv_out(h, pbuf):
                hq, hl = h // 4, h % 4
                ob = 4 + hq % 2
                po = self.bank(ob)
                Rpo = self.Rbank[ob]
                for jj, j in enumerate(js):
                    ksl = j % RS
                    lastj = (jj == len(js) - 1)
                    P.op("pe", lambda e, jj=jj, ksl=ksl, lastj=lastj: e.matmul(
                        po[:, hl * 128:hl * 128 + 65], PT[pbuf][:, jj * 128:(jj + 1) * 128], V[:, ksl, h, :],
                        start=(jj == 0), stop=lastj), reads=[RPT[pbuf], RV[ksl]], writes=[Rpo])
                if hl == 3:
                    po3 = po.rearrange("p (a b) -> p a b", a=4)
                    rb = hq % 2
                    P.op("dve", lambda e: e.reciprocal(out=rec[:, rb, :].unsqueeze(2), in_=po3[:, :, 64:65]),
                         reads=[Rpo], writes=[Rrec[rb], Rpo])
                    P.op("dve", lambda e: e.tensor_tensor(
                        out=O[:, hq * 256:(hq + 1) * 256].rearrange("p (a b) -> p a b", a=4), in0=po3[:, :, 0:64],
                        in1=rec[:, rb, :].unsqueeze(2).to_broadcast([128, 4, 64]), op=ALU.mult),
                        reads=[Rpo, Rrec[rb]], writes=[RO, Rpo])

            LAG = 2
            pend = []
            for h in range(AH):
                pend.append((h, scores(h)))
                if len(pend) > LAG:
                    pv_out(*pend.pop(0))
            while pend:
                pv_out(*pend.pop(0))
            pt = self.bank(4, BF16)
            Rpt = self.Rbank[4]
            for k in range(KC):
                P.op("pe", lambda e, k=k: e.transpose(pt[:, k * 128:(k + 1) * 128], O[:, k * 128:(k + 1) * 128], iB),
                     reads=[RO, Rc], writes=[Rpt])
            P.op("act", lambda e: e.copy(out=OT, in_=pt.rearrange("p (a b) -> p a b", a=8)), reads=[Rpt],
                 writes=[ROT, Rpt])
            for n in range(2):
                pm = self.bank(n)
                Rpm = self.Rbank[n]
                ns = slice(n * 512, (n + 1) * 512)
                for k in range(KC):
                    P.op("pe", lambda e, pm=pm, k=k, ns=ns: e.matmul(pm, OT[:, k, :], wo[:, k, ns], start=(k == 0),
                                                                      stop=False), reads=[ROT, Rwo], writes=[Rpm])
                P.op("pe", lambda e, pm=pm, n=n: e.matmul(pm, oB[0:1, 0:128],
                                                          browB[0:1, 3 * D + n * 512:3 * D + (n + 1) * 512],
                                                          start=False, stop=True), reads=[Rbr, Rc], writes=[Rpm])
                P.op("dve", lambda e, pm=pm, ns=ns: e.scalar_tensor_tensor(
                    out=r[b][:, ns], in0=xr[b][:, ns], scalar=ALPHA, in1=pm, op0=ALU.mult, op1=ALU.add),
                    reads=[Rld[b], Rpm], writes=[Rr[b], Rpm])
            self.layer_norm(r[b], Rr[b], st, mv, Rs, g_b, b_b, Rgb)
            P.store(self.dq(), x3_d[t * 128:(t + 1) * 128, :], r[b], Rr[b], Rx3_d)
            self.make_xT(r[b], Rr[b], 2, xTb[b], RxTb[b], xTf[b], RxTf[b])
            P.store(self.dq(), x3T_d[t], xTb[b], RxTb[b], Rx3T_d)
            self.router(xTf[b], RxTf[b], 5, cb[b], Rcb[b])
            P.store(self.dq(), comb_d[t], cb[b], Rcb[b], Rcomb_d)

        proj(0)
        for t in range(NT):
            if wsteps is not None:
                for _ in range(2 if t % 2 == 0 else 1):
                    st_ = next(wsteps, None)
                    if st_ is not None:
                        st_()
            m0 = P.mark()
            if t + 1 < NT:
                proj(t + 1)
            m1 = P.mark()
            heads(t)
            P.interleave(m0, m1)
        if wsteps is not None:
            for st_ in wsteps:
                st_()
        P.barrier()
        self.off = mark


    def phase_moe_sparse(self, layer, x_d, comb_d, Rin, wg_d, wu_d, wd_d, lng_d, lnb_d, out_d, Rout,
                         outT_d, RoutT, xsort_d, ysort_d, Rzero, stop_after=9, dbg=None, wb=None, Rwb=None):
        P = self.P
        NT = self.NT
        mark = self.off
        ST = 512
        NSL = (2 * self.T) // ST + NE
        I32 = mybir.dt.int32
        iB, mU, oF, Rc = self.identB, self.maskU, self.onesF, self.Rc
        Rxsort, Rysort = P.R("xsort"), P.R("ysort")
        TE = NT * NE
        comb = self.sb([128, NT, NE], F32)
        M = self.sb([128, NT, NE], F32)
        M0 = self.sb([128, NT, NE], F32)
        M1 = self.sb([128, NT, NE], F32)
        T1 = self.sb([128, NT, NE], F32)
        Cc = self.sb([128, NT, NE], F32)
        tot = self.sb([128, NT, NE], F32)
        Pfx = self.sb([128, NT, NE], F32)
        w16 = self.sb([128, NE], F32)
        sml = self.sb([128, 256], F32)
        cmpT = self.sb([128, NE, 8], F32)
        cmpE = self.sb([128, NSL, NE], F32)
        posg = self.sb([128, 4, NT], F32)
        pos_i = self.sb([128, 2, NT], I32)
        esb = self.sb([128, NSL], F32)
        sthr = self.sb([128, NSL], F32)
        widx_i = self.sb([128, 2, NSL], I32)
        widxd_i = self.sb([128, 4, NSL], I32)
        esd = self.sb([128, NSL], F32)
        Rr_ = P.R("route")
        R1 = [Rr_]
        d = lambda fn, rd=(), wr=(): P.op("dve", fn, reads=R1 + list(rd), writes=R1 + list(wr))
        pl = lambda fn, rd=(), wr=(): P.op("pool", fn, reads=R1 + list(rd), writes=R1 + list(wr))
        P.dma("sp", comb, comb_d.rearrange("t p e -> p t e"), reads=Rin, writes=[Rr_])
        Rk = []
        for e_ in range(NE):
            Rk.append(P.R("k"))
            P.op("pool", lambda e, e_=e_: e.memset(w16[:, e_:e_ + 1], float(NE - e_)), writes=[Rk[-1]])
        for j in range(8):
            Rk.append(P.R("k"))
            P.op("pool", lambda e, j=j: e.memset(sml[:, 64 + j:65 + j], float(ST * j)), writes=[Rk[-1]])
        for s_ in range(NSL):
            Rk.append(P.R("k"))
            P.op("pool", lambda e, s_=s_: e.memset(sthr[:, s_:s_ + 1], float(ST * s_)), writes=[Rk[-1]])
        P.op("pool", lambda e: e.memset(sml[:, 120:121], 0.0), reads=Rk, writes=[Rr_])
        thr = sml[:, 64:72]
        n_e, ntile, npad, off, offend = (sml[:, 0:16], sml[:, 16:32], sml[:, 32:48], sml[:, 80:96], sml[:, 96:112])
        pidx = sml[:, 112:113]

        def bt(a):
            return a.unsqueeze(1).to_broadcast([128, NT, NE])

        def be(a):
            return a.unsqueeze(2).to_broadcast([128, NT, NE])

        self._Rroute = Rr_
        self.route_batched(comb, M, M0, M1, T1, posg.rearrange("p a t -> p (a t)").rearrange("p (a t) -> p a t", a=4)
                           if False else self.sb([128, 8, NT], F32), d)
        d(lambda e: e.tensor_single_scalar(out=M, in_=comb, scalar=0.0, op=ALU.is_gt))
        d(lambda e: e.tensor_tensor(out=T1, in0=M, in1=bt(w16), op=ALU.mult))
        d(lambda e: e.tensor_reduce(out=posg[:, 0, :], in_=T1, axis=AX.X, op=ALU.max))
        d(lambda e: e.tensor_tensor(out=M0, in0=T1, in1=be(posg[:, 0, :]), op=ALU.is_equal))
        d(lambda e: e.tensor_tensor(out=M1, in0=M, in1=M0, op=ALU.subtract))
        pbC, pbT = self.bank(0), self.bank(1)
        Mf = M.rearrange("p t e -> p (t e)")
        P.op("pe", lambda e: e.matmul(pbC[:, 0:TE], mU, Mf, start=True, stop=True), reads=[Rr_, Rc],
             writes=[self.Rbank[0]])
        P.op("pe", lambda e: e.matmul(pbT[:, 0:TE], oF, Mf, start=True, stop=True), reads=[Rr_, Rc],
             writes=[self.Rbank[1]])
        d(lambda e: e.tensor_copy(out=Cc.rearrange("p t e -> p (t e)"), in_=pbC[:, 0:TE]), rd=[self.Rbank[0]],
          wr=[self.Rbank[0]])
        d(lambda e: e.tensor_copy(out=tot.rearrange("p t e -> p (t e)"), in_=pbT[:, 0:TE]), rd=[self.Rbank[1]],
          wr=[self.Rbank[1]])
        d(lambda e: e.memset(Pfx[:, 0, :], 0.0))
        for t in range(1, NT):
            d(lambda e, t=t: e.tensor_tensor(out=Pfx[:, t, :], in0=Pfx[:, t - 1, :], in1=tot[:, t - 1, :], op=ALU.add))
        d(lambda e: e.tensor_tensor(out=n_e, in0=Pfx[:, NT - 1, :], in1=tot[:, NT - 1, :], op=ALU.add))
        d(lambda e: e.tensor_tensor(out=cmpT, in0=n_e.unsqueeze(2).to_broadcast([128, NE, 8]),
                                    in1=thr.unsqueeze(1).to_broadcast([128, NE, 8]), op=ALU.is_gt))
        d(lambda e: e.tensor_reduce(out=ntile, in_=cmpT, axis=AX.X, op=ALU.add))
        d(lambda e: e.tensor_scalar(out=npad, in0=ntile, scalar1=float(ST), scalar2=None, op0=ALU.mult))
        d(lambda e: e.memset(off[:, 0:1], 0.0))
        for e_ in range(1, NE):
            d(lambda e, e_=e_: e.tensor_tensor(out=off[:, e_:e_ + 1], in0=off[:, e_ - 1:e_], in1=npad[:, e_ - 1:e_],
                                               op=ALU.add))
        d(lambda e: e.tensor_tensor(out=offend, in0=off, in1=npad, op=ALU.add))
        d(lambda e: e.tensor_tensor(out=T1, in0=Cc, in1=M, op=ALU.subtract))
        d(lambda e: e.tensor_tensor(out=T1, in0=T1, in1=Pfx, op=ALU.add))
        d(lambda e: e.tensor_tensor(out=T1, in0=T1, in1=bt(off), op=ALU.add))
        d(lambda e: e.tensor_tensor(out=Cc, in0=T1, in1=M0, op=ALU.mult))
        d(lambda e: e.tensor_reduce(out=posg[:, 0, :], in_=Cc, axis=AX.X, op=ALU.add))
        d(lambda e: e.tensor_tensor(out=Cc, in0=T1, in1=M1, op=ALU.mult))
        d(lambda e: e.tensor_reduce(out=posg[:, 1, :], in_=Cc, axis=AX.X, op=ALU.add))
        d(lambda e: e.tensor_tensor(out=Cc, in0=comb, in1=M0, op=ALU.mult))
        d(lambda e: e.tensor_reduce(out=posg[:, 2, :], in_=Cc, axis=AX.X, op=ALU.add))
        d(lambda e: e.tensor_tensor(out=Cc, in0=comb, in1=M1, op=ALU.mult))
        d(lambda e: e.tensor_reduce(out=posg[:, 3, :], in_=Cc, axis=AX.X, op=ALU.add))
        d(lambda e: e.tensor_copy(out=pos_i, in_=posg[:, 0:2, :]))
        d(lambda e: e.tensor_tensor(out=cmpE, in0=offend.unsqueeze(1).to_broadcast([128, NSL, NE]),
                                    in1=sthr.unsqueeze(2).to_broadcast([128, NSL, NE]), op=ALU.is_le))
        d(lambda e: e.tensor_reduce(out=esb, in_=cmpE, axis=AX.X, op=ALU.add))
        d(lambda e: e.tensor_scalar(out=sthr, in0=esb, scalar1=float(NE) - 0.5, scalar2=65536.0, op0=ALU.is_gt,
                                    op1=ALU.mult))
        d(lambda e: e.tensor_scalar(out=esb, in0=esb, scalar1=float(NE - 1), scalar2=None, op0=ALU.min))
        d(lambda e: e.tensor_reduce(out=pidx, in_=mU, axis=AX.X, op=ALU.add), rd=[Rc])
        d(lambda e: e.tensor_scalar(out=pidx, in0=pidx, scalar1=-2.0, scalar2=float(2 * 128),
                                    op0=ALU.mult, op1=ALU.add))
        d(lambda e: e.tensor_scalar(out=esd, in0=esb, scalar1=512.0, scalar2=None, op0=ALU.mult))
        d(lambda e: e.tensor_tensor(out=esd, in0=esd, in1=sthr, op=ALU.add))
        d(lambda e: e.tensor_scalar(out=sml[:, 113:114], in0=pidx, scalar1=0.5, scalar2=None, op0=ALU.mult))
        for k in range(4):
            d(lambda e, k=k: e.tensor_scalar(out=cmpE[:, :, 0], in0=esd, scalar1=sml[:, 113:114], scalar2=float(k * 128),
                                             op0=ALU.add, op1=ALU.add))
            d(lambda e, k=k: e.tensor_copy(out=widxd_i[:, k, :], in_=cmpE[:, :, 0]))
        d(lambda e: e.tensor_scalar(out=esb, in0=esb, scalar1=128.0, scalar2=sml[:, 113:114], op0=ALU.mult,
                                    op1=ALU.add))
        d(lambda e: e.tensor_tensor(out=esb, in0=esb, in1=sthr, op=ALU.add))
        d(lambda e: e.tensor_copy(out=widx_i[:, 0, :], in_=esb))

        if dbg is not None:
            P.store("sp", dbg["pos"], pos_i, Rr_, dbg["R"])
            P.store("sp", dbg["widx"], widx_i, Rr_, dbg["R"])
            P.store("sp", dbg["posg"], posg, Rr_, dbg["R"])
        if stop_after < 2:
            P.barrier()
            self.off = mark
            return
        mark4 = self.off
        xf = [self.sb([128, D], F32) for _ in range(4)]
        Rxf = P.Rs("sp_xf", 4)
        xb = [self.sb([128, D], BF16) for _ in range(4)]
        Rxb = P.Rs("sp_xb", 4)
        for t in range(NT):
            b = t % 4
            P.dma("sp", xf[b], x_d[t * 128:(t + 1) * 128, :], reads=Rin, writes=[Rxf[b]])
            P.op("act", lambda e, b=b: e.copy(out=xb[b], in_=xf[b]), reads=[Rxf[b]], writes=[Rxb[b]])
            for k in range(2):
                i = Ins("pool", (lambda e, b=b, k=k, t=t: e.indirect_dma_start(
                    out=xsort_d[:, :], out_offset=bass.IndirectOffsetOnAxis(ap=pos_i[:, k, t:t + 1], axis=0),
                    in_=xb[b], in_offset=None)), (Rxb[b], Rr_, Rzero), (Rxsort,), True)
                i.key = Rxb[b]
                i.phase = P.phase
                P.ins.append(i)

        if stop_after < 2.4:
            P.barrier()
            self.off = mark
            return
        wg_v, wu_v, wd_v = wb
        wgs = [self.sb([128, KC, DFF], BF16) for _ in range(2)]
        wus = [self.sb([128, KC, DFF], BF16) for _ in range(2)]
        wds = [self.sb([128, 4, D], BF16) for _ in range(2)]
        Rwg, Rwu, Rwd = P.Rs("s_wg", 2), P.Rs("s_wu", 2), P.Rs("s_wd", 2)
        xs_sb = [self.sb([128, 4, D], BF16) for _ in range(2)]
        Rxs = P.Rs("s_xs", 2)
        xTs = [self.sb([128, KC, ST], BF16) for _ in range(2)]
        RxTs = P.Rs("s_xTs", 2)
        sg = [self.sb([128, 512], F32) for _ in range(2)]
        Rsg = P.Rs("s_sg", 2)
        hT = [self.sb([128, 4, 512], BF16) for _ in range(2)]
        RhT = P.Rs("s_hT", 2)
        ysb = [self.sb([128, D], F32) for _ in range(4)]
        Rysb = P.Rs("s_ysb", 4)

        def wgather(dst, src_v, s_, Rw):
            d2 = dst.rearrange("p k f -> p (k f)")
            i = Ins("pool", (lambda e: e.indirect_dma_start(
                out=d2, out_offset=None, in_=src_v,
                in_offset=bass.IndirectOffsetOnAxis(ap=widx_i[:, 0, s_:s_ + 1], axis=0),
                bounds_check=self._bc_reg(e), oob_is_err=False)),
                (Rr_, Rwb), (Rw,), True)
            i.key = Rw
            i.phase = P.phase
            P.ins.append(i)

        def fetch(s_):
            wb = s_ % 2
            wgather(wgs[wb], wg_v, s_, Rwg[wb])
            wgather(wus[wb], wu_v, s_, Rwu[wb])
            for k in range(4):
                i = Ins("pool", (lambda e, k=k: e.indirect_dma_start(
                    out=wds[wb][:, k, :], out_offset=None, in_=wd_v,
                    in_offset=bass.IndirectOffsetOnAxis(ap=widxd_i[:, k, s_:s_ + 1], axis=0),
                    bounds_check=self._bc_reg2(e), oob_is_err=False)), (Rr_, Rwb), (Rwd[wb],), True)
                i.key = Rwd[wb]
                i.phase = P.phase
                P.ins.append(i)
            P.dma("sp", xs_sb[wb], xsort_d[s_ * ST:(s_ + 1) * ST, :].rearrange("(a p) d -> p a d", p=128),
                  reads=[Rxsort], writes=[Rxs[wb]])

        yc = 0
        fetch(0)
        for s_ in range(NSL):
            wb = s_ % 2
            if s_ + 1 < NSL:
                fetch(s_ + 1)
            if stop_after < 2.6:
                continue
            for a in range(4):
                pt = self.bank(a % 2, BF16)
                Rpt = self.Rbank[a % 2]
                xv = xs_sb[wb][:, a, :].rearrange("p (c k) -> p k c", k=KC)
                for k in range(KC):
                    P.op("pe", lambda e, pt=pt, xv=xv, k=k: e.transpose(pt[:, k * 128:(k + 1) * 128], xv[:, k, :], iB),
                         reads=[Rxs[wb], Rc], writes=[Rpt])
                if a % 2 == 0:
                    P.op("act", lambda e, pt=pt, a=a, wb=wb: e.copy(
                        out=xTs[wb][:, :, a * 128:(a + 1) * 128], in_=pt.rearrange("p (k c) -> p k c", k=KC)),
                        reads=[Rpt], writes=[RxTs[wb], Rpt])
                else:
                    P.op("dve", lambda e, pt=pt, a=a, wb=wb: e.tensor_copy(
                        out=xTs[wb][:, :, a * 128:(a + 1) * 128], in_=pt.rearrange("p (k c) -> p k c", k=KC)),
                        reads=[Rpt], writes=[RxTs[wb], Rpt])
            if stop_after < 2.8:
                continue
            hb = s_ % 2
            for c in range(4):
                pg = self.bank(2)
                Rpg = self.Rbank[2]
                pu = self.bank(3)
                Rpu = self.Rbank[3]
                sgb = c % 2
                wgc = wgs[wb].rearrange("p k (c q) -> p k c q", c=4)
                wuc = wus[wb].rearrange("p k (c q) -> p k c q", c=4)
                for k in range(KC):
                    P.op("pe", lambda e, pg=pg, k=k, c=c, wgc=wgc, wb=wb: e.matmul(
                        pg, wgc[:, k, c, :], xTs[wb][:, k, :], start=(k == 0), stop=(k == KC - 1)),
                        reads=[Rwg[wb], RxTs[wb]], writes=[Rpg])
                for k in range(KC):
                    P.op("pe", lambda e, pu=pu, k=k, c=c, wuc=wuc, wb=wb: e.matmul(
                        pu, wuc[:, k, c, :], xTs[wb][:, k, :], start=(k == 0), stop=(k == KC - 1)),
                        reads=[Rwu[wb], RxTs[wb]], writes=[Rpu])
                P.op("act", lambda e, pg=pg, sgb=sgb: e.activation(out=sg[sgb], in_=pg, func=AF.Silu),
                     reads=[Rpg], writes=[Rsg[sgb], Rpg])
                P.op("dve", lambda e, pu=pu, sgb=sgb, hb=hb, c=c: e.tensor_tensor(
                    out=hT[hb][:, c, :], in0=pu, in1=sg[sgb], op=ALU.mult),
                    reads=[Rpu, Rsg[sgb]], writes=[RhT[hb], Rpu])
            for a in range(4):
                yb = yc % 4
                yc += 1
                for n in range(2):
                    bi = 4 + (a % 2) * 2 + n
                    po = self.bank(bi)
                    Rpo = self.Rbank[bi]
                    for c in range(4):
                        P.op("pe", lambda e, po=po, c=c, a=a, n=n, hb=hb, wb=wb: e.matmul(
                            po, hT[hb][:, c, a * 128:(a + 1) * 128], wds[wb][:, c, n * 512:(n + 1) * 512],
                            start=(c == 0), stop=(c == 3)), reads=[RhT[hb], Rwd[wb]], writes=[Rpo])
                    if n == 0:
                        P.op("act", lambda e, po=po, yb=yb: e.copy(out=ysb[yb][:, 0:512], in_=po),
                             reads=[Rpo], writes=[Rysb[yb], Rpo])
                    else:
                        P.op("dve", lambda e, po=po, yb=yb: e.tensor_copy(out=ysb[yb][:, 512:1024], in_=po),
                             reads=[Rpo], writes=[Rysb[yb], Rpo])
                r0 = s_ * ST + a * 128
                P.store("sp", ysort_d[r0:r0 + 128, :], ysb[yb], Rysb[yb], Rysort)

        if stop_after < 4:
            P.barrier()
            self.off = mark
            return
        P.barrier()
        self.off = mark4
        NB4 = 8
        g_b, Rg = self.bcast_load(lng_d[layer:layer + 1, :], D, "sp")
        b_b, Rb_ = self.bcast_load(lnb_d[layer:layer + 1, :], D, "sp")
        Rgb = P.R("sp_gb")
        dmy = self.sb([128, 8], F32)
        P.op("pool", lambda e: e.tensor_copy(out=dmy[:, 7:8], in_=g_b[:, 0:1]), reads=[Rg, Rb_], writes=[Rgb])
        st = [self.sb([128, 2, 6], F32) for _ in range(NB4)]
        mv = [self.sb([128, 8], F32) for _ in range(NB4)]
        Rs = P.Rs("sp_ln", NB4)
        acc = [self.sb([128, D], F32) for _ in range(NB4)]
        Racc = P.Rs("sp_acc", NB4)
        yg = [[self.sb([128, D], F32) for _ in range(NB4)] for _ in range(2)]
        Ryg = [P.Rs("sp_yg%d" % k, NB4) for k in range(2)]
        xTo = [self.sb([128, KC, 128], BF16) for _ in range(NB4)]
        RxTo = P.Rs("sp_xTo", NB4)

        def fetch4(t):
            b = t % NB4
            P.dma("sp", acc[b], x_d[t * 128:(t + 1) * 128, :], reads=Rin, writes=[Racc[b]])
            for k in range(2):
                i = Ins("pool", (lambda e, k=k: e.indirect_dma_start(
                    out=yg[k][b], out_offset=None, in_=ysort_d[:, :],
                    in_offset=bass.IndirectOffsetOnAxis(ap=pos_i[:, k, t:t + 1], axis=0))),
                    (Rysort, Rr_), (Ryg[k][b],), True)
                i.key = Ryg[k][b]
                i.phase = P.phase
                P.ins.append(i)

        def comb4(t):
            b = t % NB4
            for k in range(2):
                P.op("act", lambda e, k=k: e.activation(out=yg[k][b], in_=yg[k][b], func=AF.Identity,
                                                        scale=posg[:, 2 + k, t:t + 1]),
                     reads=[Ryg[k][b], Rr_], writes=[Ryg[k][b]])
            P.op("pool", lambda e: e.tensor_tensor(out=yg[0][b], in0=yg[0][b], in1=yg[1][b], op=ALU.add),
                 reads=[Ryg[0][b], Ryg[1][b]], writes=[Ryg[0][b]])
            P.op("dve", lambda e: e.scalar_tensor_tensor(out=acc[b], in0=acc[b], scalar=ALPHA, in1=yg[0][b],
                                                         op0=ALU.mult, op1=ALU.add),
                 reads=[Ryg[0][b], Racc[b]], writes=[Racc[b]])
            self.layer_norm(acc[b], Racc[b], st[b], mv[b], Rs[b], g_b, b_b, Rgb, eng2="dve")
            if outT_d is not None:
                self.make_xT(acc[b], Racc[b], 2 * (b % 4), xTo[b], RxTo[b])
            P.store("sp", out_d[t * 128:(t + 1) * 128, :], acc[b], Racc[b], Rout)
            if outT_d is not None:
                P.store("sp", outT_d[t], xTo[b], RxTo[b], RoutT)

        GS = 4
        for t in range(min(GS, NT)):
            fetch4(t)
        for t0 in range(0, NT, GS):
            ts = list(range(t0, min(NT, t0 + GS)))
            for t in ts:
                if t + GS < NT:
                    fetch4(t + GS)
            ms = []
            for t in ts:
                ms.append(P.mark())
                comb4(t)
            if len(ts) == 4:
                P.interleave(ms[2], ms[3])
                tail = P.ins[ms[2]:]
                del P.ins[ms[2]:]
                P.interleave(ms[0], ms[1])
                mid = len(P.ins)
                P.ins.extend(tail)
                P.interleave(ms[0], mid)
            elif len(ts) >= 2:
                P.interleave(ms[0], ms[1])
        P.barrier()
        self.off = mark


W_SHAPES = {
    "ssm_w_in": [1, D, SSD_IN], "ssm_conv_w": [1, 4, 3072], "ssm_conv_b": [1, 3072], "ssm_dt_bias": [1, NH],
    "ssm_a_log": [1, NH], "ssm_d": [1, NH], "ssm_norm_w": [1, D_INNER], "ssm_w_out": [1, D_INNER, D],
    "att_w_qkv": [1, D, 3 * D], "att_b_qkv": [1, 3 * D], "att_rel_bias": [1, 257, AH], "att_w_o": [1, D, D],
    "att_b_o": [1, D], "router_w": [D, NE], "router_bias": [NE],
    "moe_w_gate": [DEPTH, NE, D, DFF], "moe_w_up": [DEPTH, NE, D, DFF], "moe_w_down": [DEPTH, NE, DFF, D],
    "ln_mix_g": [DEPTH, D], "ln_mix_b": [DEPTH, D], "ln_ffn_g": [DEPTH, D], "ln_ffn_b": [DEPTH, D],
}


def build_full(T=4096, debug=False, sparse=True):
    B = Builder(T)
    NT = B.NT
    P = B.P
    x_d = B.dt_in("x", [T, D])
    w = {k: B.dt_in(k, v) for k, v in W_SHAPES.items()}
    scr = lambda n, shp, dt=F32: B.dt_scr(n, shp, dt, debug=debug)
    xT0 = scr("xT0", [NT, 128, KC, 128], BF16)
    sz = scr("sz", [NT, 128, D_INNER], BF16)
    dt = scr("dt", [128, NT, 5, NH], F32)
    xs = scr("xs", [NT, 128, D_INNER], BF16)
    bt = scr("bt", [NT, 128, 512], BF16)
    bct = scr("bct", [NT, 128, 8, 128], BF16)
    x1 = scr("x1", [T, D])
    x1T = scr("x1T", [NT, 128, KC, 128], BF16)
    c1 = scr("comb1", [NT, 128, NE])
    x2 = scr("x2", [T, D])
    x2T = scr("x2T", [NT, 128, KC, 128], BF16)
    x3 = scr("x3", [T, D])
    x3T = scr("x3T", [NT, 128, KC, 128], BF16)
    c3 = scr("comb3", [NT, 128, NE])
    y = B.dt_out("y", [T, D])
    R = {n: P.R("d_" + n) for n in "xT0 sz dt xs bt bct x1 x1T c1 x2 x2T x3 x3T c3 y".split()}
    frep = B.dt_scr("frep", [AH, 128, 768], F32)
    B.consts()
    B.router_setup(w["router_w"], w["router_bias"])
    NSLOT = ((2 * T) // 512 + NE) * 512
    xsort = [B.dt_scr("xsort%d" % l, [NSLOT, D], BF16) for l in range(2)]
    ysort = B.dt_scr("ysort", [NSLOT, D], F32)
    Rz = [P.R("zero%d" % l) for l in range(2)]
    expb = B.dt_scr("expb", [128, AH, 5, 128], BF16)
    Rexpb = P.R("d_expb")
    wbs = [(B.dt_scr("wgb%d" % l, [NE * 128, 4096], BF16), B.dt_scr("wub%d" % l, [NE * 128, 4096], BF16),
            B.dt_scr("wdb%d" % l, [NE * DFF, D], BF16)) for l in range(2)]
    Rwbs = [P.R("d_wb%d" % l) for l in range(2)]
    mkconv = lambda l, rng=None, nb=2: (lambda: B.wconv_steps(l, w["moe_w_gate"], w["moe_w_up"], w["moe_w_down"],
                                                           wbs[l], Rwbs[l], ex_range=rng, nbuf=nb))
    B.phase_a0z(x_d, w["ssm_w_in"], w["ssm_dt_bias"], w["ssm_a_log"], xT0, R["xT0"], sz, R["sz"], dt, R["dt"],
                early=lambda: B.attn_bias_setup(w["att_rel_bias"], frep, expb, Rexpb),
                wconv=(mkconv(0, None, 4) if sparse else None))
    B.phase_a0x(w["ssm_w_in"], w["ssm_conv_w"], w["ssm_conv_b"], xT0, R["xT0"], xs, R["xs"], bt, R["bt"], bct,
                R["bct"], zfill=([(xsort[0], Rz[0]), (xsort[1], Rz[1])] if sparse else ()),
                wconv=None)
    B.phase_b0(x_d, sz, dt, xs, bt, bct, [R["sz"], R["dt"], R["xs"], R["bt"], R["bct"]], w["ssm_a_log"], w["ssm_d"],
               w["ssm_norm_w"], w["ssm_w_out"], w["ln_mix_g"], w["ln_mix_b"], x1, R["x1"], x1T, R["x1T"], c1, R["c1"])
    if sparse:
        B.phase_moe_sparse(0, x1, c1, [R["x1"], R["c1"]], w["moe_w_gate"], w["moe_w_up"], w["moe_w_down"],
                           w["ln_ffn_g"], w["ln_ffn_b"], x2, R["x2"], x2T, R["x2T"], xsort[0], ysort, Rz[0],
                           wb=wbs[0], Rwb=Rwbs[0])
    else:
        B.phase_moe(0, x1, x1T, c1, [R["x1"], R["x1T"], R["c1"]], w["moe_w_gate"], w["moe_w_up"], w["moe_w_down"],
                    w["ln_ffn_g"], w["ln_ffn_b"], x2, R["x2"], x2T, R["x2T"])
    B.phase_attn(x2, x2T, [R["x2"], R["x2T"]], w["att_w_qkv"], w["att_b_qkv"], w["att_w_o"], w["att_b_o"],
                 expb, Rexpb, w["ln_mix_g"], w["ln_mix_b"], x3, R["x3"], x3T, R["x3T"], c3, R["c3"],
                 wconv=(mkconv(1) if sparse else None))
    if sparse:
        B.phase_moe_sparse(1, x3, c3, [R["x3"], R["c3"]], w["moe_w_gate"], w["moe_w_up"], w["moe_w_down"],
                           w["ln_ffn_g"], w["ln_ffn_b"], y, R["y"], None, None, xsort[1], ysort, Rz[1],
                           wb=wbs[1], Rwb=Rwbs[1])
    else:
        B.phase_moe(1, x3, x3T, c3, [R["x3"], R["x3T"], R["c3"]], w["moe_w_gate"], w["moe_w_up"], w["moe_w_down"],
                    w["ln_ffn_g"], w["ln_ffn_b"], y, R["y"])
    fw = [R["y"]]
    if debug:
        fw = list(R.values())
    P.emit(final_wait=fw)
    B.es.close()
    return B


def kernel(**inputs):
    x = np.ascontiguousarray(np.asarray(inputs["x"], dtype=np.float32))
    nb, T, _ = x.shape
    B = build_full(T)
    ws = {k: np.ascontiguousarray(np.asarray(inputs[k], dtype=np.float32)) for k in W_SHAPES}
    in_maps = []
    for c in range(nb):
        m = {"x": x[c]}
        m.update(ws)
        in_maps.append(m)
    res = run_bass_kernel_spmd(B.nc, in_maps, core_ids=list(range(nb)))
    return np.stack([np.asarray(r["y"], dtype=np.float32) for r in res.results], axis=0)
```
